# Optimizing a Trainium2 kernel written in Bass

```python
import jax, jax.numpy as jnp
from jax import lax
import numpy as np

D_MODEL = 1024
BATCH = 1
SEQ = 16384
DEPTH = 4

GRID_W = 64
CTX_LEN = 256

MLA_HEADS = 8
QK_NOPE = 64
QK_ROPE = 32
V_HEAD = 64
Q_LORA = 384
KV_LORA = 256
ROPE_BASE = 10000.0
Q_BLOCK = 128
MLA_SCALE = (QK_NOPE + QK_ROPE) ** -0.5

MLSTM_HEADS = 4
MQK = 64
MV = 128
CHUNK = 64
FGATE_BIAS_LO = 3.0
FGATE_BIAS_HI = 6.0

N_GROUPS = 4
EXPERTS_PER_GROUP = 8
N_EXPERTS = N_GROUPS * EXPERTS_PER_GROUP
TOP_K_IN_GROUP = 2
D_EXPERT = 256

DEEPNORM_ALPHA = (2 * DEPTH) ** 0.25
DEEPNORM_BETA = (8 * DEPTH) ** -0.25
LN_EPS = 1e-6

IN_SIZES = (Q_LORA, KV_LORA + QK_ROPE, MLSTM_HEADS * MQK, MLSTM_HEADS * MQK,
            MLSTM_HEADS * MV, MLSTM_HEADS * MV, 4 * MLSTM_HEADS, 2 * D_MODEL)
D_IN = sum(IN_SIZES)

F32 = jnp.float32

kernel_name = 'hybrid_mla_mlstm_hmoe_dit'


def layer_norm(x, g=None, b=None):
    xf = x.astype(F32)
    mu = xf.mean(-1, keepdims=True)
    var = jnp.square(xf - mu).mean(-1, keepdims=True)
    y = (xf - mu) * lax.rsqrt(var + LN_EPS)
    if g is not None:
        y = y * g + b
    return y.astype(x.dtype)


def rms_norm(x, g):
    xf = x.astype(F32)
    y = xf * lax.rsqrt(jnp.mean(xf * xf, -1, keepdims=True) + LN_EPS) * g
    return y.astype(x.dtype)


def modulate(x, shift, scale):
    return layer_norm(x) * (1 + scale) + shift


def rope_tables(rows):
    row, col = jnp.meshgrid(jnp.arange(rows, dtype=F32), jnp.arange(GRID_W, dtype=F32), indexing='ij')
    row, col = row.reshape(-1), col.reshape(-1)
    half = QK_ROPE // 2
    inv = ROPE_BASE ** (-jnp.arange(0, half, 2, dtype=F32) / half)
    ar, ac = row[:, None] * inv, col[:, None] * inv
    ang = jnp.concatenate([ar, ar, ac, ac], axis=-1)
    ang = jnp.concatenate([jnp.zeros((CTX_LEN, QK_ROPE), F32), ang], axis=0)
    return jnp.cos(ang), jnp.sin(ang)


def apply_rope(x, cos, sin):
    a1, a2, b1, b2 = jnp.split(x, 4, axis=-1)
    rot = jnp.concatenate([-a2, a1, -b2, b1], axis=-1)
    return (x * cos + rot * sin).astype(x.dtype)


def attend(qn, qr, kn, kr, v):
    s = jnp.einsum('bqhd,bkhd->bhqk', qn, kn) + jnp.einsum('bqhr,bkr->bhqk', qr, kr)
    p = jax.nn.softmax(s.astype(F32) * MLA_SCALE, axis=-1)
    return jnp.einsum('bhqk,bkhd->bqhd', p.astype(v.dtype), v)


def mla_branch(pqd, pkv, cos, sin, w_uq, w_uk, w_uv, g_qn, g_kvn):
    B, T, _ = pqd.shape
    S = T - CTX_LEN
    q = (rms_norm(pqd, g_qn) @ w_uq).reshape(B, T, MLA_HEADS, QK_NOPE + QK_ROPE)
    qn = q[..., :QK_NOPE]
    qr = apply_rope(q[..., QK_NOPE:], cos[:, None], sin[:, None])
    ckv = rms_norm(pkv[..., :KV_LORA], g_kvn)
    kr = apply_rope(pkv[..., KV_LORA:], cos, sin)
    kn = (ckv @ w_uk).reshape(B, T, MLA_HEADS, QK_NOPE)
    v = (ckv @ w_uv).reshape(B, T, MLA_HEADS, V_HEAD)
    oc = attend(qn[:, :CTX_LEN], qr[:, :CTX_LEN], kn[:, :CTX_LEN], kr[:, :CTX_LEN], v[:, :CTX_LEN])
    nb = S // Q_BLOCK

    def blocks(a):
        return jnp.moveaxis(a[:, CTX_LEN:].reshape(B, nb, Q_BLOCK, *a.shape[2:]), 1, 0)

    ol = lax.map(lambda qs: attend(qs[0], qs[1], kn, kr, v), (blocks(qn), blocks(qr)))
    ol = jnp.moveaxis(ol, 0, 1).reshape(B, S, MLA_HEADS * V_HEAD)
    return jnp.concatenate([oc.reshape(B, CTX_LEN, MLA_HEADS * V_HEAD), ol], axis=1)


def mlstm_chunked(q, k, v, ig, lf, state):
    B, H, T, _ = q.shape
    nc = T // CHUNK

    def chunks(a):
        return jnp.moveaxis(a.reshape(B, H, nc, CHUNK, *a.shape[3:]), 2, 0)

    tril = jnp.tril(jnp.ones((CHUNK, CHUNK), bool))

    def step(carry, xs):
        C, n, m = carry
        qc, kc, vc, ic, fc = xs
        b = jnp.cumsum(fc, axis=-1)
        dlog = jnp.where(tril, b[..., :, None] - b[..., None, :] + ic[..., None, :], -jnp.inf)
        inter = b + m[..., None]
        mj = jnp.maximum(inter, dlog.max(-1))
        w = jnp.exp(dlog - mj[..., None])
        sc = jnp.exp(inter - mj)
        qk = jnp.einsum('bhjd,bhsd->bhjs', qc, kc) * w
        num = jnp.einsum('bhjs,bhsv->bhjv', qk, vc) + sc[..., None] * jnp.einsum('bhjd,bhdv->bhjv', qc, C)
        nq = qk.sum(-1) + sc * jnp.einsum('bhjd,bhd->bhj', qc, n)
        h = num / jnp.maximum(jnp.abs(nq), jnp.exp(-mj))[..., None]
        bl = b[..., -1]
        wlog = bl[..., None] - b + ic
        m_new = jnp.maximum(bl + m, wlog.max(-1))
        ws = jnp.exp(wlog - m_new[..., None])
        sd = jnp.exp(bl + m - m_new)
        C_new = sd[..., None, None] * C + jnp.einsum('bhs,bhsd,bhsv->bhdv', ws, kc, vc)
        n_new = sd[..., None] * n + jnp.einsum('bhs,bhsd->bhd', ws, kc)
        return (C_new, n_new, m_new), h

    state, h = lax.scan(step, state, tuple(chunks(a) for a in (q, k, v, ig, lf)))
    return jnp.moveaxis(h, 0, 2).reshape(B, H, T, MV), state


def mlstm_branch(pq, pk, pv, po, pg, b_gates, g_mh):
    B, T, _ = pq.shape

    def heads(a):
        return jnp.moveaxis(a.reshape(B, T, MLSTM_HEADS, -1), 1, 2).astype(F32)

    q = heads(pq) * MQK ** -0.5
    k = heads(pk)
    v = heads(pv)
    gates = jnp.moveaxis((pg.astype(F32) + b_gates.astype(F32)).reshape(B, T, 4, MLSTM_HEADS), 1, 3)
    zero = (jnp.zeros((B, MLSTM_HEADS, MQK, MV), F32),
            jnp.zeros((B, MLSTM_HEADS, MQK), F32),
            jnp.zeros((B, MLSTM_HEADS), F32))

    def direction(ig, f_pre, reverse):
        lf = jax.nn.log_sigmoid(f_pre)

        def seg(a, lo, hi):
            a = a[:, :, lo:hi]
            return jnp.flip(a, axis=2) if reverse else a

        args = (q, k, v, ig, lf)
        hc, st = mlstm_chunked(*(seg(a, 0, CTX_LEN) for a in args), zero)
        hl, _ = mlstm_chunked(*(seg(a, CTX_LEN, T) for a in args), st)
        if reverse:
            hc, hl = jnp.flip(hc, axis=2), jnp.flip(hl, axis=2)
        return jnp.concatenate([hc, hl], axis=2)

    h = direction(gates[:, 0], gates[:, 1], False) + direction(gates[:, 2], gates[:, 3], True)
    h = layer_norm(h) * g_mh.reshape(MLSTM_HEADS, 1, MV)
    h = jnp.moveaxis(h, 1, 2).reshape(B, T, MLSTM_HEADS * MV)
    return (h * jax.nn.sigmoid(po.astype(F32))).astype(pq.dtype)


def token_mixer(hc, hl, cos, sin, w_in, b_gates, w_uq, w_uk, w_uv, g_qn, g_kvn, g_mh,
                w_bo_mla, w_bo_mlstm, w_out):
    h = jnp.concatenate([hc, hl], axis=1)
    p = h @ w_in
    pqd, pkv, pq, pk, pv, po, pg, pm = jnp.split(p, np.cumsum(IN_SIZES)[:-1].tolist(), axis=-1)
    y_mla = mla_branch(pqd, pkv, cos, sin, w_uq, w_uk, w_uv, g_qn, g_kvn) @ w_bo_mla
    y_mlstm = mlstm_branch(pq, pk, pv, po, pg, b_gates, g_mh) @ w_bo_mlstm
    g_a, g_b = jnp.split(jax.nn.sigmoid(pm), 2, axis=-1)
    y = (g_a * y_mla + g_b * y_mlstm) @ w_out
    return y[:, :CTX_LEN], y[:, CTX_LEN:]


def hier_moe(h, w_rg, b_rg, w_re, b_re, w_e_gate, w_e_up, w_e_down):
    B, N, _ = h.shape
    lg = (h @ w_rg + b_rg).astype(F32)
    grp = jnp.argmax(lg, axis=-1)
    p_grp = jnp.take_along_axis(jax.nn.softmax(lg, axis=-1), grp[..., None], axis=-1)
    le = (h @ w_re + b_re).astype(F32).reshape(B, N, N_GROUPS, EXPERTS_PER_GROUP)
    le = jnp.take_along_axis(le, grp[..., None, None], axis=2)[..., 0, :]
    w_top, i_top = lax.top_k(jax.nn.softmax(le, axis=-1), TOP_K_IN_GROUP)
    w_top = w_top / w_top.sum(-1, keepdims=True) * p_grp
    eid = grp[..., None] * EXPERTS_PER_GROUP + i_top
    comb = jnp.einsum('bnk,bnke->bne', w_top, jax.nn.one_hot(eid, N_EXPERTS, dtype=F32))
    y = jnp.zeros_like(h)
    for g in range(N_GROUPS):
        sl = slice(g * EXPERTS_PER_GROUP, (g + 1) * EXPERTS_PER_GROUP)
        a = jnp.einsum('bnd,edf->bnef', h, w_e_gate[sl])
        u = jnp.einsum('bnd,edf->bnef', h, w_e_up[sl])
        hid = jax.nn.silu(a) * u * comb[..., sl, None].astype(h.dtype)
        y = y + jnp.einsum('bnef,efd->bnd', hid, w_e_down[sl])
    return y


def setup_inputs(seed: int = 0) -> dict:
    key = jax.random.key(seed)
    ks = iter(jax.random.split(key, 40))
    L, D = DEPTH, D_MODEL

    def nrm(shape, scale):
        return jax.random.normal(next(ks), shape, F32) * scale

    def gain(shape):
        return 1.0 + nrm(shape, 0.05)

    fg = jnp.linspace(FGATE_BIAS_LO, FGATE_BIAS_HI, MLSTM_HEADS, dtype=F32)
    b_gates = jnp.concatenate([nrm((L, MLSTM_HEADS), 0.1), fg + nrm((L, MLSTM_HEADS), 0.1),
                               nrm((L, MLSTM_HEADS), 0.1), fg + nrm((L, MLSTM_HEADS), 0.1)], axis=-1)
    return {
        'x': nrm((BATCH, SEQ, D), 1.0),
        'c': nrm((BATCH, D), 1.0),
        'ctx': nrm((BATCH, CTX_LEN, D), 1.0),
        'c_ctx': nrm((D,), 1.0),
        'w_ada': nrm((L, D, 6 * D), D ** -0.5),
        'b_ada': nrm((L, 6 * D), 0.1),
        'w_in': nrm((L, D, D_IN), D ** -0.5),
        'b_gates': b_gates,
        'w_uq': nrm((L, Q_LORA, MLA_HEADS * (QK_NOPE + QK_ROPE)), Q_LORA ** -0.5),
        'w_uk': nrm((L, KV_LORA, MLA_HEADS * QK_NOPE), KV_LORA ** -0.5),
        'w_uv': nrm((L, KV_LORA, MLA_HEADS * V_HEAD), KV_LORA ** -0.5),
        'g_qn': gain((L, Q_LORA)),
        'g_kvn': gain((L, KV_LORA)),
        'g_mh': gain((L, MLSTM_HEADS * MV)),
        'w_bo_mla': nrm((L, MLA_HEADS * V_HEAD, D), (MLA_HEADS * V_HEAD) ** -0.5 * DEEPNORM_BETA),
        'w_bo_mlstm': nrm((L, MLSTM_HEADS * MV, D), (MLSTM_HEADS * MV) ** -0.5 * DEEPNORM_BETA),
        'w_out': nrm((L, D, D), D ** -0.5 * DEEPNORM_BETA),
        'ln1_g': gain((L, D)),
        'ln1_b': nrm((L, D), 0.02),
        'w_rg': nrm((L, D, N_GROUPS), D ** -0.5),
        'b_rg': nrm((L, N_GROUPS), 0.01),
        'w_re': nrm((L, D, N_EXPERTS), D ** -0.5),
        'b_re': nrm((L, N_EXPERTS), 0.01),
        'w_e_gate': nrm((L, N_EXPERTS, D, D_EXPERT), D ** -0.5),
        'w_e_up': nrm((L, N_EXPERTS, D, D_EXPERT), D ** -0.5),
        'w_e_down': nrm((L, N_EXPERTS, D_EXPERT, D), D_EXPERT ** -0.5 * DEEPNORM_BETA),
        'ln2_g': gain((L, D)),
        'ln2_b': nrm((L, D), 0.02),
    }


def reference(x, c, ctx, c_ctx, w_ada, b_ada, w_in, b_gates, w_uq, w_uk, w_uv, g_qn, g_kvn, g_mh,
              w_bo_mla, w_bo_mlstm, w_out, ln1_g, ln1_b, w_rg, b_rg, w_re, b_re,
              w_e_gate, w_e_up, w_e_down, ln2_g, ln2_b):
    ROWS = x.shape[1] // GRID_W
    cos, sin = rope_tables(ROWS)
    xl, xc = x, ctx
    s_lat, s_ctx = jax.nn.silu(c), jax.nn.silu(c_ctx)
    for l in range(DEPTH):
        last = l == DEPTH - 1
        sh1l, sc1l, g1l, sh2l, sc2l, g2l = [m[:, None, :] for m in jnp.split(s_lat @ w_ada[l] + b_ada[l], 6, axis=-1)]
        sh1c, sc1c, g1c, sh2c, sc2c, g2c = jnp.split(s_ctx @ w_ada[l] + b_ada[l], 6, axis=-1)
        yc, yl = token_mixer(modulate(xc, sh1c, sc1c), modulate(xl, sh1l, sc1l), cos, sin,
                             w_in[l], b_gates[l], w_uq[l], w_uk[l], w_uv[l], g_qn[l], g_kvn[l], g_mh[l],
                             w_bo_mla[l], w_bo_mlstm[l], w_out[l])
        xl = layer_norm(DEEPNORM_ALPHA * xl + g1l * yl, ln1_g[l], ln1_b[l])
        moe_args = (w_rg[l], b_rg[l], w_re[l], b_re[l], w_e_gate[l], w_e_up[l], w_e_down[l])
        if not last:
            xc = layer_norm(DEEPNORM_ALPHA * xc + g1c * yc, ln1_g[l], ln1_b[l])
            xc = layer_norm(DEEPNORM_ALPHA * xc + g2c * hier_moe(modulate(xc, sh2c, sc2c), *moe_args),
                            ln2_g[l], ln2_b[l])
        xl = layer_norm(DEEPNORM_ALPHA * xl + g2l * hier_moe(modulate(xl, sh2l, sc2l), *moe_args),
                        ln2_g[l], ln2_b[l])
    return xl
```

```python
import numpy as np
from contextlib import ExitStack
import concourse.bass as bass
import concourse.mybir as mybir
from concourse.bass_utils import run_bass_kernel_spmd

F32 = mybir.dt.float32
BF16 = mybir.dt.bfloat16
AF = mybir.ActivationFunctionType
ALU = mybir.AluOpType
AX = mybir.AxisListType

EPOCH = 16000
NDS = 12
ENGS = ('pe', 'act', 'dve', 'pool', 'sp')


class Prog:
    def __init__(self, nc, es):
        self.nc = nc
        self.es = es
        self.ops = {e: [] for e in ENGS}
        self.cnt = {e: 0 for e in ENGS}
        self.known = {e: {} for e in ENGS}
        self.lastw = {}
        self.readers = {}
        self.ndma = 0
        self.esems = {e: [] for e in ENGS}
        self.dsems = [es.enter_context(nc.semaphore("dsem%d" % j)) for j in range(NDS)]
        self.nt = 0

    def sb(self, shape, dt, name=None):
        self.nt += 1
        return self.es.enter_context(self.nc.sbuf_tensor(name or ("sb%d" % self.nt), list(shape), dt))

    def ps(self, shape, dt, name=None):
        self.nt += 1
        return self.es.enter_context(self.nc.psum_tensor(name or ("ps%d" % self.nt), list(shape), dt))

    def _esem(self, eng, idx):
        while len(self.esems[eng]) <= idx:
            self.esems[eng].append(self.es.enter_context(
                self.nc.semaphore("s_%s_%d" % (eng, len(self.esems[eng])))))
        return self.esems[eng][idx]

    def _deps(self, eng, r, w, extra=()):
        need = {}

        def add(p, v):
            if p is None:
                return
            if need.get(p, 0) < v:
                need[p] = v
        for k in r:
            lw = self.lastw.get(k)
            if lw:
                add(*lw)
        for k in w:
            lw = self.lastw.get(k)
            if lw:
                add(*lw)
            for p, v in self.readers.get(k, {}).items():
                add(p, v)
        for p, v in extra:
            add(p, v)
        waits = []
        kn = self.known[eng]
        for p, v in need.items():
            if p == eng and eng in ('pe', 'sp'):
                continue
            if kn.get(p, 0) >= v:
                continue
            kn[p] = v
            waits.append((p, v))
        return waits

    def op(self, eng, fn, r=(), w=()):
        waits = self._deps(eng, r, w)
        self.cnt[eng] += 1
        c = self.cnt[eng]
        self.ops[eng].append(('op', fn, waits, c))
        for k in r:
            d = self.readers.setdefault(k, {})
            d[eng] = c
        for k in w:
            self.lastw[k] = (eng, c)
            self.readers[k] = {}
        return c

    def dma(self, q, out, in_, r=(), w=(), **kw):
        j = self.ndma % NDS
        n = self.ndma // NDS + 1
        self.ndma += 1
        prod = ('d', j)
        waits = self._deps(q, r, w, extra=([(prod, n - 1)] if n > 1 else []))
        self.ops[q].append(('dma', (out, in_, kw), waits, (j, n)))
        for k in r:
            d = self.readers.setdefault(k, {})
            d[prod] = n
        for k in w:
            self.lastw[k] = (prod, n)
            self.readers[k] = {}

    def _emit_wait(self, e, p, v):
        if isinstance(p, tuple):
            e.wait_ge(self.dsems[p[1]], 16 * v)
        else:
            idx = (v - 1) // EPOCH
            e.wait_ge(self._esem(p, idx), (v - 1) % EPOCH + 1)

    def _run(self, name, e):
        for kind, payload, waits, c in self.ops[name]:
            for p, v in waits:
                self._emit_wait(e, p, v)
            if kind == 'op':
                ins = payload(e)
                idx = (c - 1) // EPOCH
                ins.then_inc(self._esem(name, idx), 1)
            else:
                out, in_, kw = payload
                j, n = c
                e.dma_start(out=out, in_=in_, **kw).then_inc(self.dsems[j], 16)
        if name == 'sp':
            tot = {}
            for j in range(NDS):
                n = (self.ndma - j + NDS - 1) // NDS if self.ndma > j else 0
                if n > 0:
                    e.wait_ge(self.dsems[j], 16 * n)

    def finish(self):
        nc = self.nc
        for e in ENGS:
            if self.cnt[e] > 0:
                self._esem(e, (self.cnt[e] - 1) // EPOCH)
        with nc.Block() as block:
            @block.sync
            def _(sync):
                self._run('sp', sync)

            @block.tensor
            def _(tensor):
                self._run('pe', tensor)

            @block.scalar
            def _(scalar):
                self._run('act', scalar)

            @block.vector
            def _(vector):
                self._run('dve', vector)

            @block.gpsimd
            def _(gpsimd):
                self._run('pool', gpsimd)


D = 1024
SEQ = 16384
CTX = 256
T = SEQ + CTX
NCORE = 8
LAT_C = SEQ // NCORE
TC = CTX + LAT_C
NTILE = TC // 128
GROUPS = [(0, 256), (256, 512), (768, 512), (1280, 512), (1792, 512)]
DEPTH = 4
EPS = 1e-6
ALPHA = (2 * DEPTH) ** 0.25
MLA_SCALE = 96 ** -0.5
NCH = T // 64


def dram_in(nc, name, shape, dt=F32):
    return nc.dram_tensor(name, list(shape), dt, kind="ExternalInput").ap()


def dram_out(nc, name, shape, dt=F32):
    return nc.dram_tensor(name, list(shape), dt, kind="ExternalOutput").ap()


def build_M():
    nc = bass.Bass("TRN2", target_bir_lowering=False)
    cc = dram_in(nc, "cc", [128, 8, 2])
    wa = dram_in(nc, "wa", [128, 8, 3072])
    ba = dram_in(nc, "ba", [128, 24])
    mo = dram_out(nc, "mo", [128, 24, 2])
    with ExitStack() as es:
        P = Prog(nc, es)
        ccs = P.sb([128, 8, 2], F32)
        was = P.sb([128, 8, 3072], F32)
        bas = P.sb([128, 24], F32)
        mos = P.sb([128, 24, 2], F32)
        pm = P.ps([128, 24, 2], F32)
        P.dma('sp', ccs[:], cc, w=['cc'])
        P.dma('sp', bas[:], ba, w=['ba'])
        for k in range(8):
            P.dma('sp', was[:, k, :], wa[:, k, :], w=['wa%d' % k])
        P.op('act', lambda e: e.activation(ccs[:], ccs[:], AF.Silu), r=['cc'], w=['cc'])
        for j in range(24):
            for k in range(8):
                P.op('pe', lambda e, j=j, k=k: e.matmul(pm[:, j, :], was[:, k, j * 128:(j + 1) * 128], ccs[:, k, :],
                                                       start=(k == 0), stop=(k == 7)),
                     r=['cc', 'wa%d' % k], w=['pm'])
        P.op('dve', lambda e: e.tensor_tensor(mos[:], pm[:], bas[:].unsqueeze(2).to_broadcast([128, 24, 2]), ALU.add),
             r=['pm', 'ba'], w=['mo'])
        P.dma('sp', mo, mos[:], r=['mo'])
        P.finish()
    return nc


A_QD, A_KVD, A_KR, A_KRP, A_MQ, A_MK, A_PG = 0, 384, 640, 672, 704, 960, 1216
A_TM = 1232
A_NCOL = A_TM + 256 + 512 + 512 + 2048


class Common:
    def __init__(self, P, nc):
        self.P = P
        self.nc = nc

    def load(self, q, dst, src, key, maxcols=None):
        self.P.dma(q, dst, src, w=[key])


def ln_rstd(P, var_ap, out_ap, rkeys, wkey, scale=1.0):
    P.op('act', lambda e: e.activation(out_ap, var_ap, AF.Ln, bias=EPS, scale=scale), r=rkeys, w=[wkey])
    P.op('act', lambda e: e.activation(out_ap, out_ap, AF.Exp, scale=-0.5), r=[wkey], w=[wkey])


def build_A():
    nc = bass.Bass("TRN2", target_bir_lowering=False)
    x = dram_in(nc, "x", [TC, D])
    mod1 = dram_in(nc, "mod1", [128, 8, 4])
    ident = dram_in(nc, "ident", [128, 128])
    ones_d = dram_in(nc, "ones", [128, 128])
    w_in = dram_in(nc, "w_in", [128, 8, A_NCOL])
    w_uq = dram_in(nc, "w_uq", [128, 3, 1024])
    w_uk = dram_in(nc, "w_uk", [128, 2, 512])
    w_uv = dram_in(nc, "w_uv", [128, 2, 512])
    gq_d = dram_in(nc, "gq", [128, 3])
    gkv_d = dram_in(nc, "gkv", [128, 2])
    bg_d = dram_in(nc, "bg", [16, 1])
    cos_d = dram_in(nc, "cos4", [128, TC])
    sin_d = dram_in(nc, "ssin4", [128, TC])
    QT = dram_out(nc, "QT", [6, 128, TC])
    KnT = dram_out(nc, "KnT", [4, 128, TC])
    KrT = dram_out(nc, "KrT", [32, TC])
    V = dram_out(nc, "V", [TC, 512])
    mqT = dram_out(nc, "mqT", [2, 128, TC])
    mkT = dram_out(nc, "mkT", [2, 128, TC])
    mkv = dram_out(nc, "mkv", [TC, 768])
    graw = dram_out(nc, "graw", [16, TC])
    glsg = dram_out(nc, "glsg", [16, TC])
    spo = dram_out(nc, "spo", [TC, 512])
    spm = dram_out(nc, "spm", [TC, 2048])
    with ExitStack() as es:
        P = Prog(nc, es)
        idt = P.sb([128, 128], F32)
        ones = P.sb([128, 128], F32)
        mods = P.sb([128, 8, 4], F32)
        win = P.sb([128, 8, A_NCOL], BF16)
        wuq = P.sb([128, 3, 1024], BF16)
        wuk = P.sb([128, 2, 512], BF16)
        wuv = P.sb([128, 2, 512], BF16)
        gq = P.sb([128, 3], F32)
        gkv = P.sb([128, 2], F32)
        bg = P.sb([16, 1], F32)
        cos4 = P.sb([128, TC], F32)
        sin4 = P.sb([128, TC], F32)
        hT = P.sb([128, 8, TC], BF16)
        xs = [P.sb([128, D], F32) for _ in range(2)]
        xn = [P.sb([128, D], F32) for _ in range(2)]
        st = [P.sb([128, 12], F32) for _ in range(2)]
        mv = [P.sb([128, 2], F32) for _ in range(2)]
        rs = [P.sb([128, 1], F32) for _ in range(2)]
        tp = P.ps([128, 8, 128], F32)
        pf = [P.ps([128, 512], F32) for _ in range(2)]
        pt = [P.ps([128, 512], F32) for _ in range(2)]
        pst = P.ps([128, 512], F32)
        dn = P.sb([128, 3, 512], F32)
        sq = P.sb([128, 3, 512], F32)
        rq = P.sb([128, 512], F32)
        dnn = P.sb([128, 3, 512], BF16)
        stg = [P.sb([128, 512], F32) for _ in range(3)]
        t1 = P.sb([128, 512], F32)
        t2 = P.sb([128, 512], F32)
        g1 = P.sb([16, 512], F32)
        g2 = P.sb([16, 512], F32)
        g3 = P.sb([16, 512], F32)

        P.dma('sp', idt[:], ident, w=['idt'])
        P.dma('sp', ones[:], ones_d, w=['ones'])
        P.dma('sp', mods[:], mod1, w=['mods'])
        P.dma('sp', gq[:], gq_d, w=['gq'])
        P.dma('sp', gkv[:], gkv_d, w=['gkv'])
        P.dma('sp', bg[:], bg_d, w=['bg'])
        P.dma('sp', cos4[:], cos_d, w=['cos'])
        P.dma('sp', sin4[:], sin_d, w=['sin'])
        for k in range(8):
            for c0 in range(0, A_NCOL, 1520):
                P.dma('pool', win[:, k, c0:c0 + 1520], w_in[:, k, c0:c0 + 1520], w=['win'])
        for j in range(3):
            P.dma('pool', wuq[:, j, :], w_uq[:, j, :], w=['wuq'])
        for j in range(2):
            P.dma('pool', wuk[:, j, :], w_uk[:, j, :], w=['wuk'])
            P.dma('pool', wuv[:, j, :], w_uv[:, j, :], w=['wuv'])
        P.op('dve', lambda e: e.tensor_scalar(mods[:, :, 0:1], mods[:, :, 0:1], 1.0, None, ALU.add), r=['mods'], w=['mods'])
        P.op('dve', lambda e: e.tensor_scalar(mods[:, :, 2:3], mods[:, :, 2:3], 1.0, None, ALU.add), r=['mods'], w=['mods'])

        for i in range(NTILE):
            b = i % 2
            sel = 2 if i < 2 else 0
            P.dma('sp', xs[b][:], x[i * 128:(i + 1) * 128, :], w=['xs%d' % b])
            for hh in range(2):
                P.op('dve', lambda e, b=b, hh=hh: e.bn_stats(st[b][:, hh * 6:(hh + 1) * 6], xs[b][:, hh * 512:(hh + 1) * 512]),
                     r=['xs%d' % b], w=['st%d' % b])
            P.op('dve', lambda e, b=b: e.bn_aggr(mv[b][:], st[b][:]), r=['st%d' % b], w=['mv%d' % b])
            ln_rstd(P, mv[b][:, 1:2], rs[b][:], ['mv%d' % b], 'rs%d' % b)
            P.op('dve', lambda e, b=b: e.tensor_scalar(xn[b][:], xs[b][:], mv[b][:, 0:1], rs[b][:, 0:1], ALU.subtract, ALU.mult),
                 r=['xs%d' % b, 'mv%d' % b, 'rs%d' % b], w=['xn%d' % b])
            for k in range(8):
                P.op('pe', lambda e, b=b, k=k: e.transpose(tp[:, k, :], xn[b][:, k * 128:(k + 1) * 128], idt[:]),
                     r=['xn%d' % b, 'idt'], w=['tp%d' % (k // 4)])
            for k in range(8):
                P.op('act', lambda e, i=i, k=k, sel=sel: e.activation(
                    hT[:, k, i * 128:(i + 1) * 128], tp[:, k, :], AF.Identity,
                    bias=mods[:, k, sel + 1:sel + 2], scale=mods[:, k, sel:sel + 1]),
                    r=['tp%d' % (k // 4), 'mods'], w=['hT%d' % i])

        fmi = [0]

        def fm_mm(col0, m, s, n):
            bi = fmi[0] % 2
            fmi[0] += 1
            ps = pf[bi]
            tiles = ['hT%d' % t for t in range(s // 128, (s + n) // 128)]
            for k in range(8):
                P.op('pe', lambda e, k=k, ps=ps: e.matmul(ps[0:m, 0:n], win[:, k, col0:col0 + m], hT[:, k, s:s + n],
                                                          start=(k == 0), stop=(k == 7)),
                     r=['win'] + tiles, w=['pf%d' % bi])
            return ps, 'pf%d' % bi

        sgi = [0]

        def stage_out(ps, pkey, m, n, dst, func=AF.Copy, scale=1.0, bias=None):
            si = sgi[0] % 3
            sgi[0] += 1
            sg = stg[si]
            if bias is None:
                P.op('act', lambda e: e.activation(sg[0:m, 0:n], ps[0:m, 0:n], func, scale=scale),
                     r=[pkey], w=['stg%d' % si])
            else:
                P.op('act', lambda e: e.activation(sg[0:m, 0:n], ps[0:m, 0:n], func, bias=bias, scale=scale),
                     r=[pkey, 'bg'], w=['stg%d' % si])
            P.dma('sp', dst, sg[0:m, 0:n], r=['stg%d' % si])

        def rms_block(col0, nch, gvec, gkey, dim, s, n):
            for j in range(nch):
                ps, pk = fm_mm(col0 + j * 128, 128, s, n)
                P.op('act', lambda e, j=j, ps=ps: e.activation(dn[:, j, 0:n], ps[:, 0:n], AF.Copy), r=[pk], w=['dn%d' % j])
                P.op('act', lambda e, j=j, ps=ps: e.activation(sq[:, j, 0:n], ps[:, 0:n], AF.Square), r=[pk], w=['sq%d' % j])
            for j in range(nch):
                P.op('pe', lambda e, j=j: e.matmul(pst[:, 0:n], ones[:], sq[:, j, 0:n], start=(j == 0), stop=(j == nch - 1)),
                     r=['ones', 'sq%d' % j], w=['pst'])
            ln_rstd(P, pst[:, 0:n], rq[:, 0:n], ['pst'], 'rq', scale=1.0 / dim)
            for j in range(nch):
                P.op('dve', lambda e, j=j: e.scalar_tensor_tensor(dnn[:, j, 0:n], dn[:, j, 0:n], gvec[:, j:j + 1], rq[:, 0:n],
                                                                 ALU.mult, ALU.mult),
                     r=['dn%d' % j, gkey, 'rq'], w=['dnn%d' % j])

        def up_mm(wt, wkey, nch, col0, m, n, tok0=None, tm=False):
            bi = fmi[0] % 2
            fmi[0] += 1
            ps = pf[bi]
            for j in range(nch):
                if not tm:
                    P.op('pe', lambda e, j=j, ps=ps: e.matmul(ps[0:m, 0:n], wt[:, j, col0:col0 + m], dnn[:, j, 0:n],
                                                              start=(j == 0), stop=(j == nch - 1)),
                         r=[wkey, 'dnn%d' % j], w=['pf%d' % bi])
                else:
                    P.op('pe', lambda e, j=j, ps=ps: e.matmul(ps[:, 0:m], dnn[:, j, tok0:tok0 + 128], wt[:, j, col0:col0 + m],
                                                              start=(j == 0), stop=(j == nch - 1)),
                         r=[wkey, 'dnn%d' % j], w=['pf%d' % bi])
            return ps, 'pf%d' % bi

        def rope_out(psA, kA, psP, kP, m, s, n, dst):
            P.op('dve', lambda e: e.tensor_tensor(t1[0:m, 0:n], psA[0:m, 0:n], cos4[0:m, s:s + n], ALU.mult),
                 r=[kA, 'cos'], w=['t1'])
            P.op('dve', lambda e: e.tensor_tensor(t2[0:m, 0:n], psP[0:m, 0:n], sin4[0:m, s:s + n], ALU.mult),
                 r=[kP, 'sin'], w=['t2'])
            P.op('dve', lambda e: e.tensor_tensor(t1[0:m, 0:n], t1[0:m, 0:n], t2[0:m, 0:n], ALU.add),
                 r=['t1', 't2'], w=['t1'])
            P.dma('sp', dst, t1[0:m, 0:n], r=['t1'])

        for (s, n) in GROUPS:
            rms_block(A_QD, 3, gq, 'gq', 384, s, n)
            for oc in range(4):
                ps, pk = up_mm(wuq, 'wuq', 3, oc * 128, 128, n)
                stage_out(ps, pk, 128, n, QT[oc, :, s:s + n])
            for c in range(2):
                psA, kA = up_mm(wuq, 'wuq', 3, 512 + c * 128, 128, n)
                psP, kP = up_mm(wuq, 'wuq', 3, 768 + c * 128, 128, n)
                rope_out(psA, kA, psP, kP, 128, s, n, QT[4 + c, :, s:s + n])
            rms_block(A_KVD, 2, gkv, 'gkv', 256, s, n)
            for oc in range(4):
                ps, pk = up_mm(wuk, 'wuk', 2, oc * 128, 128, n)
                stage_out(ps, pk, 128, n, KnT[oc, :, s:s + n])
            for tt in range(n // 128):
                ps, pk = up_mm(wuv, 'wuv', 2, 0, 512, n, tok0=tt * 128, tm=True)
                stage_out(ps, pk, 128, 512, V[s + tt * 128:s + (tt + 1) * 128, :])
            psA, kA = fm_mm(A_KR, 32, s, n)
            psP, kP = fm_mm(A_KRP, 32, s, n)
            rope_out(psA, kA, psP, kP, 32, s, n, KrT[:, s:s + n])
            for c in range(2):
                ps, pk = fm_mm(A_MQ + c * 128, 128, s, n)
                stage_out(ps, pk, 128, n, mqT[c, :, s:s + n], scale=0.125)
            for c in range(2):
                ps, pk = fm_mm(A_MK + c * 128, 128, s, n)
                stage_out(ps, pk, 128, n, mkT[c, :, s:s + n])
            ps, pk = fm_mm(A_PG, 16, s, n)
            P.op('act', lambda e, ps=ps: e.activation(g1[:, 0:n], ps[0:16, 0:n], AF.Identity, bias=bg[:, 0:1], scale=1.0),
                 r=[pk, 'bg'], w=['g1'])
            P.dma('sp', graw[:, s:s + n], g1[:, 0:n], r=['g1'])
            P.op('act', lambda e: e.activation(g2[:, 0:n], g1[:, 0:n], AF.Exp, scale=-1.0), r=['g1'], w=['g2'])
            P.op('act', lambda e: e.activation(g3[:, 0:n], g2[:, 0:n], AF.Ln, bias=1.0, scale=1.0), r=['g2'], w=['g3'])
            P.op('act', lambda e: e.activation(g3[:, 0:n], g3[:, 0:n], AF.Copy, scale=-1.0), r=['g3'], w=['g3'])
            P.dma('sp', glsg[:, s:s + n], g3[:, 0:n], r=['g3'])

        tmi = [0]
        tm_groups = [(A_TM, 256, mkv, 0, AF.Copy), (A_TM + 256, 512, mkv, 256, AF.Copy),
                     (A_TM + 768, 512, spo, 0, AF.Sigmoid)] + \
                    [(A_TM + 1280 + q * 512, 512, spm, q * 512, AF.Sigmoid) for q in range(4)]
        for i in range(NTILE):
            for (c0, ncl, dst, dc0, func) in tm_groups:
                bi = tmi[0] % 2
                tmi[0] += 1
                ps = pt[bi]
                for k in range(8):
                    P.op('pe', lambda e, k=k, ps=ps, c0=c0, ncl=ncl, i=i: e.matmul(
                        ps[:, 0:ncl], hT[:, k, i * 128:(i + 1) * 128], win[:, k, c0:c0 + ncl],
                        start=(k == 0), stop=(k == 7)), r=['win', 'hT%d' % i], w=['pt%d' % bi])
                stage_out(ps, 'pt%d' % bi, 128, ncl, dst[i * 128:(i + 1) * 128, dc0:dc0 + ncl], func=func)
        P.finish()
    return nc


ROPE_PERM = np.concatenate([np.arange(8, 16), np.arange(0, 8), np.arange(24, 32), np.arange(16, 24)])
ROPE_SIGN = np.concatenate([-np.ones(8), np.ones(8), -np.ones(8), np.ones(8)]).astype(np.float32)


def rope_tables_np():
    rows = SEQ // 64
    row, col = np.meshgrid(np.arange(rows, dtype=np.float32), np.arange(64, dtype=np.float32), indexing='ij')
    row, col = row.reshape(-1), col.reshape(-1)
    half = 16
    inv = (np.float32(10000.0) ** (-np.arange(0, half, 2, dtype=np.float32) / np.float32(half))).astype(np.float32)
    ar, ac = row[:, None] * inv, col[:, None] * inv
    ang = np.concatenate([ar, ar, ac, ac], axis=-1)
    ang = np.concatenate([np.zeros((CTX, 32), np.float32), ang], axis=0).astype(np.float32)
    return np.cos(ang).astype(np.float32), (np.sin(ang) * ROPE_SIGN).astype(np.float32)


def core_tokens(i):
    return np.concatenate([np.arange(CTX), CTX + np.arange(i * LAT_C, (i + 1) * LAT_C)])


def kp(a, nk):
    return np.ascontiguousarray(a.reshape(nk, 128, -1).transpose(1, 0, 2))


def prep_A_weights(w_in, w_uq, w_uk, w_uv, g_qn, g_kvn, b_gates):
    kr = 640 + ROPE_PERM
    colsA = np.concatenate([np.arange(0, 384), np.arange(384, 640), np.arange(640, 672), kr,
                            np.arange(672, 928), np.arange(928, 1184), np.arange(2208, 2224),
                            np.arange(928, 1184), np.arange(1184, 1696), np.arange(1696, 2208),
                            np.arange(2224, 4272)])
    assert len(colsA) == A_NCOL
    hq = np.arange(8)[:, None] * 96
    nope = (hq + np.arange(64)[None, :]).reshape(-1)
    rope = (hq + 64 + np.arange(32)[None, :]).reshape(-1)
    ropep = (hq + 64 + ROPE_PERM[None, :]).reshape(-1)
    colsq = np.concatenate([nope, rope, ropep])
    return {
        "w_in": kp(w_in[:, colsA], 8),
        "w_uq": kp(w_uq[:, colsq], 3),
        "w_uk": kp(w_uk, 2),
        "w_uv": kp(w_uv, 2),
        "gq": np.ascontiguousarray(g_qn.reshape(3, 128).T),
        "gkv": np.ascontiguousarray(g_kvn.reshape(2, 128).T),
        "bg": np.ascontiguousarray(b_gates.reshape(16, 1)),
    }


NKT = T // 128
QGROUPS = [(0, 256, 2)] + [(CTX + g * 512, 512, NKT) for g in range(SEQ // 512)]


def build_B1():
    nc = bass.Bass("TRN2", target_bir_lowering=False)
    Qd = dram_in(nc, "Q", [96, T])
    Kd = dram_in(nc, "K", [96, T])
    Vd = dram_in(nc, "V", [128, NKT, 65])
    OT = dram_out(nc, "OT", [65, T])
    with ExitStack() as es:
        P = Prog(nc, es)
        Qs = P.sb([96, T], BF16)
        Ks = P.sb([96, T], BF16)
        Vs = P.sb([128, NKT, 65], BF16)
        pTs = [P.sb([128, 512], BF16) for _ in range(3)]
        ost = [P.sb([65, 512], F32) for _ in range(2)]
        pss = [P.ps([128, 512], F32) for _ in range(3)]
        pso = [P.ps([128, 512], F32) for _ in range(2)]
        CW = 1280
        for c0 in range(0, T, CW):
            P.dma('pool', Ks[:, c0:c0 + CW], Kd[:, c0:c0 + CW], w=['K%d' % (c0 // CW)])
            P.dma('pool', Qs[:, c0:c0 + CW], Qd[:, c0:c0 + CW], w=['Q%d' % (c0 // CW)])
        for t0 in range(0, NKT, 26):
            P.dma('pool', Vs[:, t0:t0 + 26, :], Vd[:, t0:t0 + 26, :], w=['V%d' % (t0 // 26)])
        step = 0
        for gi, (q0, nq, nkt) in enumerate(QGROUPS):
            ob = gi % 2
            qkeys = ['Q%d' % j for j in range(q0 // CW, (q0 + nq - 1) // CW + 1)]
            for kt in range(nkt):
                sb_ = step % 3
                step += 1
                kkey = 'K%d' % ((kt * 128) // CW)
                P.op('pe', lambda e, sb_=sb_, kt=kt, q0=q0, nq=nq: e.matmul(
                    pss[sb_][:, 0:nq], Ks[:, kt * 128:(kt + 1) * 128], Qs[:, q0:q0 + nq], start=True, stop=True),
                    r=[kkey] + qkeys, w=['pss%d' % sb_])
                P.op('act', lambda e, sb_=sb_, nq=nq: e.activation(pTs[sb_][:, 0:nq], pss[sb_][:, 0:nq], AF.Exp, scale=MLA_SCALE),
                     r=['pss%d' % sb_], w=['pT%d' % sb_])
                P.op('pe', lambda e, sb_=sb_, kt=kt, nq=nq, ob=ob, nkt=nkt: e.matmul(
                    pso[ob][0:65, 0:nq], Vs[:, kt, :], pTs[sb_][:, 0:nq], start=(kt == 0), stop=(kt == nkt - 1)),
                    r=['V%d' % (kt // 26), 'pT%d' % sb_], w=['pso%d' % ob])
            P.op('dve', lambda e, ob=ob, nq=nq: e.tensor_copy(ost[ob][:, 0:nq], pso[ob][0:65, 0:nq]),
                 r=['pso%d' % ob], w=['ost%d' % ob])
            P.dma('sp', OT[:, q0:q0 + nq], ost[ob][:, 0:nq], r=['ost%d' % ob])
        P.finish()
    return nc


CB = 20
NBLK = NCH // CB
RING = 4


def build_B2():
    nc = bass.Bass("TRN2", target_bir_lowering=False)
    qTd = dram_in(nc, "qT", [64, T])
    kTd = dram_in(nc, "kT", [64, T])
    ktd = dram_in(nc, "kt", [64, NCH, 64])
    vxd = dram_in(nc, "vx", [64, NCH, 129])
    igd = dram_in(nc, "ig", [64, NCH])
    lfd = dram_in(nc, "lf", [64, NCH])
    trid = dram_in(nc, "tri", [64, 64])
    oned = dram_in(nc, "ones", [64, 64])
    H = dram_out(nc, "H", [64, NCH, 128])
    with ExitStack() as es:
        P = Prog(nc, es)
        qT = P.sb([64, T], BF16)
        kT = P.sb([64, T], BF16)
        kt = P.sb([64, NCH, 64], BF16)
        ig = P.sb([64, NCH], F32)
        lf = P.sb([64, NCH], F32)
        tri = P.sb([64, 64], F32)
        ones = P.sb([64, 64], F32)
        bb = P.sb([64, NCH], F32)
        ebl = P.sb([64, NCH], F32)
        ek = P.sb([64, NCH], F32)
        ek2 = P.sb([64, NCH], F32)
        eb = P.sb([64, NCH], F32)
        tmp = P.sb([64, NCH], F32)
        vx = [P.sb([64, CB, 129], F32) for _ in range(2)]
        v1 = [P.sb([64, CB, 129], BF16) for _ in range(2)]
        v2 = [P.sb([64, CB, 129], BF16) for _ in range(2)]
        hr = [P.sb([64, CB, 129], F32) for _ in range(2)]
        ho = [P.sb([64, CB, 128], F32) for _ in range(2)]
        tn = [P.sb([64, CB], F32) for _ in range(2)]
        MT = [P.sb([64, 64], BF16) for _ in range(3)]
        Cst = [P.sb([64, 129], F32) for _ in range(2)]
        Cbf = [P.sb([64, 129], BF16) for _ in range(RING)]
        pb = P.ps([64, 512], F32)
        pbl = P.ps([64, 512], F32)
        ps_s = [P.ps([64, 512], F32) for _ in range(2)]
        ps_h = [P.ps([64, 512], F32) for _ in range(2)]
        ps_u = [P.ps([64, 512], F32) for _ in range(2)]
        CW = 1280
        for c0 in range(0, T, CW):
            P.dma('pool', qT[:, c0:c0 + CW], qTd[:, c0:c0 + CW], w=['qT%d' % (c0 // CW)])
            P.dma('pool', kT[:, c0:c0 + CW], kTd[:, c0:c0 + CW], w=['kT%d' % (c0 // CW)])
        for b in range(NBLK):
            P.dma('pool', kt[:, b * CB:(b + 1) * CB, :], ktd[:, b * CB:(b + 1) * CB, :], w=['kt%d' % b])
        P.dma('sp', ig[:], igd, w=['ig'])
        P.dma('sp', lf[:], lfd, w=['lf'])
        P.dma('sp', tri[:], trid, w=['tri'])
        P.dma('sp', ones[:], oned, w=['ones'])
        P.op('pe', lambda e: e.matmul(pb[:, 0:NCH], tri[:], lf[:], start=True, stop=True), r=['tri', 'lf'], w=['pb'])
        P.op('pe', lambda e: e.matmul(pbl[:, 0:NCH], ones[:], lf[:], start=True, stop=True), r=['ones', 'lf'], w=['pbl'])
        P.op('dve', lambda e: e.tensor_copy(bb[:], pb[:, 0:NCH]), r=['pb'], w=['bb'])
        P.op('act', lambda e: e.activation(ebl[:], pbl[:, 0:NCH], AF.Exp), r=['pbl'], w=['ebl'])
        P.op('act', lambda e: e.activation(eb[:], bb[:], AF.Exp), r=['bb'], w=['eb'])
        P.op('dve', lambda e: e.tensor_tensor(tmp[:], ig[:], bb[:], ALU.subtract), r=['ig', 'bb'], w=['tmp'])
        P.op('act', lambda e: e.activation(ek[:], tmp[:], AF.Exp), r=['tmp'], w=['ek'])
        P.op('dve', lambda e: e.tensor_tensor(tmp[:], tmp[:], pbl[:, 0:NCH], ALU.add), r=['tmp', 'pbl', 'ek'], w=['tmp2'])
        P.op('act', lambda e: e.activation(ek2[:], tmp[:], AF.Exp), r=['tmp2'], w=['ek2'])
        P.op('dve', lambda e: e.memset(Cst[0][:], 0.0), w=['Cst0'])
        P.op('dve', lambda e: e.memset(Cbf[0][:], 0.0), w=['Cbf0'])

        def load_block(b):
            s = b % 2
            P.dma('sp', vx[s][:], vxd[:, b * CB:(b + 1) * CB, :], w=['vx%d' % s])
            P.op('dve', lambda e: e.tensor_tensor(v1[s][:], vx[s][:],
                                                  ek[:, b * CB:(b + 1) * CB].unsqueeze(2).to_broadcast([64, CB, 129]), ALU.mult),
                 r=['vx%d' % s, 'ek'], w=['v1_%d' % s])
            P.op('dve', lambda e: e.tensor_tensor(v2[s][:], vx[s][:],
                                                  ek2[:, b * CB:(b + 1) * CB].unsqueeze(2).to_broadcast([64, CB, 129]), ALU.mult),
                 r=['vx%d' % s, 'ek2'], w=['v2_%d' % s])

        def front(c):
            b, ci = divmod(c, CB)
            s = b % 2
            tk = ['qT%d' % ((c * 64) // CW), 'kT%d' % ((c * 64) // CW)]
            P.op('pe', lambda e: e.matmul(ps_s[c % 2][:, 0:64], kT[:, c * 64:(c + 1) * 64], qT[:, c * 64:(c + 1) * 64],
                                          start=True, stop=True), r=tk, w=['ps_s%d' % (c % 2)])
            P.op('dve', lambda e: e.tensor_tensor(MT[c % 3][:], ps_s[c % 2][:, 0:64], tri[:], ALU.mult),
                 r=['ps_s%d' % (c % 2), 'tri'], w=['MT%d' % (c % 3)])
            P.op('pe', lambda e: e.matmul(ps_u[c % 2][:, 0:129], kt[:, c, :], v2[s][:, ci, :], start=True, stop=True),
                 r=['kt%d' % b, 'v2_%d' % s], w=['ps_u%d' % (c % 2)])

        load_block(0)
        front(0)
        for c in range(NCH):
            b, ci = divmod(c, CB)
            s = b % 2
            if ci == 0 and b + 1 < NBLK:
                load_block(b + 1)
            if c + 1 < NCH:
                front(c + 1)
            P.op('pe', lambda e, c=c, s=s, ci=ci: e.matmul(ps_h[c % 2][:, 0:129], MT[c % 3][:], v1[s][:, ci, :], start=True, stop=False),
                 r=['MT%d' % (c % 3), 'v1_%d' % s], w=['ps_h%d' % (c % 2)])
            P.op('pe', lambda e, c=c: e.matmul(ps_h[c % 2][:, 0:129], qT[:, c * 64:(c + 1) * 64], Cbf[c % RING][:], start=False, stop=True),
                 r=['qT%d' % ((c * 64) // CW), 'Cbf%d' % (c % RING)], w=['ps_h%d' % (c % 2)])
            P.op('act', lambda e, c=c, s=s, ci=ci: e.activation(hr[s][:, ci, :], ps_h[c % 2][:, 0:129], AF.Copy),
                 r=['ps_h%d' % (c % 2)], w=['hr%d' % s])
            if c + 1 < NCH:
                P.op('dve', lambda e, c=c: e.scalar_tensor_tensor(Cst[(c + 1) % 2][:], Cst[c % 2][:], ebl[:, c:c + 1],
                                                                 ps_u[c % 2][:, 0:129], ALU.mult, ALU.add),
                     r=['Cst%d' % (c % 2), 'ebl', 'ps_u%d' % (c % 2)], w=['Cst%d' % ((c + 1) % 2)])
                P.op('act', lambda e, c=c: e.activation(Cbf[(c + 1) % RING][:], Cst[(c + 1) % 2][:], AF.Copy),
                     r=['Cst%d' % ((c + 1) % 2)], w=['Cbf%d' % ((c + 1) % RING)])
            if ci == CB - 1:
                sl = slice(b * CB, (b + 1) * CB)
                P.op('dve', lambda e, s=s, sl=sl: e.tensor_tensor(tn[s][:], hr[s][:, :, 128], eb[:, sl], ALU.mult),
                     r=['hr%d' % s, 'eb'], w=['tn%d' % s])
                P.op('act', lambda e, s=s: e.activation(tn[s][:], tn[s][:], AF.Abs), r=['tn%d' % s], w=['tn%d' % s])
                P.op('dve', lambda e, s=s: e.tensor_scalar(tn[s][:], tn[s][:], 1.0, None, ALU.max), r=['tn%d' % s], w=['tn%d' % s])
                P.op('dve', lambda e, s=s: e.reciprocal(tn[s][:], tn[s][:]), r=['tn%d' % s], w=['tn%d' % s])
                P.op('dve', lambda e, s=s, sl=sl: e.tensor_tensor(tn[s][:], tn[s][:], eb[:, sl], ALU.mult),
                     r=['tn%d' % s, 'eb'], w=['tn%d' % s])
                P.op('dve', lambda e, s=s: e.tensor_tensor(ho[s][:], hr[s][:, :, 0:128],
                                                           tn[s][:].unsqueeze(2).to_broadcast([64, CB, 128]), ALU.mult),
                     r=['hr%d' % s, 'tn%d' % s], w=['ho%d' % s])
                P.dma('sp', H[:, sl, :], ho[s][:], r=['ho%d' % s])
        P.finish()
    return nc


def ln_tile(P, z, zkey, st, mv, rs, sfx, gB=None, bB=None, gkeys=()):
    for hh in range(2):
        P.op('dve', lambda e, hh=hh: e.bn_stats(st[:, hh * 6:(hh + 1) * 6], z[:, hh * 512:(hh + 1) * 512]),
             r=[zkey], w=['st' + sfx])
    P.op('dve', lambda e: e.bn_aggr(mv[:], st[:]), r=['st' + sfx], w=['mv' + sfx])
    ln_rstd(P, mv[:, 1:2], rs[:], ['mv' + sfx], 'rs' + sfx)
    P.op('dve', lambda e: e.tensor_scalar(z[:], z[:], mv[:, 0:1], rs[:, 0:1], ALU.subtract, ALU.mult),
         r=[zkey, 'mv' + sfx, 'rs' + sfx], w=[zkey])
    if gB is not None:
        P.op('dve', lambda e: e.tensor_tensor(z[:], z[:], gB[:], ALU.mult), r=[zkey] + list(gkeys), w=[zkey])
        P.op('dve', lambda e: e.tensor_tensor(z[:], z[:], bB[:], ALU.add), r=[zkey] + list(gkeys), w=[zkey])


def transpose_to(P, src, skey, nch, tp, idt, dst_fn, dkeys, evac):
    for k in range(nch):
        P.op('pe', lambda e, k=k: e.transpose(tp[:, k, :], src[:, k * 128:(k + 1) * 128], idt[:]),
             r=[skey, 'idt'], w=['tp%d' % (k // 4)])
    for k in range(nch):
        evac(k, tp[:, k, :], 'tp%d' % (k // 4))


def build_C1():
    nc = bass.Bass("TRN2", target_bir_lowering=False)
    xd = dram_in(nc, "x", [TC, D])
    od = dram_in(nc, "o", [TC, 8, 65])
    hfd = dram_in(nc, "hf", [TC, 512])
    hbd = dram_in(nc, "hb", [TC, 512])
    spod = dram_in(nc, "spo", [TC, 512])
    spmd = dram_in(nc, "spm", [TC, 2048])
    ident = dram_in(nc, "ident", [128, 128])
    wmla_d = dram_in(nc, "wmla", [128, 4, D])
    wmls_d = dram_in(nc, "wmls", [128, 4, D])
    wout_d = dram_in(nc, "wout", [128, 8, D])
    gmh_d = dram_in(nc, "gmhB", [128, 512])
    g1_d = dram_in(nc, "g1B", [128, 2, D])
    lng_d = dram_in(nc, "lngB", [128, D])
    lnb_d = dram_in(nc, "lnbB", [128, D])
    xo = dram_out(nc, "xo", [TC, D])
    with ExitStack() as es:
        P = Prog(nc, es)
        idt = P.sb([128, 128], F32)
        wmla = P.sb([128, 4, D], BF16)
        wmls = P.sb([128, 4, D], BF16)
        wout = P.sb([128, 8, D], BF16)
        gmh = P.sb([128, 512], F32)
        g1B = P.sb([128, 2, D], F32)
        lng = P.sb([128, D], F32)
        lnb = P.sb([128, D], F32)
        xs = [P.sb([128, D], F32) for _ in range(2)]
        os_ = [P.sb([128, 8, 65], F32) for _ in range(2)]
        hf = [P.sb([128, 512], F32) for _ in range(2)]
        hb = [P.sb([128, 512], F32) for _ in range(2)]
        po = [P.sb([128, 512], F32) for _ in range(2)]
        pm = [P.sb([128, 2048], F32) for _ in range(2)]
        rec = P.sb([128, 8], F32)
        on = P.sb([128, 512], F32)
        onT = P.sb([128, 4, 128], BF16)
        hnT = P.sb([128, 4, 128], BF16)
        ymT = P.sb([128, 8, 128], BF16)
        s4 = P.sb([128, 4], F32)
        v4 = P.sb([128, 4], F32)
        cen = P.sb([128, 512], F32)
        sqv = P.sb([128, 512], F32)
        ta = P.sb([128, D], F32)
        tb = P.sb([128, D], F32)
        st = P.sb([128, 12], F32)
        mv = P.sb([128, 2], F32)
        rs = P.sb([128, 1], F32)
        tp = P.ps([128, 8, 128], F32)
        pA = P.ps([128, 2, 512], F32)
        pB = P.ps([128, 2, 512], F32)
        pY = P.ps([128, 2, 512], F32)
        P.dma('sp', idt[:], ident, w=['idt'])
        P.dma('sp', gmh[:], gmh_d, w=['gmh'])
        P.dma('sp', g1B[:], g1_d, w=['g1B'])
        P.dma('sp', lng[:], lng_d, w=['lng'])
        P.dma('sp', lnb[:], lnb_d, w=['lng'])
        for k in range(4):
            P.dma('pool', wmla[:, k, :], wmla_d[:, k, :], w=['wmla'])
            P.dma('pool', wmls[:, k, :], wmls_d[:, k, :], w=['wmls'])
        for k in range(8):
            P.dma('pool', wout[:, k, :], wout_d[:, k, :], w=['wout'])

        def evac_to(dstT, dkey):
            def f(k, src, skey):
                P.op('act', lambda e: e.activation(dstT[:, k, :], src, AF.Copy), r=[skey], w=[dkey])
            return f

        def proj(ps, pskey, lT, lkey, w, wkey, nk):
            for half in range(2):
                for k in range(nk):
                    P.op('pe', lambda e, half=half, k=k: e.matmul(ps[:, half, :], lT[:, k, :], w[:, k, half * 512:(half + 1) * 512],
                                                                 start=(k == 0), stop=(k == nk - 1)),
                         r=[lkey, wkey], w=[pskey + str(half)])

        for i in range(NTILE):
            b = i % 2
            sel = 1 if i < 2 else 0
            rows = slice(i * 128, (i + 1) * 128)
            P.dma('sp', os_[b][:], od[rows], w=['o%d' % b])
            P.dma('sp', hf[b][:], hfd[rows], w=['hf%d' % b])
            P.dma('sp', hb[b][:], hbd[rows], w=['hb%d' % b])
            P.dma('sp', po[b][:], spod[rows], w=['po%d' % b])
            P.dma('sp', pm[b][:], spmd[rows], w=['pm%d' % b])
            P.dma('sp', xs[b][:], xd[rows], w=['xs%d' % b])
            P.op('dve', lambda e, b=b: e.reciprocal(rec[:], os_[b][:, :, 64]), r=['o%d' % b], w=['rec'])
            P.op('dve', lambda e, b=b: e.tensor_tensor(on[:].rearrange("p (h d) -> p h d", h=8), os_[b][:, :, 0:64],
                                                       rec[:].unsqueeze(2).to_broadcast([128, 8, 64]), ALU.mult),
                 r=['o%d' % b, 'rec'], w=['on'])
            transpose_to(P, on, 'on', 4, tp, idt, None, None, evac_to(onT, 'onT'))
            proj(pA, 'pA', onT, 'onT', wmla, 'wmla', 4)
            P.op('dve', lambda e, b=b: e.tensor_tensor(cen[:], hf[b][:], hb[b][:], ALU.add), r=['hf%d' % b, 'hb%d' % b], w=['cen'])
            c3 = cen[:].rearrange("p (h d) -> p h d", h=4)
            P.op('dve', lambda e: e.tensor_reduce(s4[:], c3, AX.X, ALU.add), r=['cen'], w=['s4'])
            P.op('dve', lambda e: e.tensor_scalar(s4[:], s4[:], -1.0 / 128, None, ALU.mult), r=['s4'], w=['s4'])
            P.op('dve', lambda e: e.tensor_tensor(c3, c3, s4[:].unsqueeze(2).to_broadcast([128, 4, 128]), ALU.add),
                 r=['cen', 's4'], w=['cen'])
            P.op('dve', lambda e: e.tensor_tensor(sqv[:], cen[:], cen[:], ALU.mult), r=['cen'], w=['sqv'])
            P.op('dve', lambda e: e.tensor_reduce(v4[:], sqv[:].rearrange("p (h d) -> p h d", h=4), AX.X, ALU.add),
                 r=['sqv'], w=['v4'])
            ln_rstd(P, v4[:], v4[:], ['v4'], 'v4', scale=1.0 / 128)
            P.op('dve', lambda e: e.tensor_tensor(c3, c3, v4[:].unsqueeze(2).to_broadcast([128, 4, 128]), ALU.mult),
                 r=['cen', 'v4'], w=['cen'])
            P.op('dve', lambda e: e.tensor_tensor(cen[:], cen[:], gmh[:], ALU.mult), r=['cen', 'gmh'], w=['cen'])
            P.op('dve', lambda e, b=b: e.tensor_tensor(cen[:], cen[:], po[b][:], ALU.mult), r=['cen', 'po%d' % b], w=['cen'])
            transpose_to(P, cen, 'cen', 4, tp, idt, None, None, evac_to(hnT, 'hnT'))
            proj(pB, 'pB', hnT, 'hnT', wmls, 'wmls', 4)
            for half in range(2):
                hs = slice(half * 512, (half + 1) * 512)
                P.op('dve', lambda e, b=b, half=half, hs=hs: e.tensor_tensor(ta[:, hs], pA[:, half, :], pm[b][:, hs], ALU.mult),
                     r=['pA%d' % half, 'pm%d' % b], w=['ta'])
                P.op('dve', lambda e, b=b, half=half, hs=hs: e.tensor_tensor(
                    tb[:, hs], pB[:, half, :], pm[b][:, 1024 + half * 512:1024 + (half + 1) * 512], ALU.mult),
                    r=['pB%d' % half, 'pm%d' % b], w=['tb'])
            P.op('dve', lambda e: e.tensor_tensor(ta[:], ta[:], tb[:], ALU.add), r=['ta', 'tb'], w=['ta'])
            transpose_to(P, ta, 'ta', 8, tp, idt, None, None, evac_to(ymT, 'ymT'))
            proj(pY, 'pY', ymT, 'ymT', wout, 'wout', 8)
            for half in range(2):
                hs = slice(half * 512, (half + 1) * 512)
                P.op('dve', lambda e, half=half, hs=hs, sel=sel: e.tensor_tensor(tb[:, hs], pY[:, half, :], g1B[:, sel, hs], ALU.mult),
                     r=['pY%d' % half, 'g1B'], w=['tb'])
            P.op('dve', lambda e, b=b: e.scalar_tensor_tensor(tb[:], xs[b][:], ALPHA, tb[:], ALU.mult, ALU.add),
                 r=['xs%d' % b, 'tb'], w=['tb'])
            ln_tile(P, tb, 'tb', st, mv, rs, '', lng, lnb, ['lng'])
            P.dma('sp', xo[rows], tb[:], r=['tb'])
        P.finish()
    return nc


def bcast(v, p=128):
    return np.ascontiguousarray(np.broadcast_to(np.asarray(v, np.float32).reshape(1, -1), (p, np.asarray(v).size)))


def prep_C1_weights(w_bo_mla, w_bo_mlstm, w_out, g_mh, ln1_g, ln1_b, g1_lat, g1_ctx):
    return {"wmla": kp(w_bo_mla, 4), "wmls": kp(w_bo_mlstm, 4), "wout": kp(w_out, 8),
            "gmhB": bcast(g_mh), "lngB": bcast(ln1_g), "lnbB": bcast(ln1_b),
            "g1B": np.ascontiguousarray(np.stack([bcast(g1_lat), bcast(g1_ctx)], axis=1)),
            "ident": np.eye(128, dtype=np.float32)}


NEXP = 32
DEXP = 256


def build_C2():
    nc = bass.Bass("TRN2", target_bir_lowering=False)
    xd = dram_in(nc, "x", [TC, D])
    ident = dram_in(nc, "ident", [128, 128])
    mod2 = dram_in(nc, "mod2", [128, 8, 4])
    wr_d = dram_in(nc, "wr", [128, 8, 36])
    br_d = dram_in(nc, "brB", [128, 36])
    sel_d = dram_in(nc, "sel", [32, NEXP, 128])
    g2_d = dram_in(nc, "g2B", [128, 2, D])
    lng_d = dram_in(nc, "lngB", [128, D])
    lnb_d = dram_in(nc, "lnbB", [128, D])
    wg_d = dram_in(nc, "wg", [NEXP, 128, 8, DEXP])
    wu_d = dram_in(nc, "wu", [NEXP, 128, 8, DEXP])
    wd_d = dram_in(nc, "wd", [NEXP, 128, 2, D])
    xo = dram_out(nc, "xo", [TC, D])
    with ExitStack() as es:
        P = Prog(nc, es)
        idt = P.sb([128, 128], F32)
        mods = P.sb([128, 8, 4], F32)
        wr = P.sb([128, 8, 36], F32)
        brB = P.sb([128, 36], F32)
        sel = P.sb([32, NEXP, 128], F32)
        g2B = P.sb([128, 2, D], F32)
        lng = P.sb([128, D], F32)
        lnb = P.sb([128, D], F32)
        h2T = P.sb([128, 8, TC], BF16)
        hTf = P.sb([128, 8, 128], F32)
        yacc = P.sb([128, NTILE, D], F32)
        combT = P.sb([32, TC], F32)
        wg = [P.sb([128, 8, DEXP], BF16) for _ in range(2)]
        wu = [P.sb([128, 8, DEXP], BF16) for _ in range(2)]
        wd = [P.sb([128, 2, D], BF16) for _ in range(2)]
        xs = [P.sb([128, D], F32) for _ in range(2)]
        st = P.sb([128, 12], F32)
        mv = P.sb([128, 2], F32)
        rs = P.sb([128, 1], F32)
        lg = P.sb([128, 36], F32)
        sm = [P.sb([128, 8], F32, name="sm%d" % j) for j in range(12)]
        selg = P.sb([128, 4, 8], F32)
        comb = P.sb([128, 4, 8], F32)
        sa = [P.sb([128, 512], F32) for _ in range(2)]
        hid = [P.sb([128, 2, 512], BF16) for _ in range(2)]
        tp = P.ps([128, 8, 128], F32)
        pA = [P.ps([128, 512], F32) for _ in range(2)]
        pU = [P.ps([128, 512], F32) for _ in range(1)]
        pC = P.ps([128, 512], F32)
        pY = [P.ps([128, 512], F32) for _ in range(2)]
        P.dma('sp', idt[:], ident, w=['idt'])
        P.dma('sp', mods[:], mod2, w=['mods'])
        P.dma('sp', wr[:], wr_d, w=['wr'])
        P.dma('sp', brB[:], br_d, w=['brB'])
        P.dma('sp', sel[:], sel_d, w=['sel'])
        P.dma('sp', g2B[:], g2_d, w=['g2B'])
        P.dma('sp', lng[:], lng_d, w=['lng'])
        P.dma('sp', lnb[:], lnb_d, w=['lng'])
        P.op('dve', lambda e: e.tensor_scalar(mods[:, :, 0:1], mods[:, :, 0:1], 1.0, None, ALU.add), r=['mods'], w=['mods'])
        P.op('dve', lambda e: e.tensor_scalar(mods[:, :, 2:3], mods[:, :, 2:3], 1.0, None, ALU.add), r=['mods'], w=['mods'])

        def load_expert(e_):
            s = e_ % 2
            for k0 in range(0, 8, 4):
                P.dma('pool', wg[s][:, k0:k0 + 4, :], wg_d[e_, :, k0:k0 + 4, :], w=['wg%d' % s])
                P.dma('pool', wu[s][:, k0:k0 + 4, :], wu_d[e_, :, k0:k0 + 4, :], w=['wu%d' % s])
            for f in range(2):
                P.dma('pool', wd[s][:, f, :], wd_d[e_, :, f, :], w=['wd%d' % s])

        load_expert(0)
        for i in range(NTILE):
            b = i % 2
            msel = 2 if i < 2 else 0
            rows = slice(i * 128, (i + 1) * 128)
            P.dma('sp', xs[b][:], xd[rows], w=['xs%d' % b])
            ln_tile(P, xs[b], 'xs%d' % b, st, mv, rs, '')

            def evac(k, src, skey, i=i, msel=msel):
                P.op('act', lambda e: e.activation(h2T[:, k, i * 128:(i + 1) * 128], src, AF.Identity,
                                                   bias=mods[:, k, msel + 1:msel + 2], scale=mods[:, k, msel:msel + 1]),
                     r=[skey, 'mods'], w=['h2T%d' % i])
                P.op('act', lambda e: e.activation(hTf[:, k, :], src, AF.Identity,
                                                   bias=mods[:, k, msel + 1:msel + 2], scale=mods[:, k, msel:msel + 1]),
                     r=[skey, 'mods'], w=['hTf'])
            transpose_to(P, xs[b], 'xs%d' % b, 8, tp, idt, None, None, evac)
            for k in range(8):
                P.op('pe', lambda e, k=k: e.matmul(pC[:, 0:36], hTf[:, k, :], wr[:, k, :], start=(k == 0), stop=(k == 7)),
                     r=['hTf', 'wr'], w=['pC'])
            P.op('dve', lambda e: e.tensor_tensor(lg[:], pC[:, 0:36], brB[:], ALU.add), r=['pC', 'brB'], w=['lg'])
            gmax, ohg, negm, eg, sume, lsel, m1, mk1 = [sm[j] for j in range(8)]
            l2, m2, mk2, tt = sm[8], sm[9], sm[10], sm[11]
            lgG = lg[:, 0:4]
            lgE = lg[:, 4:36].rearrange("p (g e) -> p g e", g=4)

            def dv(fn, r, w):
                P.op('dve', fn, r=r, w=w)
            dv(lambda e: e.tensor_reduce(gmax[:, 0:1], lgG, AX.X, ALU.max), ['lg'], ['gmax'])
            dv(lambda e: e.tensor_scalar(ohg[:, 0:4], lgG, gmax[:, 0:1], None, ALU.is_equal), ['lg', 'gmax'], ['ohg'])
            dv(lambda e: e.tensor_scalar(negm[:, 0:1], gmax[:, 0:1], -1.0, None, ALU.mult), ['gmax'], ['negm'])
            P.op('act', lambda e: e.activation(eg[:, 0:4], lgG, AF.Exp, bias=negm[:, 0:1], scale=1.0), r=['lg', 'negm'], w=['eg'])
            dv(lambda e: e.tensor_reduce(sume[:, 0:1], eg[:, 0:4], AX.X, ALU.add), ['eg'], ['sume'])
            dv(lambda e: e.reciprocal(sume[:, 0:1], sume[:, 0:1]), ['sume'], ['sume'])
            dv(lambda e: e.tensor_tensor(selg[:], lgE, ohg[:, 0:4].unsqueeze(2).to_broadcast([128, 4, 8]), ALU.mult),
               ['lg', 'ohg'], ['selg'])
            dv(lambda e: e.tensor_reduce(lsel[:], selg[:].rearrange("p g e -> p e g"), AX.X, ALU.add), ['selg'], ['lsel'])
            dv(lambda e: e.tensor_reduce(m1[:, 0:1], lsel[:], AX.X, ALU.max), ['lsel'], ['m1'])
            dv(lambda e: e.tensor_scalar(mk1[:], lsel[:], m1[:, 0:1], None, ALU.is_equal), ['lsel', 'm1'], ['mk1'])
            dv(lambda e: e.scalar_tensor_tensor(l2[:], mk1[:], -1e30, lsel[:], ALU.mult, ALU.add), ['mk1', 'lsel'], ['l2'])
            dv(lambda e: e.tensor_reduce(m2[:, 0:1], l2[:], AX.X, ALU.max), ['l2'], ['m2'])
            dv(lambda e: e.tensor_scalar(mk2[:], l2[:], m2[:, 0:1], None, ALU.is_equal), ['l2', 'm2'], ['mk2'])
            dv(lambda e: e.tensor_tensor(tt[:, 0:1], m2[:, 0:1], m1[:, 0:1], ALU.subtract), ['m2', 'm1'], ['tt'])
            P.op('act', lambda e: e.activation(tt[:, 1:2], tt[:, 0:1], AF.Exp), r=['tt'], w=['tt'])
            dv(lambda e: e.tensor_scalar(tt[:, 2:3], tt[:, 1:2], 1.0, None, ALU.add), ['tt'], ['tt'])
            dv(lambda e: e.reciprocal(tt[:, 2:3], tt[:, 2:3]), ['tt'], ['tt'])
            dv(lambda e: e.tensor_tensor(tt[:, 3:4], tt[:, 2:3], sume[:, 0:1], ALU.mult), ['tt', 'sume'], ['tt'])
            dv(lambda e: e.tensor_tensor(tt[:, 4:5], tt[:, 3:4], tt[:, 1:2], ALU.mult), ['tt'], ['tt'])
            dv(lambda e: e.tensor_scalar(mk1[:], mk1[:], tt[:, 3:4], None, ALU.mult), ['mk1', 'tt'], ['mk1'])
            dv(lambda e: e.scalar_tensor_tensor(mk2[:], mk2[:], tt[:, 4:5], mk1[:], ALU.mult, ALU.add), ['mk2', 'tt', 'mk1'], ['mk2'])
            dv(lambda e: e.tensor_tensor(comb[:], ohg[:, 0:4].unsqueeze(2).to_broadcast([128, 4, 8]),
                                         mk2[:].unsqueeze(1).to_broadcast([128, 4, 8]), ALU.mult), ['ohg', 'mk2'], ['comb'])
            P.op('pe', lambda e: e.transpose(pC[0:32, 128:256], comb[:].rearrange("p g e -> p (g e)"), idt[:]),
                 r=['comb', 'idt'], w=['pC'])
            P.op('act', lambda e, i=i: e.activation(combT[:, i * 128:(i + 1) * 128], pC[0:32, 128:256], AF.Copy),
                 r=['pC'], w=['combT'])

        stepn = [0]
        for e_ in range(NEXP):
            s = e_ % 2
            if e_ + 1 < NEXP:
                load_expert(e_ + 1)
            for (t0, n) in GROUPS:
                hb_ = stepn[0] % 2
                stepn[0] += 1
                tiles = ['h2T%d' % t for t in range(t0 // 128, (t0 + n) // 128)]
                P.op('pe', lambda e, e_=e_, t0=t0, n=n: e.matmul(pC[:, 0:n], sel[:, e_, :], combT[:, t0:t0 + n], start=True, stop=True),
                     r=['sel', 'combT'], w=['pC'])
                for fc in range(2):
                    ab = (2 * stepn[0] + fc) % 2
                    for k in range(8):
                        P.op('pe', lambda e, k=k, fc=fc, ab=ab, s=s, t0=t0, n=n: e.matmul(
                            pA[ab][:, 0:n], wg[s][:, k, fc * 128:(fc + 1) * 128], h2T[:, k, t0:t0 + n],
                            start=(k == 0), stop=(k == 7)), r=['wg%d' % s] + tiles, w=['pA%d' % ab])
                    for k in range(8):
                        P.op('pe', lambda e, k=k, fc=fc, s=s, t0=t0, n=n: e.matmul(
                            pU[0][:, 0:n], wu[s][:, k, fc * 128:(fc + 1) * 128], h2T[:, k, t0:t0 + n],
                            start=(k == 0), stop=(k == 7)), r=['wu%d' % s] + tiles, w=['pU0'])
                    P.op('act', lambda e, ab=ab, n=n: e.activation(sa[ab][:, 0:n], pA[ab][:, 0:n], AF.Silu),
                         r=['pA%d' % ab], w=['sa%d' % ab])
                    P.op('dve', lambda e, ab=ab, n=n: e.tensor_tensor(sa[ab][:, 0:n], sa[ab][:, 0:n], pU[0][:, 0:n], ALU.mult),
                         r=['sa%d' % ab, 'pU0'], w=['sa%d' % ab])
                    P.op('dve', lambda e, ab=ab, n=n, fc=fc, hb_=hb_: e.tensor_tensor(hid[hb_][:, fc, 0:n], sa[ab][:, 0:n], pC[:, 0:n], ALU.mult),
                         r=['sa%d' % ab, 'pC'], w=['hid%d' % hb_])
                for tt_ in range(n // 128):
                    ti = t0 // 128 + tt_
                    for half in range(2):
                        yb = (2 * ti + half) % 2
                        for fc in range(2):
                            P.op('pe', lambda e, fc=fc, half=half, yb=yb, hb_=hb_, tt_=tt_, s=s: e.matmul(
                                pY[yb][:, :], hid[hb_][:, fc, tt_ * 128:(tt_ + 1) * 128], wd[s][:, fc, half * 512:(half + 1) * 512],
                                start=(fc == 0), stop=(fc == 1)), r=['hid%d' % hb_, 'wd%d' % s], w=['pY%d' % yb])
                        ysl = yacc[:, ti, half * 512:(half + 1) * 512]
                        if e_ == 0:
                            P.op('dve', lambda e, yb=yb, ysl=ysl: e.tensor_copy(ysl, pY[yb][:, :]), r=['pY%d' % yb], w=['yacc%d' % ti])
                        else:
                            P.op('dve', lambda e, yb=yb, ysl=ysl: e.tensor_tensor(ysl, ysl, pY[yb][:, :], ALU.add),
                                 r=['pY%d' % yb, 'yacc%d' % ti], w=['yacc%d' % ti])
        for i in range(NTILE):
            b = i % 2
            gs = 1 if i < 2 else 0
            rows = slice(i * 128, (i + 1) * 128)
            P.dma('sp', xs[b][:], xd[rows], w=['xs%d' % b])
            P.op('dve', lambda e, i=i, gs=gs: e.tensor_tensor(yacc[:, i, :], yacc[:, i, :], g2B[:, gs, :], ALU.mult),
                 r=['yacc%d' % i, 'g2B'], w=['yacc%d' % i])
            P.op('dve', lambda e, i=i, b=b: e.scalar_tensor_tensor(xs[b][:], xs[b][:], ALPHA, yacc[:, i, :], ALU.mult, ALU.add),
                 r=['xs%d' % b, 'yacc%d' % i], w=['xs%d' % b])
            ln_tile(P, xs[b], 'xs%d' % b, st, mv, rs, '', lng, lnb, ['lng'])
            P.dma('sp', xo[rows], xs[b][:], r=['xs%d' % b])
        P.finish()
    return nc


def prep_C2_weights(w_rg, b_rg, w_re, b_re, w_e_gate, w_e_up, w_e_down, ln2_g, ln2_b, g2_lat, g2_ctx):
    wr = np.concatenate([w_rg, w_re], axis=1)
    br = np.concatenate([b_rg, b_re], axis=0)
    sel = np.zeros((32, NEXP, 128), np.float32)
    for e_ in range(NEXP):
        sel[e_, e_, :] = 1.0
    return {"wr": kp(wr, 8), "brB": bcast(br), "sel": sel,
            "wg": np.ascontiguousarray(w_e_gate.reshape(NEXP, 8, 128, DEXP).transpose(0, 2, 1, 3)),
            "wu": np.ascontiguousarray(w_e_up.reshape(NEXP, 8, 128, DEXP).transpose(0, 2, 1, 3)),
            "wd": np.ascontiguousarray(w_e_down.reshape(NEXP, 2, 128, D).transpose(0, 2, 1, 3)),
            "lngB": bcast(ln2_g), "lnbB": bcast(ln2_b),
            "g2B": np.ascontiguousarray(np.stack([bcast(g2_lat), bcast(g2_ctx)], axis=1)),
            "ident": np.eye(128, dtype=np.float32)}


_PROGS = {}


def _prog(name):
    if name not in _PROGS:
        _PROGS[name] = {"M": build_M, "A": build_A, "B1": build_B1, "B2": build_B2, "C1": build_C1, "C2": build_C2}[name]()
    return _PROGS[name]


def _run(name, in_maps):
    res = run_bass_kernel_spmd(_prog(name), in_maps, core_ids=list(range(NCORE)))
    return res.results


def _gather_tok(outs, key, axis):
    parts = [np.take(outs[0][key], np.arange(CTX), axis=axis)]
    for i in range(NCORE):
        parts.append(np.take(outs[i][key], np.arange(CTX, TC), axis=axis))
    return np.concatenate(parts, axis=axis)


def kernel(x, c, ctx, c_ctx, w_ada, b_ada, w_in, b_gates, w_uq, w_uk, w_uv, g_qn, g_kvn, g_mh,
           w_bo_mla, w_bo_mlstm, w_out, ln1_g, ln1_b, w_rg, b_rg, w_re, b_re,
           w_e_gate, w_e_up, w_e_down, ln2_g, ln2_b):
    f32 = np.float32
    x = np.asarray(x, f32)
    ctx = np.asarray(ctx, f32)
    ident = np.eye(128, dtype=f32)
    ones128 = np.ones((128, 128), f32)
    cc = np.ascontiguousarray(np.stack([np.asarray(c, f32)[0].reshape(8, 128).T, np.asarray(c_ctx, f32).reshape(8, 128).T], axis=-1))
    wall = np.concatenate([np.asarray(w_ada[l], f32) for l in range(DEPTH)], axis=1)
    ball = np.concatenate([np.asarray(b_ada[l], f32) for l in range(DEPTH)], axis=0)
    ins = []
    for i in range(NCORE):
        ins.append({"cc": cc, "wa": kp(wall[:, i * 3072:(i + 1) * 3072], 8),
                    "ba": np.ascontiguousarray(ball[i * 3072:(i + 1) * 3072].reshape(24, 128).T)})
    outs = _run("M", ins)
    mod = np.concatenate([o["mo"].transpose(1, 0, 2).reshape(3072, 2) for o in outs], axis=0).reshape(DEPTH, 6 * D, 2)
    del wall, ins

    cosT, ssinT = rope_tables_np()
    toks = [core_tokens(i) for i in range(NCORE)]
    cos4 = [np.ascontiguousarray(np.tile(cosT[t].T, (4, 1))) for t in toks]
    sin4 = [np.ascontiguousarray(np.tile(ssinT[t].T, (4, 1))) for t in toks]
    xc = [np.ascontiguousarray(np.concatenate([ctx[0], x[0, i * LAT_C:(i + 1) * LAT_C]], axis=0)) for i in range(NCORE)]
    perm_f = np.arange(T)
    perm_b = np.concatenate([np.arange(CTX - 1, -1, -1), np.arange(T - 1, CTX - 1, -1)])
    tri = np.triu(np.ones((64, 64), f32))
    ones64 = np.ones((64, 64), f32)
    onescol = np.ones((T, 1), f32)

    for l in range(DEPTH):
        m = mod[l]
        sh1, sc1, g1, sh2, sc2, g2 = [m[j * D:(j + 1) * D] for j in range(6)]
        mod1 = kp(np.stack([sc1[:, 0], sh1[:, 0], sc1[:, 1], sh1[:, 1]], axis=-1), 8)
        mod2 = kp(np.stack([sc2[:, 0], sh2[:, 0], sc2[:, 1], sh2[:, 1]], axis=-1), 8)
        wA = prep_A_weights(np.asarray(w_in[l], f32), np.asarray(w_uq[l], f32), np.asarray(w_uk[l], f32),
                            np.asarray(w_uv[l], f32), np.asarray(g_qn[l], f32), np.asarray(g_kvn[l], f32),
                            np.asarray(b_gates[l], f32))
        ins = []
        for i in range(NCORE):
            d_ = {"x": xc[i], "mod1": mod1, "ident": ident, "ones": ones128, "cos4": cos4[i], "ssin4": sin4[i]}
            d_.update(wA)
            ins.append(d_)
        oA = _run("A", ins)
        del ins, wA
        QT = _gather_tok(oA, "QT", 2).reshape(768, T)
        KnT = _gather_tok(oA, "KnT", 2).reshape(512, T)
        KrT = _gather_tok(oA, "KrT", 1)
        Vall = _gather_tok(oA, "V", 0)
        mqT = _gather_tok(oA, "mqT", 2).reshape(256, T)
        mkT = _gather_tok(oA, "mkT", 2).reshape(256, T)
        mkv = _gather_tok(oA, "mkv", 0)
        graw = _gather_tok(oA, "graw", 1)
        glsg = _gather_tok(oA, "glsg", 1)
        ins = []
        for h in range(8):
            Q = np.concatenate([QT[h * 64:(h + 1) * 64], QT[512 + h * 32:512 + (h + 1) * 32]], axis=0)
            Kk = np.concatenate([KnT[h * 64:(h + 1) * 64], KrT], axis=0)
            Vx = np.concatenate([Vall[:, h * 64:(h + 1) * 64], onescol], axis=1).reshape(NKT, 128, 65).transpose(1, 0, 2)
            ins.append({"Q": np.ascontiguousarray(Q), "K": np.ascontiguousarray(Kk), "V": np.ascontiguousarray(Vx)})
        oB1 = _run("B1", ins)
        ins = []
        for cidx in range(8):
            hd, dr = cidx // 2, cidx % 2
            perm = perm_b if dr else perm_f
            q_ = mqT[hd * 64:(hd + 1) * 64][:, perm]
            k_ = mkT[hd * 64:(hd + 1) * 64][:, perm]
            kt_ = mkv[perm, hd * 64:(hd + 1) * 64].reshape(NCH, 64, 64).transpose(1, 0, 2)
            vx_ = np.concatenate([mkv[perm, 256 + hd * 128:256 + (hd + 1) * 128], onescol], axis=1).reshape(NCH, 64, 129).transpose(1, 0, 2)
            ig_ = graw[(2 * dr) * 4 + hd][perm].reshape(NCH, 64).T
            lf_ = glsg[(2 * dr + 1) * 4 + hd][perm].reshape(NCH, 64).T
            ins.append({"qT": np.ascontiguousarray(q_), "kT": np.ascontiguousarray(k_), "kt": np.ascontiguousarray(kt_),
                        "vx": np.ascontiguousarray(vx_), "ig": np.ascontiguousarray(ig_), "lf": np.ascontiguousarray(lf_),
                        "tri": tri, "ones": ones64})
        oB2 = _run("B2", ins)
        del ins
        hf_all = np.empty((T, 512), f32)
        hb_all = np.empty((T, 512), f32)
        for cidx in range(8):
            hd, dr = cidx // 2, cidx % 2
            hp = oB2[cidx]["H"].transpose(1, 0, 2).reshape(T, 128)
            if dr:
                hb_all[perm_b, hd * 128:(hd + 1) * 128] = hp
            else:
                hf_all[:, hd * 128:(hd + 1) * 128] = hp
        o_all = np.stack([oB1[h]["OT"].T for h in range(8)], axis=1)
        wC1 = prep_C1_weights(np.asarray(w_bo_mla[l], f32), np.asarray(w_bo_mlstm[l], f32), np.asarray(w_out[l], f32),
                              np.asarray(g_mh[l], f32), np.asarray(ln1_g[l], f32), np.asarray(ln1_b[l], f32), g1[:, 0], g1[:, 1])
        ins = []
        for i in range(NCORE):
            d_ = {"x": xc[i], "o": np.ascontiguousarray(o_all[toks[i]]), "hf": np.ascontiguousarray(hf_all[toks[i]]),
                  "hb": np.ascontiguousarray(hb_all[toks[i]]), "spo": oA[i]["spo"], "spm": oA[i]["spm"]}
            d_.update(wC1)
            ins.append(d_)
        oC1 = _run("C1", ins)
        del ins, oA, o_all, hf_all, hb_all
        wC2 = prep_C2_weights(np.asarray(w_rg[l], f32), np.asarray(b_rg[l], f32), np.asarray(w_re[l], f32), np.asarray(b_re[l], f32),
                              np.asarray(w_e_gate[l], f32), np.asarray(w_e_up[l], f32), np.asarray(w_e_down[l], f32),
                              np.asarray(ln2_g[l], f32), np.asarray(ln2_b[l], f32), g2[:, 0], g2[:, 1])
        wC2["mod2"] = mod2
        ins = []
        for i in range(NCORE):
            d_ = {"x": oC1[i]["xo"]}
            d_.update(wC2)
            ins.append(d_)
        oC2 = _run("C2", ins)
        del ins, wC2
        xc = [np.ascontiguousarray(oC2[i]["xo"]) for i in range(NCORE)]
    out = np.concatenate([xc[i][CTX:] for i in range(NCORE)], axis=0)[None]
    return np.ascontiguousarray(out.astype(np.float32))
```

```python
import numpy as np
from contextlib import ExitStack
import concourse.bass as bass
import concourse.mybir as mybir
from concourse.bass_utils import run_bass_kernel_spmd

F32 = mybir.dt.float32
BF16 = mybir.dt.bfloat16
AF = mybir.ActivationFunctionType
ALU = mybir.AluOpType
AX = mybir.AxisListType

EPOCH = 16000
NDS = 12
ENGS = ('pe', 'act', 'dve', 'pool', 'sp')


class Prog:
    def __init__(self, nc, es):
        self.nc = nc
        self.es = es
        self.ops = {e: [] for e in ENGS}
        self.cnt = {e: 0 for e in ENGS}
        self.known = {e: {} for e in ENGS}
        self.lastw = {}
        self.readers = {}
        self.ndma = 0
        self.esems = {e: [] for e in ENGS}
        self.dsems = [es.enter_context(nc.semaphore("dsem%d" % j)) for j in range(NDS)]
        self.nt = 0

    def sb(self, shape, dt, name=None):
        self.nt += 1
        return self.es.enter_context(self.nc.sbuf_tensor(name or ("sb%d" % self.nt), list(shape), dt))

    def ps(self, shape, dt, name=None):
        self.nt += 1
        return self.es.enter_context(self.nc.psum_tensor(name or ("ps%d" % self.nt), list(shape), dt))

    def _esem(self, eng, idx):
        while len(self.esems[eng]) <= idx:
            self.esems[eng].append(self.es.enter_context(
                self.nc.semaphore("s_%s_%d" % (eng, len(self.esems[eng])))))
        return self.esems[eng][idx]

    def _deps(self, eng, r, w, extra=()):
        need = {}

        def add(p, v):
            if p is None:
                return
            if need.get(p, 0) < v:
                need[p] = v
        for k in r:
            lw = self.lastw.get(k)
            if lw:
                add(*lw)
        for k in w:
            lw = self.lastw.get(k)
            if lw:
                add(*lw)
            for p, v in self.readers.get(k, {}).items():
                add(p, v)
        for p, v in extra:
            add(p, v)
        waits = []
        kn = self.known[eng]
        for p, v in need.items():
            if p == eng and eng in ('pe', 'sp'):
                continue
            if kn.get(p, 0) >= v:
                continue
            kn[p] = v
            waits.append((p, v))
        return waits

    def op(self, eng, fn, r=(), w=()):
        waits = self._deps(eng, r, w)
        self.cnt[eng] += 1
        c = self.cnt[eng]
        self.ops[eng].append(('op', fn, waits, c))
        for k in r:
            d = self.readers.setdefault(k, {})
            d[eng] = c
        for k in w:
            self.lastw[k] = (eng, c)
            self.readers[k] = {}
        return c

    def dma(self, q, out, in_, r=(), w=(), **kw):
        j = self.ndma % NDS
        n = self.ndma // NDS + 1
        self.ndma += 1
        prod = ('d', j)
        waits = self._deps(q, r, w, extra=([(prod, n - 1)] if n > 1 else []))
        self.ops[q].append(('dma', (out, in_, kw), waits, (j, n)))
        for k in r:
            d = self.readers.setdefault(k, {})
            d[prod] = n
        for k in w:
            self.lastw[k] = (prod, n)
            self.readers[k] = {}

    def _emit_wait(self, e, p, v):
        if isinstance(p, tuple):
            e.wait_ge(self.dsems[p[1]], 16 * v)
        else:
            idx = (v - 1) // EPOCH
            e.wait_ge(self._esem(p, idx), (v - 1) % EPOCH + 1)

    def _run(self, name, e):
        for kind, payload, waits, c in self.ops[name]:
            for p, v in waits:
                self._emit_wait(e, p, v)
            if kind == 'op':
                ins = payload(e)
                idx = (c - 1) // EPOCH
                ins.then_inc(self._esem(name, idx), 1)
            else:
                out, in_, kw = payload
                j, n = c
                e.dma_start(out=out, in_=in_, **kw).then_inc(self.dsems[j], 16)
        if name == 'sp':
            tot = {}
            for j in range(NDS):
                n = (self.ndma - j + NDS - 1) // NDS if self.ndma > j else 0
                if n > 0:
                    e.wait_ge(self.dsems[j], 16 * n)

    def finish(self):
        nc = self.nc
        for e in ENGS:
            if self.cnt[e] > 0:
                self._esem(e, (self.cnt[e] - 1) // EPOCH)
        with nc.Block() as block:
            @block.sync
            def _(sync):
                self._run('sp', sync)

            @block.tensor
            def _(tensor):
                self._run('pe', tensor)

            @block.scalar
            def _(scalar):
                self._run('act', scalar)

            @block.vector
            def _(vector):
                self._run('dve', vector)

            @block.gpsimd
            def _(gpsimd):
                self._run('pool', gpsimd)


D = 1024
SEQ = 16384
CTX = 256
T = SEQ + CTX
NCORE = 8
LAT_C = SEQ // NCORE
TC = CTX + LAT_C
NTILE = TC // 128
GROUPS = [(0, 256), (256, 512), (768, 512), (1280, 512), (1792, 512)]
DEPTH = 4
EPS = 1e-6
ALPHA = (2 * DEPTH) ** 0.25
MLA_SCALE = 96 ** -0.5
NCH = T // 64


def dram_in(nc, name, shape, dt=F32):
    return nc.dram_tensor(name, list(shape), dt, kind="ExternalInput").ap()


def dram_out(nc, name, shape, dt=F32):
    return nc.dram_tensor(name, list(shape), dt, kind="ExternalOutput").ap()


def build_M():
    nc = bass.Bass("TRN2", target_bir_lowering=False)
    cc = dram_in(nc, "cc", [128, 8, 2])
    wa = dram_in(nc, "wa", [128, 8, 3072])
    ba = dram_in(nc, "ba", [128, 24])
    mo = dram_out(nc, "mo", [128, 24, 2])
    with ExitStack() as es:
        P = Prog(nc, es)
        ccs = P.sb([128, 8, 2], F32)
        was = P.sb([128, 8, 3072], F32)
        bas = P.sb([128, 24], F32)
        mos = P.sb([128, 24, 2], F32)
        pm = P.ps([128, 24, 2], F32)
        P.dma('sp', ccs[:], cc, w=['cc'])
        P.dma('sp', bas[:], ba, w=['ba'])
        for k in range(8):
            P.dma('sp', was[:, k, :], wa[:, k, :], w=['wa%d' % k])
        P.op('act', lambda e: e.activation(ccs[:], ccs[:], AF.Silu), r=['cc'], w=['cc'])
        for j in range(24):
            for k in range(8):
                P.op('pe', lambda e, j=j, k=k: e.matmul(pm[:, j, :], was[:, k, j * 128:(j + 1) * 128], ccs[:, k, :],
                                                       start=(k == 0), stop=(k == 7)),
                     r=['cc', 'wa%d' % k], w=['pm'])
        P.op('dve', lambda e: e.tensor_tensor(mos[:], pm[:], bas[:].unsqueeze(2).to_broadcast([128, 24, 2]), ALU.add),
             r=['pm', 'ba'], w=['mo'])
        P.dma('sp', mo, mos[:], r=['mo'])
        P.finish()
    return nc


A_QD, A_KVD, A_KR, A_KRP, A_MQ, A_MK, A_PG = 0, 384, 640, 672, 704, 960, 1216
A_TM = 1232
A_NCOL = A_TM + 256 + 512 + 512 + 2048


class Common:
    def __init__(self, P, nc):
        self.P = P
        self.nc = nc

    def load(self, q, dst, src, key, maxcols=None):
        self.P.dma(q, dst, src, w=[key])


def ln_rstd(P, var_ap, out_ap, rkeys, wkey, scale=1.0):
    P.op('act', lambda e: e.activation(out_ap, var_ap, AF.Ln, bias=EPS, scale=scale), r=rkeys, w=[wkey])
    P.op('act', lambda e: e.activation(out_ap, out_ap, AF.Exp, scale=-0.5), r=[wkey], w=[wkey])


def build_A():
    nc = bass.Bass("TRN2", target_bir_lowering=False)
    x = dram_in(nc, "x", [TC, D])
    mod1 = dram_in(nc, "mod1", [128, 8, 4])
    ident = dram_in(nc, "ident", [128, 128])
    ones_d = dram_in(nc, "ones", [128, 128])
    w_in = dram_in(nc, "w_in", [128, 8, A_NCOL])
    w_uq = dram_in(nc, "w_uq", [128, 3, 1024])
    w_uk = dram_in(nc, "w_uk", [128, 2, 512])
    w_uv = dram_in(nc, "w_uv", [128, 2, 512])
    gq_d = dram_in(nc, "gq", [128, 3])
    gkv_d = dram_in(nc, "gkv", [128, 2])
    bg_d = dram_in(nc, "bg", [16, 1])
    cos_d = dram_in(nc, "cos4", [128, TC])
    sin_d = dram_in(nc, "ssin4", [128, TC])
    QT = dram_out(nc, "QT", [6, 128, TC])
    KnT = dram_out(nc, "KnT", [4, 128, TC])
    KrT = dram_out(nc, "KrT", [32, TC])
    V = dram_out(nc, "V", [TC, 512])
    mqT = dram_out(nc, "mqT", [2, 128, TC])
    mkT = dram_out(nc, "mkT", [2, 128, TC])
    mkv = dram_out(nc, "mkv", [TC, 768])
    graw = dram_out(nc, "graw", [16, TC])
    glsg = dram_out(nc, "glsg", [16, TC])
    spo = dram_out(nc, "spo", [TC, 512])
    spm = dram_out(nc, "spm", [TC, 2048])
    with ExitStack() as es:
        P = Prog(nc, es)
        idt = P.sb([128, 128], F32)
        ones = P.sb([128, 128], F32)
        mods = P.sb([128, 8, 4], F32)
        win = P.sb([128, 8, A_NCOL], BF16)
        wuq = P.sb([128, 3, 1024], BF16)
        wuk = P.sb([128, 2, 512], BF16)
        wuv = P.sb([128, 2, 512], BF16)
        gq = P.sb([128, 3], F32)
        gkv = P.sb([128, 2], F32)
        bg = P.sb([16, 1], F32)
        cos4 = P.sb([128, TC], F32)
        sin4 = P.sb([128, TC], F32)
        hT = P.sb([128, 8, TC], BF16)
        xs = [P.sb([128, D], F32) for _ in range(2)]
        xn = [P.sb([128, D], F32) for _ in range(2)]
        st = [P.sb([128, 12], F32) for _ in range(2)]
        mv = [P.sb([128, 2], F32) for _ in range(2)]
        rs = [P.sb([128, 1], F32) for _ in range(2)]
        tp = P.ps([128, 8, 128], F32)
        pf = [P.ps([128, 512], F32) for _ in range(2)]
        pt = [P.ps([128, 512], F32) for _ in range(2)]
        pst = P.ps([128, 512], F32)
        dn = P.sb([128, 3, 512], F32)
        sq = P.sb([128, 3, 512], F32)
        rq = P.sb([128, 512], F32)
        dnn = P.sb([128, 3, 512], BF16)
        stg = [P.sb([128, 512], F32) for _ in range(3)]
        t1 = P.sb([128, 512], F32)
        t2 = P.sb([128, 512], F32)
        g1 = P.sb([16, 512], F32)
        g2 = P.sb([16, 512], F32)
        g3 = P.sb([16, 512], F32)

        P.dma('sp', idt[:], ident, w=['idt'])
        P.dma('sp', ones[:], ones_d, w=['ones'])
        P.dma('sp', mods[:], mod1, w=['mods'])
        P.dma('sp', gq[:], gq_d, w=['gq'])
        P.dma('sp', gkv[:], gkv_d, w=['gkv'])
        P.dma('sp', bg[:], bg_d, w=['bg'])
        P.dma('sp', cos4[:], cos_d, w=['cos'])
        P.dma('sp', sin4[:], sin_d, w=['sin'])
        for k in range(8):
            for c0 in range(0, A_NCOL, 1520):
                P.dma('pool', win[:, k, c0:c0 + 1520], w_in[:, k, c0:c0 + 1520], w=['win'])
        for j in range(3):
            P.dma('pool', wuq[:, j, :], w_uq[:, j, :], w=['wuq'])
        for j in range(2):
            P.dma('pool', wuk[:, j, :], w_uk[:, j, :], w=['wuk'])
            P.dma('pool', wuv[:, j, :], w_uv[:, j, :], w=['wuv'])
        P.op('dve', lambda e: e.tensor_scalar(mods[:, :, 0:1], mods[:, :, 0:1], 1.0, None, ALU.add), r=['mods'], w=['mods'])
        P.op('dve', lambda e: e.tensor_scalar(mods[:, :, 2:3], mods[:, :, 2:3], 1.0, None, ALU.add), r=['mods'], w=['mods'])

        for i in range(NTILE):
            b = i % 2
            sel = 2 if i < 2 else 0
            P.dma('sp', xs[b][:], x[i * 128:(i + 1) * 128, :], w=['xs%d' % b])
            for hh in range(2):
                P.op('dve', lambda e, b=b, hh=hh: e.bn_stats(st[b][:, hh * 6:(hh + 1) * 6], xs[b][:, hh * 512:(hh + 1) * 512]),
                     r=['xs%d' % b], w=['st%d' % b])
            P.op('dve', lambda e, b=b: e.bn_aggr(mv[b][:], st[b][:]), r=['st%d' % b], w=['mv%d' % b])
            ln_rstd(P, mv[b][:, 1:2], rs[b][:], ['mv%d' % b], 'rs%d' % b)
            P.op('dve', lambda e, b=b: e.tensor_scalar(xn[b][:], xs[b][:], mv[b][:, 0:1], rs[b][:, 0:1], ALU.subtract, ALU.mult),
                 r=['xs%d' % b, 'mv%d' % b, 'rs%d' % b], w=['xn%d' % b])
            for k in range(8):
                P.op('pe', lambda e, b=b, k=k: e.transpose(tp[:, k, :], xn[b][:, k * 128:(k + 1) * 128], idt[:]),
                     r=['xn%d' % b, 'idt'], w=['tp%d' % (k // 4)])
            for k in range(8):
                P.op('act', lambda e, i=i, k=k, sel=sel: e.activation(
                    hT[:, k, i * 128:(i + 1) * 128], tp[:, k, :], AF.Identity,
                    bias=mods[:, k, sel + 1:sel + 2], scale=mods[:, k, sel:sel + 1]),
                    r=['tp%d' % (k // 4), 'mods'], w=['hT%d' % i])

        fmi = [0]

        def fm_mm(col0, m, s, n):
            bi = fmi[0] % 2
            fmi[0] += 1
            ps = pf[bi]
            tiles = ['hT%d' % t for t in range(s // 128, (s + n) // 128)]
            for k in range(8):
                P.op('pe', lambda e, k=k, ps=ps: e.matmul(ps[0:m, 0:n], win[:, k, col0:col0 + m], hT[:, k, s:s + n],
                                                          start=(k == 0), stop=(k == 7)),
                     r=['win'] + tiles, w=['pf%d' % bi])
            return ps, 'pf%d' % bi

        sgi = [0]

        def stage_out(ps, pkey, m, n, dst, func=AF.Copy, scale=1.0, bias=None):
            si = sgi[0] % 3
            sgi[0] += 1
            sg = stg[si]
            if bias is None:
                P.op('act', lambda e: e.activation(sg[0:m, 0:n], ps[0:m, 0:n], func, scale=scale),
                     r=[pkey], w=['stg%d' % si])
            else:
                P.op('act', lambda e: e.activation(sg[0:m, 0:n], ps[0:m, 0:n], func, bias=bias, scale=scale),
                     r=[pkey, 'bg'], w=['stg%d' % si])
            P.dma('sp', dst, sg[0:m, 0:n], r=['stg%d' % si])

        def rms_block(col0, nch, gvec, gkey, dim, s, n):
            for j in range(nch):
                ps, pk = fm_mm(col0 + j * 128, 128, s, n)
                P.op('act', lambda e, j=j, ps=ps: e.activation(dn[:, j, 0:n], ps[:, 0:n], AF.Copy), r=[pk], w=['dn%d' % j])
                P.op('act', lambda e, j=j, ps=ps: e.activation(sq[:, j, 0:n], ps[:, 0:n], AF.Square), r=[pk], w=['sq%d' % j])
            for j in range(nch):
                P.op('pe', lambda e, j=j: e.matmul(pst[:, 0:n], ones[:], sq[:, j, 0:n], start=(j == 0), stop=(j == nch - 1)),
                     r=['ones', 'sq%d' % j], w=['pst'])
            ln_rstd(P, pst[:, 0:n], rq[:, 0:n], ['pst'], 'rq', scale=1.0 / dim)
            for j in range(nch):
                P.op('dve', lambda e, j=j: e.scalar_tensor_tensor(dnn[:, j, 0:n], dn[:, j, 0:n], gvec[:, j:j + 1], rq[:, 0:n],
                                                                 ALU.mult, ALU.mult),
                     r=['dn%d' % j, gkey, 'rq'], w=['dnn%d' % j])

        def up_mm(wt, wkey, nch, col0, m, n, tok0=None, tm=False):
            bi = fmi[0] % 2
            fmi[0] += 1
            ps = pf[bi]
            for j in range(nch):
                if not tm:
                    P.op('pe', lambda e, j=j, ps=ps: e.matmul(ps[0:m, 0:n], wt[:, j, col0:col0 + m], dnn[:, j, 0:n],
                                                              start=(j == 0), stop=(j == nch - 1)),
                         r=[wkey, 'dnn%d' % j], w=['pf%d' % bi])
                else:
                    P.op('pe', lambda e, j=j, ps=ps: e.matmul(ps[:, 0:m], dnn[:, j, tok0:tok0 + 128], wt[:, j, col0:col0 + m],
                                                              start=(j == 0), stop=(j == nch - 1)),
                         r=[wkey, 'dnn%d' % j], w=['pf%d' % bi])
            return ps, 'pf%d' % bi

        def rope_out(psA, kA, psP, kP, m, s, n, dst):
            P.op('dve', lambda e: e.tensor_tensor(t1[0:m, 0:n], psA[0:m, 0:n], cos4[0:m, s:s + n], ALU.mult),
                 r=[kA, 'cos'], w=['t1'])
            P.op('dve', lambda e: e.tensor_tensor(t2[0:m, 0:n], psP[0:m, 0:n], sin4[0:m, s:s + n], ALU.mult),
                 r=[kP, 'sin'], w=['t2'])
            P.op('dve', lambda e: e.tensor_tensor(t1[0:m, 0:n], t1[0:m, 0:n], t2[0:m, 0:n], ALU.add),
                 r=['t1', 't2'], w=['t1'])
            P.dma('sp', dst, t1[0:m, 0:n], r=['t1'])

        for (s, n) in GROUPS:
            rms_block(A_QD, 3, gq, 'gq', 384, s, n)
            for oc in range(4):
                ps, pk = up_mm(wuq, 'wuq', 3, oc * 128, 128, n)
                stage_out(ps, pk, 128, n, QT[oc, :, s:s + n])
            for c in range(2):
                psA, kA = up_mm(wuq, 'wuq', 3, 512 + c * 128, 128, n)
                psP, kP = up_mm(wuq, 'wuq', 3, 768 + c * 128, 128, n)
                rope_out(psA, kA, psP, kP, 128, s, n, QT[4 + c, :, s:s + n])
            rms_block(A_KVD, 2, gkv, 'gkv', 256, s, n)
            for oc in range(4):
                ps, pk = up_mm(wuk, 'wuk', 2, oc * 128, 128, n)
                stage_out(ps, pk, 128, n, KnT[oc, :, s:s + n])
            for tt in range(n // 128):
                ps, pk = up_mm(wuv, 'wuv', 2, 0, 512, n, tok0=tt * 128, tm=True)
                stage_out(ps, pk, 128, 512, V[s + tt * 128:s + (tt + 1) * 128, :])
            psA, kA = fm_mm(A_KR, 32, s, n)
            psP, kP = fm_mm(A_KRP, 32, s, n)
            rope_out(psA, kA, psP, kP, 32, s, n, KrT[:, s:s + n])
            for c in range(2):
                ps, pk = fm_mm(A_MQ + c * 128, 128, s, n)
                stage_out(ps, pk, 128, n, mqT[c, :, s:s + n], scale=0.125)
            for c in range(2):
                ps, pk = fm_mm(A_MK + c * 128, 128, s, n)
                stage_out(ps, pk, 128, n, mkT[c, :, s:s + n])
            ps, pk = fm_mm(A_PG, 16, s, n)
            P.op('act', lambda e, ps=ps: e.activation(g1[:, 0:n], ps[0:16, 0:n], AF.Identity, bias=bg[:, 0:1], scale=1.0),
                 r=[pk, 'bg'], w=['g1'])
            P.dma('sp', graw[:, s:s + n], g1[:, 0:n], r=['g1'])
            P.op('act', lambda e: e.activation(g2[:, 0:n], g1[:, 0:n], AF.Exp, scale=-1.0), r=['g1'], w=['g2'])
            P.op('act', lambda e: e.activation(g3[:, 0:n], g2[:, 0:n], AF.Ln, bias=1.0, scale=1.0), r=['g2'], w=['g3'])
            P.op('act', lambda e: e.activation(g3[:, 0:n], g3[:, 0:n], AF.Copy, scale=-1.0), r=['g3'], w=['g3'])
            P.dma('sp', glsg[:, s:s + n], g3[:, 0:n], r=['g3'])

        tmi = [0]
        tm_groups = [(A_TM, 256, mkv, 0, AF.Copy), (A_TM + 256, 512, mkv, 256, AF.Copy),
                     (A_TM + 768, 512, spo, 0, AF.Sigmoid)] + \
                    [(A_TM + 1280 + q * 512, 512, spm, q * 512, AF.Sigmoid) for q in range(4)]
        for i in range(NTILE):
            for (c0, ncl, dst, dc0, func) in tm_groups:
                bi = tmi[0] % 2
                tmi[0] += 1
                ps = pt[bi]
                for k in range(8):
                    P.op('pe', lambda e, k=k, ps=ps, c0=c0, ncl=ncl, i=i: e.matmul(
                        ps[:, 0:ncl], hT[:, k, i * 128:(i + 1) * 128], win[:, k, c0:c0 + ncl],
                        start=(k == 0), stop=(k == 7)), r=['win', 'hT%d' % i], w=['pt%d' % bi])
                stage_out(ps, 'pt%d' % bi, 128, ncl, dst[i * 128:(i + 1) * 128, dc0:dc0 + ncl], func=func)
        P.finish()
    return nc


ROPE_PERM = np.concatenate([np.arange(8, 16), np.arange(0, 8), np.arange(24, 32), np.arange(16, 24)])
ROPE_SIGN = np.concatenate([-np.ones(8), np.ones(8), -np.ones(8), np.ones(8)]).astype(np.float32)


def rope_tables_np():
    rows = SEQ // 64
    row, col = np.meshgrid(np.arange(rows, dtype=np.float32), np.arange(64, dtype=np.float32), indexing='ij')
    row, col = row.reshape(-1), col.reshape(-1)
    half = 16
    inv = (np.float32(10000.0) ** (-np.arange(0, half, 2, dtype=np.float32) / np.float32(half))).astype(np.float32)
    ar, ac = row[:, None] * inv, col[:, None] * inv
    ang = np.concatenate([ar, ar, ac, ac], axis=-1)
    ang = np.concatenate([np.zeros((CTX, 32), np.float32), ang], axis=0).astype(np.float32)
    return np.cos(ang).astype(np.float32), (np.sin(ang) * ROPE_SIGN).astype(np.float32)


def core_tokens(i):
    return np.concatenate([np.arange(CTX), CTX + np.arange(i * LAT_C, (i + 1) * LAT_C)])


def kp(a, nk):
    return np.ascontiguousarray(a.reshape(nk, 128, -1).transpose(1, 0, 2))


def prep_A_weights(w_in, w_uq, w_uk, w_uv, g_qn, g_kvn, b_gates):
    kr = 640 + ROPE_PERM
    colsA = np.concatenate([np.arange(0, 384), np.arange(384, 640), np.arange(640, 672), kr,
                            np.arange(672, 928), np.arange(928, 1184), np.arange(2208, 2224),
                            np.arange(928, 1184), np.arange(1184, 1696), np.arange(1696, 2208),
                            np.arange(2224, 4272)])
    assert len(colsA) == A_NCOL
    hq = np.arange(8)[:, None] * 96
    nope = (hq + np.arange(64)[None, :]).reshape(-1)
    rope = (hq + 64 + np.arange(32)[None, :]).reshape(-1)
    ropep = (hq + 64 + ROPE_PERM[None, :]).reshape(-1)
    colsq = np.concatenate([nope, rope, ropep])
    return {
        "w_in": kp(w_in[:, colsA], 8),
        "w_uq": kp(w_uq[:, colsq], 3),
        "w_uk": kp(w_uk, 2),
        "w_uv": kp(w_uv, 2),
        "gq": np.ascontiguousarray(g_qn.reshape(3, 128).T),
        "gkv": np.ascontiguousarray(g_kvn.reshape(2, 128).T),
        "bg": np.ascontiguousarray(b_gates.reshape(16, 1)),
    }


NKT = T // 128
QGROUPS = [(0, 256, 2)] + [(CTX + g * 512, 512, NKT) for g in range(SEQ // 512)]


def build_B1():
    nc = bass.Bass("TRN2", target_bir_lowering=False)
    Qd = dram_in(nc, "Q", [96, T])
    Kd = dram_in(nc, "K", [96, T])
    Vd = dram_in(nc, "V", [128, NKT, 65])
    Od = dram_out(nc, "O", [T, 65])
    with ExitStack() as es:
        P = Prog(nc, es)
        Qs = P.sb([96, T], BF16)
        Ks = P.sb([96, T], BF16)
        Vs = P.sb([128, NKT, 65], BF16)
        pTs = [P.sb([128, 512], BF16) for _ in range(3)]
        ost = [P.sb([128, 4, 65], F32) for _ in range(2)]
        pss = [P.ps([128, 512], F32) for _ in range(3)]
        pso = [P.ps([128, 512], F32) for _ in range(4)]
        CW = 1280
        for c0 in range(0, T, CW):
            P.dma('pool', Ks[:, c0:c0 + CW], Kd[:, c0:c0 + CW], w=['K%d' % (c0 // CW)])
            P.dma('pool', Qs[:, c0:c0 + CW], Qd[:, c0:c0 + CW], w=['Q%d' % (c0 // CW)])
        for t0 in range(0, NKT, 26):
            P.dma('pool', Vs[:, t0:t0 + 26, :], Vd[:, t0:t0 + 26, :], w=['V%d' % (t0 // 26)])
        steps = [(gi, kt) for gi, (q0, nq, nkt) in enumerate(QGROUPS) for kt in range(nkt)]

        def mm1(si):
            gi, kt = steps[si]
            q0, nq, nkt = QGROUPS[gi]
            sb_ = si % 3
            qkeys = ['Q%d' % j for j in range(q0 // CW, (q0 + nq - 1) // CW + 1)]
            P.op('pe', lambda e: e.matmul(pss[sb_][:, 0:nq], Ks[:, kt * 128:(kt + 1) * 128], Qs[:, q0:q0 + nq],
                                          start=True, stop=True),
                 r=['K%d' % ((kt * 128) // CW)] + qkeys, w=['pss%d' % sb_])

        LA = 2
        for si in range(min(LA, len(steps))):
            mm1(si)
        for si, (gi, kt) in enumerate(steps):
            q0, nq, nkt = QGROUPS[gi]
            sb_ = si % 3
            if si + LA < len(steps):
                mm1(si + LA)
            P.op('act', lambda e, sb_=sb_, nq=nq: e.activation(pTs[sb_][:, 0:nq], pss[sb_][:, 0:nq], AF.Exp, scale=MLA_SCALE),
                 r=['pss%d' % sb_], w=['pT%d' % sb_])
            for qb in range(nq // 128):
                P.op('pe', lambda e, sb_=sb_, kt=kt, qb=qb, nkt=nkt: e.matmul(
                    pso[qb][:, 0:65], pTs[sb_][:, qb * 128:(qb + 1) * 128], Vs[:, kt, :],
                    start=(kt == 0), stop=(kt == nkt - 1)),
                    r=['V%d' % (kt // 26), 'pT%d' % sb_], w=['pso%d' % qb])
            if kt == nkt - 1:
                ob = gi % 2
                nb = nq // 128
                for qb in range(nb):
                    P.op('dve', lambda e, ob=ob, qb=qb: e.tensor_copy(ost[ob][:, qb, :], pso[qb][:, 0:65]),
                         r=['pso%d' % qb], w=['ost%d' % ob])
                P.dma('sp', Od[q0:q0 + nq, :].rearrange("(b p) c -> p b c", p=128), ost[ob][:, 0:nb, :], r=['ost%d' % ob])
        P.finish()
    return nc


CB = 20
NBLK = NCH // CB
RING = 4


def build_B2():
    nc = bass.Bass("TRN2", target_bir_lowering=False)
    qTd = dram_in(nc, "qT", [64, T])
    kTd = dram_in(nc, "kT", [64, T])
    ktd = dram_in(nc, "kt", [64, NCH, 64])
    vxd = dram_in(nc, "vx", [64, NCH, 129])
    igd = dram_in(nc, "ig", [64, NCH])
    lfd = dram_in(nc, "lf", [64, NCH])
    trid = dram_in(nc, "tri", [64, 64])
    oned = dram_in(nc, "ones", [64, 64])
    H = dram_out(nc, "H", [64, NCH, 128])
    with ExitStack() as es:
        P = Prog(nc, es)
        qT = P.sb([64, T], BF16)
        kT = P.sb([64, T], BF16)
        kt = P.sb([64, NCH, 64], BF16)
        ig = P.sb([64, NCH], F32)
        lf = P.sb([64, NCH], F32)
        tri = P.sb([64, 64], F32)
        ones = P.sb([64, 64], F32)
        bb = P.sb([64, NCH], F32)
        ebl = P.sb([64, NCH], F32)
        ek = P.sb([64, NCH], F32)
        ek2 = P.sb([64, NCH], F32)
        eb = P.sb([64, NCH], F32)
        tmp = P.sb([64, NCH], F32)
        vx = [P.sb([64, CB, 129], F32) for _ in range(2)]
        v1 = [P.sb([64, CB, 129], BF16) for _ in range(2)]
        v2 = [P.sb([64, CB, 129], BF16) for _ in range(2)]
        hr = [P.sb([64, CB, 129], F32) for _ in range(2)]
        ho = [P.sb([64, CB, 128], F32) for _ in range(2)]
        tn = [P.sb([64, CB], F32) for _ in range(2)]
        MT = [P.sb([64, 64], BF16) for _ in range(3)]
        Cst = [P.sb([64, 129], F32) for _ in range(2)]
        Cbf = [P.sb([64, 129], BF16) for _ in range(RING)]
        pb = P.ps([64, 512], F32)
        pbl = P.ps([64, 512], F32)
        ps_s = [P.ps([64, 512], F32) for _ in range(2)]
        ps_h = [P.ps([64, 512], F32) for _ in range(2)]
        ps_u = [P.ps([64, 512], F32) for _ in range(2)]
        CW = 1280
        for c0 in range(0, T, CW):
            P.dma('pool', qT[:, c0:c0 + CW], qTd[:, c0:c0 + CW], w=['qT%d' % (c0 // CW)])
            P.dma('pool', kT[:, c0:c0 + CW], kTd[:, c0:c0 + CW], w=['kT%d' % (c0 // CW)])
        for b in range(NBLK):
            P.dma('pool', kt[:, b * CB:(b + 1) * CB, :], ktd[:, b * CB:(b + 1) * CB, :], w=['kt%d' % b])
        P.dma('sp', ig[:], igd, w=['ig'])
        P.dma('sp', lf[:], lfd, w=['lf'])
        P.dma('sp', tri[:], trid, w=['tri'])
        P.dma('sp', ones[:], oned, w=['ones'])
        P.op('pe', lambda e: e.matmul(pb[:, 0:NCH], tri[:], lf[:], start=True, stop=True), r=['tri', 'lf'], w=['pb'])
        P.op('pe', lambda e: e.matmul(pbl[:, 0:NCH], ones[:], lf[:], start=True, stop=True), r=['ones', 'lf'], w=['pbl'])
        P.op('dve', lambda e: e.tensor_copy(bb[:], pb[:, 0:NCH]), r=['pb'], w=['bb'])
        P.op('act', lambda e: e.activation(ebl[:], pbl[:, 0:NCH], AF.Exp), r=['pbl'], w=['ebl'])
        P.op('act', lambda e: e.activation(eb[:], bb[:], AF.Exp), r=['bb'], w=['eb'])
        P.op('dve', lambda e: e.tensor_tensor(tmp[:], ig[:], bb[:], ALU.subtract), r=['ig', 'bb'], w=['tmp'])
        P.op('act', lambda e: e.activation(ek[:], tmp[:], AF.Exp), r=['tmp'], w=['ek'])
        P.op('dve', lambda e: e.tensor_tensor(tmp[:], tmp[:], pbl[:, 0:NCH], ALU.add), r=['tmp', 'pbl', 'ek'], w=['tmp2'])
        P.op('act', lambda e: e.activation(ek2[:], tmp[:], AF.Exp), r=['tmp2'], w=['ek2'])
        P.op('dve', lambda e: e.memset(Cst[0][:], 0.0), w=['Cst0'])
        P.op('dve', lambda e: e.memset(Cbf[0][:], 0.0), w=['Cbf0'])

        def load_block(b):
            s = b % 2
            P.dma('sp', vx[s][:], vxd[:, b * CB:(b + 1) * CB, :], w=['vx%d' % s])
            P.op('dve', lambda e: e.tensor_tensor(v1[s][:], vx[s][:],
                                                  ek[:, b * CB:(b + 1) * CB].unsqueeze(2).to_broadcast([64, CB, 129]), ALU.mult),
                 r=['vx%d' % s, 'ek'], w=['v1_%d' % s])
            P.op('dve', lambda e: e.tensor_tensor(v2[s][:], vx[s][:],
                                                  ek2[:, b * CB:(b + 1) * CB].unsqueeze(2).to_broadcast([64, CB, 129]), ALU.mult),
                 r=['vx%d' % s, 'ek2'], w=['v2_%d' % s])

        def front(c):
            b, ci = divmod(c, CB)
            s = b % 2
            tk = ['qT%d' % ((c * 64) // CW), 'kT%d' % ((c * 64) // CW)]
            P.op('pe', lambda e: e.matmul(ps_s[c % 2][:, 0:64], kT[:, c * 64:(c + 1) * 64], qT[:, c * 64:(c + 1) * 64],
                                          start=True, stop=True), r=tk, w=['ps_s%d' % (c % 2)])
            P.op('dve', lambda e: e.tensor_tensor(MT[c % 3][:], ps_s[c % 2][:, 0:64], tri[:], ALU.mult),
                 r=['ps_s%d' % (c % 2), 'tri'], w=['MT%d' % (c % 3)])
            P.op('pe', lambda e: e.matmul(ps_u[c % 2][:, 0:129], kt[:, c, :], v2[s][:, ci, :], start=True, stop=True),
                 r=['kt%d' % b, 'v2_%d' % s], w=['ps_u%d' % (c % 2)])

        load_block(0)
        front(0)
        for c in range(NCH):
            b, ci = divmod(c, CB)
            s = b % 2
            if ci == 0 and b + 1 < NBLK:
                load_block(b + 1)
            if c + 1 < NCH:
                front(c + 1)
            P.op('pe', lambda e, c=c, s=s, ci=ci: e.matmul(ps_h[c % 2][:, 0:129], MT[c % 3][:], v1[s][:, ci, :], start=True, stop=False),
                 r=['MT%d' % (c % 3), 'v1_%d' % s], w=['ps_h%d' % (c % 2)])
            P.op('pe', lambda e, c=c: e.matmul(ps_h[c % 2][:, 0:129], qT[:, c * 64:(c + 1) * 64], Cbf[c % RING][:], start=False, stop=True),
                 r=['qT%d' % ((c * 64) // CW), 'Cbf%d' % (c % RING)], w=['ps_h%d' % (c % 2)])
            P.op('act', lambda e, c=c, s=s, ci=ci: e.activation(hr[s][:, ci, :], ps_h[c % 2][:, 0:129], AF.Copy),
                 r=['ps_h%d' % (c % 2)], w=['hr%d' % s])
            if c + 1 < NCH:
                P.op('dve', lambda e, c=c: e.scalar_tensor_tensor(Cst[(c + 1) % 2][:], Cst[c % 2][:], ebl[:, c:c + 1],
                                                                 ps_u[c % 2][:, 0:129], ALU.mult, ALU.add),
                     r=['Cst%d' % (c % 2), 'ebl', 'ps_u%d' % (c % 2)], w=['Cst%d' % ((c + 1) % 2)])
                P.op('act', lambda e, c=c: e.activation(Cbf[(c + 1) % RING][:], Cst[(c + 1) % 2][:], AF.Copy),
                     r=['Cst%d' % ((c + 1) % 2)], w=['Cbf%d' % ((c + 1) % RING)])
            if ci == CB - 1:
                sl = slice(b * CB, (b + 1) * CB)
                P.op('dve', lambda e, s=s, sl=sl: e.tensor_tensor(tn[s][:], hr[s][:, :, 128], eb[:, sl], ALU.mult),
                     r=['hr%d' % s, 'eb'], w=['tn%d' % s])
                P.op('act', lambda e, s=s: e.activation(tn[s][:], tn[s][:], AF.Abs), r=['tn%d' % s], w=['tn%d' % s])
                P.op('dve', lambda e, s=s: e.tensor_scalar(tn[s][:], tn[s][:], 1.0, None, ALU.max), r=['tn%d' % s], w=['tn%d' % s])
                P.op('dve', lambda e, s=s: e.reciprocal(tn[s][:], tn[s][:]), r=['tn%d' % s], w=['tn%d' % s])
                P.op('dve', lambda e, s=s, sl=sl: e.tensor_tensor(tn[s][:], tn[s][:], eb[:, sl], ALU.mult),
                     r=['tn%d' % s, 'eb'], w=['tn%d' % s])
                P.op('dve', lambda e, s=s: e.tensor_tensor(ho[s][:], hr[s][:, :, 0:128],
                                                           tn[s][:].unsqueeze(2).to_broadcast([64, CB, 128]), ALU.mult),
                     r=['hr%d' % s, 'tn%d' % s], w=['ho%d' % s])
                P.dma('sp', H[:, sl, :], ho[s][:], r=['ho%d' % s])
        P.finish()
    return nc


def ln_tile(P, z, zkey, st, mv, rs, sfx, gB=None, bB=None, gkeys=()):
    for hh in range(2):
        P.op('dve', lambda e, hh=hh: e.bn_stats(st[:, hh * 6:(hh + 1) * 6], z[:, hh * 512:(hh + 1) * 512]),
             r=[zkey], w=['st' + sfx])
    P.op('dve', lambda e: e.bn_aggr(mv[:], st[:]), r=['st' + sfx], w=['mv' + sfx])
    ln_rstd(P, mv[:, 1:2], rs[:], ['mv' + sfx], 'rs' + sfx)
    P.op('dve', lambda e: e.tensor_scalar(z[:], z[:], mv[:, 0:1], rs[:, 0:1], ALU.subtract, ALU.mult),
         r=[zkey, 'mv' + sfx, 'rs' + sfx], w=[zkey])
    if gB is not None:
        P.op('dve', lambda e: e.tensor_tensor(z[:], z[:], gB[:], ALU.mult), r=[zkey] + list(gkeys), w=[zkey])
        P.op('dve', lambda e: e.tensor_tensor(z[:], z[:], bB[:], ALU.add), r=[zkey] + list(gkeys), w=[zkey])


def transpose_to(P, src, skey, nch, tp, idt, dst_fn, dkeys, evac):
    for k in range(nch):
        P.op('pe', lambda e, k=k: e.transpose(tp[:, k, :], src[:, k * 128:(k + 1) * 128], idt[:]),
             r=[skey, 'idt'], w=['tp%d' % (k // 4)])
    for k in range(nch):
        evac(k, tp[:, k, :], 'tp%d' % (k // 4))


def build_C1():
    nc = bass.Bass("TRN2", target_bir_lowering=False)
    xd = dram_in(nc, "x", [TC, D])
    od = dram_in(nc, "o", [TC, 8, 65])
    hfd = dram_in(nc, "hf", [TC, 512])
    hbd = dram_in(nc, "hb", [TC, 512])
    spod = dram_in(nc, "spo", [TC, 512])
    spmd = dram_in(nc, "spm", [TC, 2048])
    ident = dram_in(nc, "ident", [128, 128])
    wmla_d = dram_in(nc, "wmla", [128, 4, D])
    wmls_d = dram_in(nc, "wmls", [128, 4, D])
    wout_d = dram_in(nc, "wout", [128, 8, D])
    gmh_d = dram_in(nc, "gmhB", [128, 512])
    g1_d = dram_in(nc, "g1B", [128, 2, D])
    lng_d = dram_in(nc, "lngB", [128, D])
    lnb_d = dram_in(nc, "lnbB", [128, D])
    xo = dram_out(nc, "xo", [TC, D])
    with ExitStack() as es:
        P = Prog(nc, es)
        idt = P.sb([128, 128], F32)
        wmla = P.sb([128, 4, D], BF16)
        wmls = P.sb([128, 4, D], BF16)
        wout = P.sb([128, 8, D], BF16)
        gmh = P.sb([128, 512], F32)
        g1B = P.sb([128, 2, D], F32)
        lng = P.sb([128, D], F32)
        lnb = P.sb([128, D], F32)
        xs = [P.sb([128, D], F32) for _ in range(2)]
        os_ = [P.sb([128, 8, 65], F32) for _ in range(2)]
        hf = [P.sb([128, 512], F32) for _ in range(2)]
        hb = [P.sb([128, 512], F32) for _ in range(2)]
        po = [P.sb([128, 512], F32) for _ in range(2)]
        pm = [P.sb([128, 2048], F32) for _ in range(2)]
        rec = P.sb([128, 8], F32)
        on = P.sb([128, 512], F32)
        onT = P.sb([128, 4, 128], BF16)
        hnT = P.sb([128, 4, 128], BF16)
        ymT = P.sb([128, 8, 128], BF16)
        s4 = P.sb([128, 4], F32)
        v4 = P.sb([128, 4], F32)
        cen = P.sb([128, 512], F32)
        sqv = P.sb([128, 512], F32)
        ta = P.sb([128, D], F32)
        tb = P.sb([128, D], F32)
        st = P.sb([128, 12], F32)
        mv = P.sb([128, 2], F32)
        rs = P.sb([128, 1], F32)
        tp = P.ps([128, 8, 128], F32)
        pA = P.ps([128, 2, 512], F32)
        pB = P.ps([128, 2, 512], F32)
        pY = P.ps([128, 2, 512], F32)
        P.dma('sp', idt[:], ident, w=['idt'])
        P.dma('sp', gmh[:], gmh_d, w=['gmh'])
        P.dma('sp', g1B[:], g1_d, w=['g1B'])
        P.dma('sp', lng[:], lng_d, w=['lng'])
        P.dma('sp', lnb[:], lnb_d, w=['lng'])
        for k in range(4):
            P.dma('pool', wmla[:, k, :], wmla_d[:, k, :], w=['wmla'])
            P.dma('pool', wmls[:, k, :], wmls_d[:, k, :], w=['wmls'])
        for k in range(8):
            P.dma('pool', wout[:, k, :], wout_d[:, k, :], w=['wout'])

        def evac_to(dstT, dkey):
            def f(k, src, skey):
                P.op('act', lambda e: e.activation(dstT[:, k, :], src, AF.Copy), r=[skey], w=[dkey])
            return f

        def proj(ps, pskey, lT, lkey, w, wkey, nk):
            for half in range(2):
                for k in range(nk):
                    P.op('pe', lambda e, half=half, k=k: e.matmul(ps[:, half, :], lT[:, k, :], w[:, k, half * 512:(half + 1) * 512],
                                                                 start=(k == 0), stop=(k == nk - 1)),
                         r=[lkey, wkey], w=[pskey + str(half)])

        for i in range(NTILE):
            b = i % 2
            sel = 1 if i < 2 else 0
            rows = slice(i * 128, (i + 1) * 128)
            P.dma('sp', os_[b][:], od[rows], w=['o%d' % b])
            P.dma('sp', hf[b][:], hfd[rows], w=['hf%d' % b])
            P.dma('sp', hb[b][:], hbd[rows], w=['hb%d' % b])
            P.dma('sp', po[b][:], spod[rows], w=['po%d' % b])
            P.dma('sp', pm[b][:], spmd[rows], w=['pm%d' % b])
            P.dma('sp', xs[b][:], xd[rows], w=['xs%d' % b])
            P.op('dve', lambda e, b=b: e.reciprocal(rec[:], os_[b][:, :, 64]), r=['o%d' % b], w=['rec'])
            P.op('dve', lambda e, b=b: e.tensor_tensor(on[:].rearrange("p (h d) -> p h d", h=8), os_[b][:, :, 0:64],
                                                       rec[:].unsqueeze(2).to_broadcast([128, 8, 64]), ALU.mult),
                 r=['o%d' % b, 'rec'], w=['on'])
            transpose_to(P, on, 'on', 4, tp, idt, None, None, evac_to(onT, 'onT'))
            proj(pA, 'pA', onT, 'onT', wmla, 'wmla', 4)
            P.op('dve', lambda e, b=b: e.tensor_tensor(cen[:], hf[b][:], hb[b][:], ALU.add), r=['hf%d' % b, 'hb%d' % b], w=['cen'])
            c3 = cen[:].rearrange("p (h d) -> p h d", h=4)
            P.op('dve', lambda e: e.tensor_reduce(s4[:], c3, AX.X, ALU.add), r=['cen'], w=['s4'])
            P.op('dve', lambda e: e.tensor_scalar(s4[:], s4[:], -1.0 / 128, None, ALU.mult), r=['s4'], w=['s4'])
            P.op('dve', lambda e: e.tensor_tensor(c3, c3, s4[:].unsqueeze(2).to_broadcast([128, 4, 128]), ALU.add),
                 r=['cen', 's4'], w=['cen'])
            P.op('dve', lambda e: e.tensor_tensor(sqv[:], cen[:], cen[:], ALU.mult), r=['cen'], w=['sqv'])
            P.op('dve', lambda e: e.tensor_reduce(v4[:], sqv[:].rearrange("p (h d) -> p h d", h=4), AX.X, ALU.add),
                 r=['sqv'], w=['v4'])
            ln_rstd(P, v4[:], v4[:], ['v4'], 'v4', scale=1.0 / 128)
            P.op('dve', lambda e: e.tensor_tensor(c3, c3, v4[:].unsqueeze(2).to_broadcast([128, 4, 128]), ALU.mult),
                 r=['cen', 'v4'], w=['cen'])
            P.op('dve', lambda e: e.tensor_tensor(cen[:], cen[:], gmh[:], ALU.mult), r=['cen', 'gmh'], w=['cen'])
            P.op('dve', lambda e, b=b: e.tensor_tensor(cen[:], cen[:], po[b][:], ALU.mult), r=['cen', 'po%d' % b], w=['cen'])
            transpose_to(P, cen, 'cen', 4, tp, idt, None, None, evac_to(hnT, 'hnT'))
            proj(pB, 'pB', hnT, 'hnT', wmls, 'wmls', 4)
            for half in range(2):
                hs = slice(half * 512, (half + 1) * 512)
                P.op('dve', lambda e, b=b, half=half, hs=hs: e.tensor_tensor(ta[:, hs], pA[:, half, :], pm[b][:, hs], ALU.mult),
                     r=['pA%d' % half, 'pm%d' % b], w=['ta'])
                P.op('dve', lambda e, b=b, half=half, hs=hs: e.tensor_tensor(
                    tb[:, hs], pB[:, half, :], pm[b][:, 1024 + half * 512:1024 + (half + 1) * 512], ALU.mult),
                    r=['pB%d' % half, 'pm%d' % b], w=['tb'])
            P.op('dve', lambda e: e.tensor_tensor(ta[:], ta[:], tb[:], ALU.add), r=['ta', 'tb'], w=['ta'])
            transpose_to(P, ta, 'ta', 8, tp, idt, None, None, evac_to(ymT, 'ymT'))
            proj(pY, 'pY', ymT, 'ymT', wout, 'wout', 8)
            for half in range(2):
                hs = slice(half * 512, (half + 1) * 512)
                P.op('dve', lambda e, half=half, hs=hs, sel=sel: e.tensor_tensor(tb[:, hs], pY[:, half, :], g1B[:, sel, hs], ALU.mult),
                     r=['pY%d' % half, 'g1B'], w=['tb'])
            P.op('dve', lambda e, b=b: e.scalar_tensor_tensor(tb[:], xs[b][:], ALPHA, tb[:], ALU.mult, ALU.add),
                 r=['xs%d' % b, 'tb'], w=['tb'])
            ln_tile(P, tb, 'tb', st, mv, rs, '', lng, lnb, ['lng'])
            P.dma('sp', xo[rows], tb[:], r=['tb'])
        P.finish()
    return nc


def bcast(v, p=128):
    return np.ascontiguousarray(np.broadcast_to(np.asarray(v, np.float32).reshape(1, -1), (p, np.asarray(v).size)))


def prep_C1_weights(w_bo_mla, w_bo_mlstm, w_out, g_mh, ln1_g, ln1_b, g1_lat, g1_ctx):
    return {"wmla": kp(w_bo_mla, 4), "wmls": kp(w_bo_mlstm, 4), "wout": kp(w_out, 8),
            "gmhB": bcast(g_mh), "lngB": bcast(ln1_g), "lnbB": bcast(ln1_b),
            "g1B": np.ascontiguousarray(np.stack([bcast(g1_lat), bcast(g1_ctx)], axis=1)),
            "ident": np.eye(128, dtype=np.float32)}


NEXP = 32
DEXP = 256


def build_C2():
    nc = bass.Bass("TRN2", target_bir_lowering=False)
    xd = dram_in(nc, "x", [TC, D])
    ident = dram_in(nc, "ident", [128, 128])
    mod2 = dram_in(nc, "mod2", [128, 8, 4])
    wr_d = dram_in(nc, "wr", [128, 8, 36])
    br_d = dram_in(nc, "brB", [128, 36])
    g2_d = dram_in(nc, "g2B", [128, 2, D])
    lng_d = dram_in(nc, "lngB", [128, D])
    lnb_d = dram_in(nc, "lnbB", [128, D])
    wg_d = dram_in(nc, "wg", [NEXP, 128, 8, DEXP])
    wu_d = dram_in(nc, "wu", [NEXP, 128, 8, DEXP])
    wd_d = dram_in(nc, "wd", [NEXP, 128, 2, D])
    xo = dram_out(nc, "xo", [TC, D])
    with ExitStack() as es:
        P = Prog(nc, es)
        idt = P.sb([128, 128], F32)
        mods = P.sb([128, 8, 4], F32)
        wr = P.sb([128, 8, 36], F32)
        brB = P.sb([128, 36], F32)
        g2B = P.sb([128, 2, D], F32)
        lng = P.sb([128, D], F32)
        lnb = P.sb([128, D], F32)
        h2T = P.sb([128, 8, TC], BF16)
        hTf = P.sb([128, 8, 128], F32)
        yacc = P.sb([128, NTILE, D], F32)
        comb_all = P.sb([128, NTILE, 32], F32)
        wg = [P.sb([128, 8, DEXP], BF16) for _ in range(2)]
        wu = [P.sb([128, 8, DEXP], BF16) for _ in range(2)]
        wd = [P.sb([128, 2, D], BF16) for _ in range(2)]
        xs = [P.sb([128, D], F32) for _ in range(2)]
        st = P.sb([128, 12], F32)
        mv = P.sb([128, 2], F32)
        rs = P.sb([128, 1], F32)
        lg = P.sb([128, 36], F32)
        sm = [P.sb([128, 8], F32, name="sm%d" % j) for j in range(12)]
        selg = P.sb([128, 4, 8], F32)
        sa = [P.sb([128, 512], F32) for _ in range(2)]
        hid = [P.sb([128, 2, 512], BF16) for _ in range(2)]
        tp = P.ps([128, 8, 128], F32)
        pA = [P.ps([128, 512], F32) for _ in range(2)]
        pU = [P.ps([128, 512], F32) for _ in range(2)]
        pY = [P.ps([128, 512], F32) for _ in range(2)]
        pC = pY[0]
        P.dma('sp', idt[:], ident, w=['idt'])
        P.dma('sp', mods[:], mod2, w=['mods'])
        P.dma('sp', wr[:], wr_d, w=['wr'])
        P.dma('sp', brB[:], br_d, w=['brB'])
        P.dma('sp', g2B[:], g2_d, w=['g2B'])
        P.dma('sp', lng[:], lng_d, w=['lng'])
        P.dma('sp', lnb[:], lnb_d, w=['lng'])
        P.op('dve', lambda e: e.tensor_scalar(mods[:, :, 0:1], mods[:, :, 0:1], 1.0, None, ALU.add), r=['mods'], w=['mods'])
        P.op('dve', lambda e: e.tensor_scalar(mods[:, :, 2:3], mods[:, :, 2:3], 1.0, None, ALU.add), r=['mods'], w=['mods'])

        def load_expert(e_):
            s = e_ % 2
            for k0 in range(0, 8, 4):
                P.dma('pool', wg[s][:, k0:k0 + 4, :], wg_d[e_, :, k0:k0 + 4, :], w=['wg%d' % s])
                P.dma('pool', wu[s][:, k0:k0 + 4, :], wu_d[e_, :, k0:k0 + 4, :], w=['wu%d' % s])
            for f in range(2):
                P.dma('pool', wd[s][:, f, :], wd_d[e_, :, f, :], w=['wd%d' % s])

        load_expert(0)
        for i in range(NTILE):
            b = i % 2
            msel = 2 if i < 2 else 0
            rows = slice(i * 128, (i + 1) * 128)
            P.dma('sp', xs[b][:], xd[rows], w=['xs%d' % b])
            ln_tile(P, xs[b], 'xs%d' % b, st, mv, rs, '')

            def evac(k, src, skey, i=i, msel=msel):
                P.op('act', lambda e: e.activation(h2T[:, k, i * 128:(i + 1) * 128], src, AF.Identity,
                                                   bias=mods[:, k, msel + 1:msel + 2], scale=mods[:, k, msel:msel + 1]),
                     r=[skey, 'mods'], w=['h2T%d' % i])
                P.op('act', lambda e: e.activation(hTf[:, k, :], src, AF.Identity,
                                                   bias=mods[:, k, msel + 1:msel + 2], scale=mods[:, k, msel:msel + 1]),
                     r=[skey, 'mods'], w=['hTf'])
            transpose_to(P, xs[b], 'xs%d' % b, 8, tp, idt, None, None, evac)
            for k in range(8):
                P.op('pe', lambda e, k=k: e.matmul(pC[:, 0:36], hTf[:, k, :], wr[:, k, :], start=(k == 0), stop=(k == 7)),
                     r=['hTf', 'wr'], w=['pY0'])
            P.op('dve', lambda e: e.tensor_tensor(lg[:], pC[:, 0:36], brB[:], ALU.add), r=['pY0', 'brB'], w=['lg'])
            gmax, ohg, negm, eg, sume, lsel, m1, mk1 = [sm[j] for j in range(8)]
            l2, m2, mk2, tt = sm[8], sm[9], sm[10], sm[11]
            lgG = lg[:, 0:4]
            lgE = lg[:, 4:36].rearrange("p (g e) -> p g e", g=4)

            def dv(fn, r, w):
                P.op('dve', fn, r=r, w=w)
            dv(lambda e: e.tensor_reduce(gmax[:, 0:1], lgG, AX.X, ALU.max), ['lg'], ['gmax'])
            dv(lambda e: e.tensor_scalar(ohg[:, 0:4], lgG, gmax[:, 0:1], None, ALU.is_equal), ['lg', 'gmax'], ['ohg'])
            dv(lambda e: e.tensor_scalar(negm[:, 0:1], gmax[:, 0:1], -1.0, None, ALU.mult), ['gmax'], ['negm'])
            P.op('act', lambda e: e.activation(eg[:, 0:4], lgG, AF.Exp, bias=negm[:, 0:1], scale=1.0), r=['lg', 'negm'], w=['eg'])
            dv(lambda e: e.tensor_reduce(sume[:, 0:1], eg[:, 0:4], AX.X, ALU.add), ['eg'], ['sume'])
            dv(lambda e: e.reciprocal(sume[:, 0:1], sume[:, 0:1]), ['sume'], ['sume'])
            dv(lambda e: e.tensor_tensor(selg[:], lgE, ohg[:, 0:4].unsqueeze(2).to_broadcast([128, 4, 8]), ALU.mult),
               ['lg', 'ohg'], ['selg'])
            dv(lambda e: e.tensor_reduce(lsel[:], selg[:].rearrange("p g e -> p e g"), AX.X, ALU.add), ['selg'], ['lsel'])
            dv(lambda e: e.tensor_reduce(m1[:, 0:1], lsel[:], AX.X, ALU.max), ['lsel'], ['m1'])
            dv(lambda e: e.tensor_scalar(mk1[:], lsel[:], m1[:, 0:1], None, ALU.is_equal), ['lsel', 'm1'], ['mk1'])
            dv(lambda e: e.scalar_tensor_tensor(l2[:], mk1[:], -1e30, lsel[:], ALU.mult, ALU.add), ['mk1', 'lsel'], ['l2'])
            dv(lambda e: e.tensor_reduce(m2[:, 0:1], l2[:], AX.X, ALU.max), ['l2'], ['m2'])
            dv(lambda e: e.tensor_scalar(mk2[:], l2[:], m2[:, 0:1], None, ALU.is_equal), ['l2', 'm2'], ['mk2'])
            dv(lambda e: e.tensor_tensor(tt[:, 0:1], m2[:, 0:1], m1[:, 0:1], ALU.subtract), ['m2', 'm1'], ['tt'])
            P.op('act', lambda e: e.activation(tt[:, 1:2], tt[:, 0:1], AF.Exp), r=['tt'], w=['tt'])
            dv(lambda e: e.tensor_scalar(tt[:, 2:3], tt[:, 1:2], 1.0, None, ALU.add), ['tt'], ['tt'])
            dv(lambda e: e.reciprocal(tt[:, 2:3], tt[:, 2:3]), ['tt'], ['tt'])
            dv(lambda e: e.tensor_tensor(tt[:, 3:4], tt[:, 2:3], sume[:, 0:1], ALU.mult), ['tt', 'sume'], ['tt'])
            dv(lambda e: e.tensor_tensor(tt[:, 4:5], tt[:, 3:4], tt[:, 1:2], ALU.mult), ['tt'], ['tt'])
            dv(lambda e: e.tensor_scalar(mk1[:], mk1[:], tt[:, 3:4], None, ALU.mult), ['mk1', 'tt'], ['mk1'])
            dv(lambda e: e.scalar_tensor_tensor(mk2[:], mk2[:], tt[:, 4:5], mk1[:], ALU.mult, ALU.add), ['mk2', 'tt', 'mk1'], ['mk2'])
            dv(lambda e, i=i: e.tensor_tensor(comb_all[:, i, :].rearrange("p (g e) -> p g e", g=4),
                                              ohg[:, 0:4].unsqueeze(2).to_broadcast([128, 4, 8]),
                                              mk2[:].unsqueeze(1).to_broadcast([128, 4, 8]), ALU.mult), ['ohg', 'mk2'], ['comb%d' % i])

        stepn = [0]
        for e_ in range(NEXP):
            s = e_ % 2
            if e_ + 1 < NEXP:
                load_expert(e_ + 1)
            for (t0, n) in GROUPS:
                hb_ = stepn[0] % 2
                stepn[0] += 1
                tiles = ['h2T%d' % t for t in range(t0 // 128, (t0 + n) // 128)]
                for fc in range(2):
                    ab = (2 * stepn[0] + fc) % 2
                    for k in range(8):
                        P.op('pe', lambda e, k=k, fc=fc, ab=ab, s=s, t0=t0, n=n: e.matmul(
                            pA[ab][:, 0:n], wg[s][:, k, fc * 128:(fc + 1) * 128], h2T[:, k, t0:t0 + n],
                            start=(k == 0), stop=(k == 7)), r=['wg%d' % s] + tiles, w=['pA%d' % ab])
                    for k in range(8):
                        P.op('pe', lambda e, k=k, fc=fc, ab=ab, s=s, t0=t0, n=n: e.matmul(
                            pU[ab][:, 0:n], wu[s][:, k, fc * 128:(fc + 1) * 128], h2T[:, k, t0:t0 + n],
                            start=(k == 0), stop=(k == 7)), r=['wu%d' % s] + tiles, w=['pU%d' % ab])
                    P.op('act', lambda e, ab=ab, n=n: e.activation(sa[ab][:, 0:n], pA[ab][:, 0:n], AF.Silu),
                         r=['pA%d' % ab], w=['sa%d' % ab])
                    P.op('dve', lambda e, ab=ab, n=n, fc=fc, hb_=hb_: e.tensor_tensor(hid[hb_][:, fc, 0:n], sa[ab][:, 0:n], pU[ab][:, 0:n], ALU.mult),
                         r=['sa%d' % ab, 'pU%d' % ab], w=['hid%d' % hb_])
                for tt_ in range(n // 128):
                    ti = t0 // 128 + tt_
                    for half in range(2):
                        yb = (2 * ti + half) % 2
                        for fc in range(2):
                            P.op('pe', lambda e, fc=fc, half=half, yb=yb, hb_=hb_, tt_=tt_, s=s: e.matmul(
                                pY[yb][:, :], hid[hb_][:, fc, tt_ * 128:(tt_ + 1) * 128], wd[s][:, fc, half * 512:(half + 1) * 512],
                                start=(fc == 0), stop=(fc == 1)), r=['hid%d' % hb_, 'wd%d' % s], w=['pY%d' % yb])
                        ysl = yacc[:, ti, half * 512:(half + 1) * 512]
                        cw = comb_all[:, ti, e_:e_ + 1]
                        if e_ == 0:
                            P.op('dve', lambda e, yb=yb, ysl=ysl, cw=cw: e.tensor_scalar(ysl, pY[yb][:, :], cw, None, ALU.mult),
                                 r=['pY%d' % yb, 'comb%d' % ti], w=['yacc%d' % ti])
                        else:
                            P.op('dve', lambda e, yb=yb, ysl=ysl, cw=cw: e.scalar_tensor_tensor(ysl, pY[yb][:, :], cw, ysl, ALU.mult, ALU.add),
                                 r=['pY%d' % yb, 'yacc%d' % ti, 'comb%d' % ti], w=['yacc%d' % ti])
        for i in range(NTILE):
            b = i % 2
            gs = 1 if i < 2 else 0
            rows = slice(i * 128, (i + 1) * 128)
            P.dma('sp', xs[b][:], xd[rows], w=['xs%d' % b])
            P.op('dve', lambda e, i=i, gs=gs: e.tensor_tensor(yacc[:, i, :], yacc[:, i, :], g2B[:, gs, :], ALU.mult),
                 r=['yacc%d' % i, 'g2B'], w=['yacc%d' % i])
            P.op('dve', lambda e, i=i, b=b: e.scalar_tensor_tensor(xs[b][:], xs[b][:], ALPHA, yacc[:, i, :], ALU.mult, ALU.add),
                 r=['xs%d' % b, 'yacc%d' % i], w=['xs%d' % b])
            ln_tile(P, xs[b], 'xs%d' % b, st, mv, rs, '', lng, lnb, ['lng'])
            P.dma('sp', xo[rows], xs[b][:], r=['xs%d' % b])
        P.finish()
    return nc


def prep_C2_weights(w_rg, b_rg, w_re, b_re, w_e_gate, w_e_up, w_e_down, ln2_g, ln2_b, g2_lat, g2_ctx):
    wr = np.concatenate([w_rg, w_re], axis=1)
    br = np.concatenate([b_rg, b_re], axis=0)
    return {"wr": kp(wr, 8), "brB": bcast(br),
            "wg": np.ascontiguousarray(w_e_gate.reshape(NEXP, 8, 128, DEXP).transpose(0, 2, 1, 3)),
            "wu": np.ascontiguousarray(w_e_up.reshape(NEXP, 8, 128, DEXP).transpose(0, 2, 1, 3)),
            "wd": np.ascontiguousarray(w_e_down.reshape(NEXP, 2, 128, D).transpose(0, 2, 1, 3)),
            "lngB": bcast(ln2_g), "lnbB": bcast(ln2_b),
            "g2B": np.ascontiguousarray(np.stack([bcast(g2_lat), bcast(g2_ctx)], axis=1)),
            "ident": np.eye(128, dtype=np.float32)}


_PROGS = {}


def _prog(name):
    if name not in _PROGS:
        _PROGS[name] = {"M": build_M, "A": build_A, "B1": build_B1, "B2": build_B2, "C1": build_C1, "C2": build_C2}[name]()
    return _PROGS[name]


def _run(name, in_maps):
    res = run_bass_kernel_spmd(_prog(name), in_maps, core_ids=list(range(NCORE)))
    return res.results


def _gather_tok(outs, key, axis):
    parts = [np.take(outs[0][key], np.arange(CTX), axis=axis)]
    for i in range(NCORE):
        parts.append(np.take(outs[i][key], np.arange(CTX, TC), axis=axis))
    return np.concatenate(parts, axis=axis)


def kernel(x, c, ctx, c_ctx, w_ada, b_ada, w_in, b_gates, w_uq, w_uk, w_uv, g_qn, g_kvn, g_mh,
           w_bo_mla, w_bo_mlstm, w_out, ln1_g, ln1_b, w_rg, b_rg, w_re, b_re,
           w_e_gate, w_e_up, w_e_down, ln2_g, ln2_b):
    f32 = np.float32
    x = np.asarray(x, f32)
    ctx = np.asarray(ctx, f32)
    ident = np.eye(128, dtype=f32)
    ones128 = np.ones((128, 128), f32)
    cc = np.ascontiguousarray(np.stack([np.asarray(c, f32)[0].reshape(8, 128).T, np.asarray(c_ctx, f32).reshape(8, 128).T], axis=-1))
    wall = np.concatenate([np.asarray(w_ada[l], f32) for l in range(DEPTH)], axis=1)
    ball = np.concatenate([np.asarray(b_ada[l], f32) for l in range(DEPTH)], axis=0)
    ins = []
    for i in range(NCORE):
        ins.append({"cc": cc, "wa": kp(wall[:, i * 3072:(i + 1) * 3072], 8),
                    "ba": np.ascontiguousarray(ball[i * 3072:(i + 1) * 3072].reshape(24, 128).T)})
    outs = _run("M", ins)
    mod = np.concatenate([o["mo"].transpose(1, 0, 2).reshape(3072, 2) for o in outs], axis=0).reshape(DEPTH, 6 * D, 2)
    del wall, ins

    cosT, ssinT = rope_tables_np()
    toks = [core_tokens(i) for i in range(NCORE)]
    cos4 = [np.ascontiguousarray(np.tile(cosT[t].T, (4, 1))) for t in toks]
    sin4 = [np.ascontiguousarray(np.tile(ssinT[t].T, (4, 1))) for t in toks]
    xc = [np.ascontiguousarray(np.concatenate([ctx[0], x[0, i * LAT_C:(i + 1) * LAT_C]], axis=0)) for i in range(NCORE)]
    perm_f = np.arange(T)
    perm_b = np.concatenate([np.arange(CTX - 1, -1, -1), np.arange(T - 1, CTX - 1, -1)])
    tri = np.triu(np.ones((64, 64), f32))
    ones64 = np.ones((64, 64), f32)
    onescol = np.ones((T, 1), f32)

    for l in range(DEPTH):
        m = mod[l]
        sh1, sc1, g1, sh2, sc2, g2 = [m[j * D:(j + 1) * D] for j in range(6)]
        mod1 = kp(np.stack([sc1[:, 0], sh1[:, 0], sc1[:, 1], sh1[:, 1]], axis=-1), 8)
        mod2 = kp(np.stack([sc2[:, 0], sh2[:, 0], sc2[:, 1], sh2[:, 1]], axis=-1), 8)
        wA = prep_A_weights(np.asarray(w_in[l], f32), np.asarray(w_uq[l], f32), np.asarray(w_uk[l], f32),
                            np.asarray(w_uv[l], f32), np.asarray(g_qn[l], f32), np.asarray(g_kvn[l], f32),
                            np.asarray(b_gates[l], f32))
        ins = []
        for i in range(NCORE):
            d_ = {"x": xc[i], "mod1": mod1, "ident": ident, "ones": ones128, "cos4": cos4[i], "ssin4": sin4[i]}
            d_.update(wA)
            ins.append(d_)
        oA = _run("A", ins)
        del ins, wA
        QT = _gather_tok(oA, "QT", 2).reshape(768, T)
        KnT = _gather_tok(oA, "KnT", 2).reshape(512, T)
        KrT = _gather_tok(oA, "KrT", 1)
        Vall = _gather_tok(oA, "V", 0)
        mqT = _gather_tok(oA, "mqT", 2).reshape(256, T)
        mkT = _gather_tok(oA, "mkT", 2).reshape(256, T)
        mkv = _gather_tok(oA, "mkv", 0)
        graw = _gather_tok(oA, "graw", 1)
        glsg = _gather_tok(oA, "glsg", 1)
        ins = []
        for h in range(8):
            Q = np.concatenate([QT[h * 64:(h + 1) * 64], QT[512 + h * 32:512 + (h + 1) * 32]], axis=0)
            Kk = np.concatenate([KnT[h * 64:(h + 1) * 64], KrT], axis=0)
            Vx = np.concatenate([Vall[:, h * 64:(h + 1) * 64], onescol], axis=1).reshape(NKT, 128, 65).transpose(1, 0, 2)
            ins.append({"Q": np.ascontiguousarray(Q), "K": np.ascontiguousarray(Kk), "V": np.ascontiguousarray(Vx)})
        oB1 = _run("B1", ins)
        ins = []
        for cidx in range(8):
            hd, dr = cidx // 2, cidx % 2
            perm = perm_b if dr else perm_f
            q_ = mqT[hd * 64:(hd + 1) * 64][:, perm]
            k_ = mkT[hd * 64:(hd + 1) * 64][:, perm]
            kt_ = mkv[perm, hd * 64:(hd + 1) * 64].reshape(NCH, 64, 64).transpose(1, 0, 2)
            vx_ = np.concatenate([mkv[perm, 256 + hd * 128:256 + (hd + 1) * 128], onescol], axis=1).reshape(NCH, 64, 129).transpose(1, 0, 2)
            ig_ = graw[(2 * dr) * 4 + hd][perm].reshape(NCH, 64).T
            lf_ = glsg[(2 * dr + 1) * 4 + hd][perm].reshape(NCH, 64).T
            ins.append({"qT": np.ascontiguousarray(q_), "kT": np.ascontiguousarray(k_), "kt": np.ascontiguousarray(kt_),
                        "vx": np.ascontiguousarray(vx_), "ig": np.ascontiguousarray(ig_), "lf": np.ascontiguousarray(lf_),
                        "tri": tri, "ones": ones64})
        oB2 = _run("B2", ins)
        del ins
        hf_all = np.empty((T, 512), f32)
        hb_all = np.empty((T, 512), f32)
        for cidx in range(8):
            hd, dr = cidx // 2, cidx % 2
            hp = oB2[cidx]["H"].transpose(1, 0, 2).reshape(T, 128)
            if dr:
                hb_all[perm_b, hd * 128:(hd + 1) * 128] = hp
            else:
                hf_all[:, hd * 128:(hd + 1) * 128] = hp
        o_all = np.stack([oB1[h]["O"] for h in range(8)], axis=1)
        wC1 = prep_C1_weights(np.asarray(w_bo_mla[l], f32), np.asarray(w_bo_mlstm[l], f32), np.asarray(w_out[l], f32),
                              np.asarray(g_mh[l], f32), np.asarray(ln1_g[l], f32), np.asarray(ln1_b[l], f32), g1[:, 0], g1[:, 1])
        ins = []
        for i in range(NCORE):
            d_ = {"x": xc[i], "o": np.ascontiguousarray(o_all[toks[i]]), "hf": np.ascontiguousarray(hf_all[toks[i]]),
                  "hb": np.ascontiguousarray(hb_all[toks[i]]), "spo": oA[i]["spo"], "spm": oA[i]["spm"]}
            d_.update(wC1)
            ins.append(d_)
        oC1 = _run("C1", ins)
        del ins, oA, o_all, hf_all, hb_all
        wC2 = prep_C2_weights(np.asarray(w_rg[l], f32), np.asarray(b_rg[l], f32), np.asarray(w_re[l], f32), np.asarray(b_re[l], f32),
                              np.asarray(w_e_gate[l], f32), np.asarray(w_e_up[l], f32), np.asarray(w_e_down[l], f32),
                              np.asarray(ln2_g[l], f32), np.asarray(ln2_b[l], f32), g2[:, 0], g2[:, 1])
        wC2["mod2"] = mod2
        ins = []
        for i in range(NCORE):
            d_ = {"x": oC1[i]["xo"]}
            d_.update(wC2)
            ins.append(d_)
        oC2 = _run("C2", ins)
        del ins, wC2
        xc = [np.ascontiguousarray(oC2[i]["xo"]) for i in range(NCORE)]
    out = np.concatenate([xc[i][CTX:] for i in range(NCORE)], axis=0)[None]
    return np.ascontiguousarray(out.astype(np.float32))
```

```python
import numpy as np
from contextlib import ExitStack
import concourse.bass as bass
import concourse.mybir as mybir
from concourse.bass_utils import run_bass_kernel_spmd

F32 = mybir.dt.float32
BF16 = mybir.dt.bfloat16
AF = mybir.ActivationFunctionType
ALU = mybir.AluOpType
AX = mybir.AxisListType

EPOCH = 16000
NDS = 12
ENGS = ('pe', 'act', 'dve', 'pool', 'sp')


class Prog:
    def __init__(self, nc, es):
        self.nc = nc
        self.es = es
        self.ops = {e: [] for e in ENGS}
        self.cnt = {e: 0 for e in ENGS}
        self.known = {e: {} for e in ENGS}
        self.lastw = {}
        self.readers = {}
        self.ndma = 0
        self.esems = {e: [] for e in ENGS}
        self.dsems = [es.enter_context(nc.semaphore("dsem%d" % j)) for j in range(NDS)]
        self.nt = 0

    def sb(self, shape, dt, name=None):
        self.nt += 1
        return self.es.enter_context(self.nc.sbuf_tensor(name or ("sb%d" % self.nt), list(shape), dt))

    def ps(self, shape, dt, name=None):
        self.nt += 1
        return self.es.enter_context(self.nc.psum_tensor(name or ("ps%d" % self.nt), list(shape), dt))

    def _esem(self, eng, idx):
        while len(self.esems[eng]) <= idx:
            self.esems[eng].append(self.es.enter_context(
                self.nc.semaphore("s_%s_%d" % (eng, len(self.esems[eng])))))
        return self.esems[eng][idx]

    def _deps(self, eng, r, w, extra=()):
        need = {}

        def add(p, v):
            if p is None:
                return
            if need.get(p, 0) < v:
                need[p] = v
        for k in r:
            lw = self.lastw.get(k)
            if lw:
                add(*lw)
        for k in w:
            lw = self.lastw.get(k)
            if lw:
                add(*lw)
            for p, v in self.readers.get(k, {}).items():
                add(p, v)
        for p, v in extra:
            add(p, v)
        waits = []
        kn = self.known[eng]
        for p, v in need.items():
            if p == eng and eng in ('pe', 'sp'):
                continue
            if kn.get(p, 0) >= v:
                continue
            kn[p] = v
            waits.append((p, v))
        return waits

    def op(self, eng, fn, r=(), w=()):
        waits = self._deps(eng, r, w)
        self.cnt[eng] += 1
        c = self.cnt[eng]
        self.ops[eng].append(('op', fn, waits, c))
        for k in r:
            d = self.readers.setdefault(k, {})
            d[eng] = c
        for k in w:
            self.lastw[k] = (eng, c)
            self.readers[k] = {}
        return c

    def dma(self, q, out, in_, r=(), w=(), **kw):
        j = self.ndma % NDS
        n = self.ndma // NDS + 1
        self.ndma += 1
        prod = ('d', j)
        waits = self._deps(q, r, w, extra=([(prod, n - 1)] if n > 1 else []))
        self.ops[q].append(('dma', (out, in_, kw), waits, (j, n)))
        for k in r:
            d = self.readers.setdefault(k, {})
            d[prod] = n
        for k in w:
            self.lastw[k] = (prod, n)
            self.readers[k] = {}

    def _emit_wait(self, e, p, v):
        if isinstance(p, tuple):
            e.wait_ge(self.dsems[p[1]], 16 * v)
        else:
            idx = (v - 1) // EPOCH
            e.wait_ge(self._esem(p, idx), (v - 1) % EPOCH + 1)

    def _run(self, name, e):
        for kind, payload, waits, c in self.ops[name]:
            for p, v in waits:
                self._emit_wait(e, p, v)
            if kind == 'op':
                ins = payload(e)
                idx = (c - 1) // EPOCH
                ins.then_inc(self._esem(name, idx), 1)
            else:
                out, in_, kw = payload
                j, n = c
                e.dma_start(out=out, in_=in_, **kw).then_inc(self.dsems[j], 16)
        if name == 'sp':
            tot = {}
            for j in range(NDS):
                n = (self.ndma - j + NDS - 1) // NDS if self.ndma > j else 0
                if n > 0:
                    e.wait_ge(self.dsems[j], 16 * n)

    def finish(self):
        nc = self.nc
        for e in ENGS:
            if self.cnt[e] > 0:
                self._esem(e, (self.cnt[e] - 1) // EPOCH)
        with nc.Block() as block:
            @block.sync
            def _(sync):
                self._run('sp', sync)

            @block.tensor
            def _(tensor):
                self._run('pe', tensor)

            @block.scalar
            def _(scalar):
                self._run('act', scalar)

            @block.vector
            def _(vector):
                self._run('dve', vector)

            @block.gpsimd
            def _(gpsimd):
                self._run('pool', gpsimd)


D = 1024
SEQ = 16384
CTX = 256
T = SEQ + CTX
NCORE = 8
LAT_C = SEQ // NCORE
TC = CTX + LAT_C
NTILE = TC // 128
GROUPS = [(0, 256), (256, 512), (768, 512), (1280, 512), (1792, 512)]
DEPTH = 4
EPS = 1e-6
ALPHA = (2 * DEPTH) ** 0.25
MLA_SCALE = 96 ** -0.5
NCH = T // 64


def dram_in(nc, name, shape, dt=F32):
    return nc.dram_tensor(name, list(shape), dt, kind="ExternalInput").ap()


def dram_out(nc, name, shape, dt=F32):
    return nc.dram_tensor(name, list(shape), dt, kind="ExternalOutput").ap()


def build_M():
    nc = bass.Bass("TRN2", target_bir_lowering=False)
    cc = dram_in(nc, "cc", [128, 8, 2])
    wa = dram_in(nc, "wa", [128, 8, 3072])
    ba = dram_in(nc, "ba", [128, 24])
    mo = dram_out(nc, "mo", [128, 24, 2])
    with ExitStack() as es:
        P = Prog(nc, es)
        ccs = P.sb([128, 8, 2], F32)
        was = P.sb([128, 8, 3072], F32)
        bas = P.sb([128, 24], F32)
        mos = P.sb([128, 24, 2], F32)
        pm = P.ps([128, 24, 2], F32)
        P.dma('sp', ccs[:], cc, w=['cc'])
        P.dma('sp', bas[:], ba, w=['ba'])
        for k in range(8):
            P.dma('sp', was[:, k, :], wa[:, k, :], w=['wa%d' % k])
        P.op('act', lambda e: e.activation(ccs[:], ccs[:], AF.Silu), r=['cc'], w=['cc'])
        for j in range(24):
            for k in range(8):
                P.op('pe', lambda e, j=j, k=k: e.matmul(pm[:, j, :], was[:, k, j * 128:(j + 1) * 128], ccs[:, k, :],
                                                       start=(k == 0), stop=(k == 7)),
                     r=['cc', 'wa%d' % k], w=['pm'])
        P.op('dve', lambda e: e.tensor_tensor(mos[:], pm[:], bas[:].unsqueeze(2).to_broadcast([128, 24, 2]), ALU.add),
             r=['pm', 'ba'], w=['mo'])
        P.dma('sp', mo, mos[:], r=['mo'])
        P.finish()
    return nc


A_QD, A_KVD, A_KR, A_KRP, A_MQ, A_MK, A_PG = 0, 384, 640, 672, 704, 960, 1216
A_TM = 1232
A_NCOL = A_TM + 256 + 512 + 512 + 2048


class Common:
    def __init__(self, P, nc):
        self.P = P
        self.nc = nc

    def load(self, q, dst, src, key, maxcols=None):
        self.P.dma(q, dst, src, w=[key])


def ln_rstd(P, var_ap, out_ap, rkeys, wkey, scale=1.0):
    P.op('act', lambda e: e.activation(out_ap, var_ap, AF.Ln, bias=EPS, scale=scale), r=rkeys, w=[wkey])
    P.op('act', lambda e: e.activation(out_ap, out_ap, AF.Exp, scale=-0.5), r=[wkey], w=[wkey])


def build_A():
    nc = bass.Bass("TRN2", target_bir_lowering=False)
    x = dram_in(nc, "x", [TC, D])
    mod1 = dram_in(nc, "mod1", [128, 8, 4])
    ident = dram_in(nc, "ident", [128, 128])
    ones_d = dram_in(nc, "ones", [128, 128])
    w_in = dram_in(nc, "w_in", [128, 8, A_NCOL])
    w_uq = dram_in(nc, "w_uq", [128, 3, 1024])
    w_uk = dram_in(nc, "w_uk", [128, 2, 512])
    w_uv = dram_in(nc, "w_uv", [128, 2, 512])
    gq_d = dram_in(nc, "gq", [128, 3])
    gkv_d = dram_in(nc, "gkv", [128, 2])
    bg_d = dram_in(nc, "bg", [16, 1])
    cos_d = dram_in(nc, "cos4", [128, TC])
    sin_d = dram_in(nc, "ssin4", [128, TC])
    QT = dram_out(nc, "QT", [6, 128, TC])
    KnT = dram_out(nc, "KnT", [4, 128, TC])
    KrT = dram_out(nc, "KrT", [32, TC])
    V = dram_out(nc, "V", [TC, 512])
    mqT = dram_out(nc, "mqT", [2, 128, TC])
    mkT = dram_out(nc, "mkT", [2, 128, TC])
    mkv = dram_out(nc, "mkv", [TC, 768])
    graw = dram_out(nc, "graw", [16, TC])
    glsg = dram_out(nc, "glsg", [16, TC])
    spo = dram_out(nc, "spo", [TC, 512])
    spm = dram_out(nc, "spm", [TC, 2048])
    with ExitStack() as es:
        P = Prog(nc, es)
        idt = P.sb([128, 128], F32)
        ones = P.sb([128, 128], F32)
        mods = P.sb([128, 8, 4], F32)
        win = P.sb([128, 8, A_NCOL], BF16)
        wuq = P.sb([128, 3, 1024], BF16)
        wuk = P.sb([128, 2, 512], BF16)
        wuv = P.sb([128, 2, 512], BF16)
        gq = P.sb([128, 3], F32)
        gkv = P.sb([128, 2], F32)
        bg = P.sb([16, 1], F32)
        cos4 = P.sb([128, TC], F32)
        sin4 = P.sb([128, TC], F32)
        hT = P.sb([128, 8, TC], BF16)
        xs = [P.sb([128, D], F32) for _ in range(2)]
        xn = [P.sb([128, D], F32) for _ in range(2)]
        st = [P.sb([128, 12], F32) for _ in range(2)]
        mv = [P.sb([128, 2], F32) for _ in range(2)]
        rs = [P.sb([128, 1], F32) for _ in range(2)]
        tp = P.ps([128, 8, 128], F32)
        pf = [P.ps([128, 512], F32) for _ in range(2)]
        pt = [P.ps([128, 512], F32) for _ in range(2)]
        pst = P.ps([128, 512], F32)
        dn = P.sb([128, 3, 512], F32)
        sq = P.sb([128, 3, 512], F32)
        rq = P.sb([128, 512], F32)
        dnn = P.sb([128, 3, 512], BF16)
        stg = [P.sb([128, 512], F32) for _ in range(3)]
        t1 = P.sb([128, 512], F32)
        t2 = P.sb([128, 512], F32)
        g1 = P.sb([16, 512], F32)
        g2 = P.sb([16, 512], F32)
        g3 = P.sb([16, 512], F32)

        P.dma('sp', idt[:], ident, w=['idt'])
        P.dma('sp', ones[:], ones_d, w=['ones'])
        P.dma('sp', mods[:], mod1, w=['mods'])
        P.dma('sp', gq[:], gq_d, w=['gq'])
        P.dma('sp', gkv[:], gkv_d, w=['gkv'])
        P.dma('sp', bg[:], bg_d, w=['bg'])
        P.dma('sp', cos4[:], cos_d, w=['cos'])
        P.dma('sp', sin4[:], sin_d, w=['sin'])
        for k in range(8):
            for c0 in range(0, A_NCOL, 1520):
                P.dma('pool', win[:, k, c0:c0 + 1520], w_in[:, k, c0:c0 + 1520], w=['win'])
        for j in range(3):
            P.dma('pool', wuq[:, j, :], w_uq[:, j, :], w=['wuq'])
        for j in range(2):
            P.dma('pool', wuk[:, j, :], w_uk[:, j, :], w=['wuk'])
            P.dma('pool', wuv[:, j, :], w_uv[:, j, :], w=['wuv'])
        P.op('dve', lambda e: e.tensor_scalar(mods[:, :, 0:1], mods[:, :, 0:1], 1.0, None, ALU.add), r=['mods'], w=['mods'])
        P.op('dve', lambda e: e.tensor_scalar(mods[:, :, 2:3], mods[:, :, 2:3], 1.0, None, ALU.add), r=['mods'], w=['mods'])

        for i in range(NTILE):
            b = i % 2
            sel = 2 if i < 2 else 0
            P.dma('sp', xs[b][:], x[i * 128:(i + 1) * 128, :], w=['xs%d' % b])
            for hh in range(2):
                P.op('dve', lambda e, b=b, hh=hh: e.bn_stats(st[b][:, hh * 6:(hh + 1) * 6], xs[b][:, hh * 512:(hh + 1) * 512]),
                     r=['xs%d' % b], w=['st%d' % b])
            P.op('dve', lambda e, b=b: e.bn_aggr(mv[b][:], st[b][:]), r=['st%d' % b], w=['mv%d' % b])
            ln_rstd(P, mv[b][:, 1:2], rs[b][:], ['mv%d' % b], 'rs%d' % b)
            P.op('dve', lambda e, b=b: e.tensor_scalar(xn[b][:], xs[b][:], mv[b][:, 0:1], rs[b][:, 0:1], ALU.subtract, ALU.mult),
                 r=['xs%d' % b, 'mv%d' % b, 'rs%d' % b], w=['xn%d' % b])
            for k in range(8):
                P.op('pe', lambda e, b=b, k=k: e.transpose(tp[:, k, :], xn[b][:, k * 128:(k + 1) * 128], idt[:]),
                     r=['xn%d' % b, 'idt'], w=['tp%d' % (k // 4)])
            for k in range(8):
                P.op('act', lambda e, i=i, k=k, sel=sel: e.activation(
                    hT[:, k, i * 128:(i + 1) * 128], tp[:, k, :], AF.Identity,
                    bias=mods[:, k, sel + 1:sel + 2], scale=mods[:, k, sel:sel + 1]),
                    r=['tp%d' % (k // 4), 'mods'], w=['hT%d' % i])

        fmi = [0]

        def fm_mm(col0, m, s, n):
            bi = fmi[0] % 2
            fmi[0] += 1
            ps = pf[bi]
            tiles = ['hT%d' % t for t in range(s // 128, (s + n) // 128)]
            for k in range(8):
                P.op('pe', lambda e, k=k, ps=ps: e.matmul(ps[0:m, 0:n], win[:, k, col0:col0 + m], hT[:, k, s:s + n],
                                                          start=(k == 0), stop=(k == 7)),
                     r=['win'] + tiles, w=['pf%d' % bi])
            return ps, 'pf%d' % bi

        sgi = [0]

        def stage_out(ps, pkey, m, n, dst, func=AF.Copy, scale=1.0, bias=None):
            si = sgi[0] % 3
            sgi[0] += 1
            sg = stg[si]
            if bias is None:
                P.op('act', lambda e: e.activation(sg[0:m, 0:n], ps[0:m, 0:n], func, scale=scale),
                     r=[pkey], w=['stg%d' % si])
            else:
                P.op('act', lambda e: e.activation(sg[0:m, 0:n], ps[0:m, 0:n], func, bias=bias, scale=scale),
                     r=[pkey, 'bg'], w=['stg%d' % si])
            P.dma('sp', dst, sg[0:m, 0:n], r=['stg%d' % si])

        def rms_block(col0, nch, gvec, gkey, dim, s, n):
            for j in range(nch):
                ps, pk = fm_mm(col0 + j * 128, 128, s, n)
                P.op('act', lambda e, j=j, ps=ps: e.activation(dn[:, j, 0:n], ps[:, 0:n], AF.Copy), r=[pk], w=['dn%d' % j])
                P.op('act', lambda e, j=j, ps=ps: e.activation(sq[:, j, 0:n], ps[:, 0:n], AF.Square), r=[pk], w=['sq%d' % j])
            for j in range(nch):
                P.op('pe', lambda e, j=j: e.matmul(pst[:, 0:n], ones[:], sq[:, j, 0:n], start=(j == 0), stop=(j == nch - 1)),
                     r=['ones', 'sq%d' % j], w=['pst'])
            ln_rstd(P, pst[:, 0:n], rq[:, 0:n], ['pst'], 'rq', scale=1.0 / dim)
            for j in range(nch):
                P.op('dve', lambda e, j=j: e.scalar_tensor_tensor(dnn[:, j, 0:n], dn[:, j, 0:n], gvec[:, j:j + 1], rq[:, 0:n],
                                                                 ALU.mult, ALU.mult),
                     r=['dn%d' % j, gkey, 'rq'], w=['dnn%d' % j])

        def up_mm(wt, wkey, nch, col0, m, n, tok0=None, tm=False):
            bi = fmi[0] % 2
            fmi[0] += 1
            ps = pf[bi]
            for j in range(nch):
                if not tm:
                    P.op('pe', lambda e, j=j, ps=ps: e.matmul(ps[0:m, 0:n], wt[:, j, col0:col0 + m], dnn[:, j, 0:n],
                                                              start=(j == 0), stop=(j == nch - 1)),
                         r=[wkey, 'dnn%d' % j], w=['pf%d' % bi])
                else:
                    P.op('pe', lambda e, j=j, ps=ps: e.matmul(ps[:, 0:m], dnn[:, j, tok0:tok0 + 128], wt[:, j, col0:col0 + m],
                                                              start=(j == 0), stop=(j == nch - 1)),
                         r=[wkey, 'dnn%d' % j], w=['pf%d' % bi])
            return ps, 'pf%d' % bi

        def rope_out(psA, kA, psP, kP, m, s, n, dst):
            P.op('dve', lambda e: e.tensor_tensor(t1[0:m, 0:n], psA[0:m, 0:n], cos4[0:m, s:s + n], ALU.mult),
                 r=[kA, 'cos'], w=['t1'])
            P.op('dve', lambda e: e.tensor_tensor(t2[0:m, 0:n], psP[0:m, 0:n], sin4[0:m, s:s + n], ALU.mult),
                 r=[kP, 'sin'], w=['t2'])
            P.op('dve', lambda e: e.tensor_tensor(t1[0:m, 0:n], t1[0:m, 0:n], t2[0:m, 0:n], ALU.add),
                 r=['t1', 't2'], w=['t1'])
            P.dma('sp', dst, t1[0:m, 0:n], r=['t1'])

        for (s, n) in GROUPS:
            rms_block(A_QD, 3, gq, 'gq', 384, s, n)
            for oc in range(4):
                ps, pk = up_mm(wuq, 'wuq', 3, oc * 128, 128, n)
                stage_out(ps, pk, 128, n, QT[oc, :, s:s + n])
            for c in range(2):
                psA, kA = up_mm(wuq, 'wuq', 3, 512 + c * 128, 128, n)
                psP, kP = up_mm(wuq, 'wuq', 3, 768 + c * 128, 128, n)
                rope_out(psA, kA, psP, kP, 128, s, n, QT[4 + c, :, s:s + n])
            rms_block(A_KVD, 2, gkv, 'gkv', 256, s, n)
            for oc in range(4):
                ps, pk = up_mm(wuk, 'wuk', 2, oc * 128, 128, n)
                stage_out(ps, pk, 128, n, KnT[oc, :, s:s + n])
            for tt in range(n // 128):
                ps, pk = up_mm(wuv, 'wuv', 2, 0, 512, n, tok0=tt * 128, tm=True)
                stage_out(ps, pk, 128, 512, V[s + tt * 128:s + (tt + 1) * 128, :])
            psA, kA = fm_mm(A_KR, 32, s, n)
            psP, kP = fm_mm(A_KRP, 32, s, n)
            rope_out(psA, kA, psP, kP, 32, s, n, KrT[:, s:s + n])
            for c in range(2):
                ps, pk = fm_mm(A_MQ + c * 128, 128, s, n)
                stage_out(ps, pk, 128, n, mqT[c, :, s:s + n], scale=0.125)
            for c in range(2):
                ps, pk = fm_mm(A_MK + c * 128, 128, s, n)
                stage_out(ps, pk, 128, n, mkT[c, :, s:s + n])
            ps, pk = fm_mm(A_PG, 16, s, n)
            P.op('act', lambda e, ps=ps: e.activation(g1[:, 0:n], ps[0:16, 0:n], AF.Identity, bias=bg[:, 0:1], scale=1.0),
                 r=[pk, 'bg'], w=['g1'])
            P.dma('sp', graw[:, s:s + n], g1[:, 0:n], r=['g1'])
            P.op('act', lambda e: e.activation(g2[:, 0:n], g1[:, 0:n], AF.Exp, scale=-1.0), r=['g1'], w=['g2'])
            P.op('act', lambda e: e.activation(g3[:, 0:n], g2[:, 0:n], AF.Ln, bias=1.0, scale=1.0), r=['g2'], w=['g3'])
            P.op('act', lambda e: e.activation(g3[:, 0:n], g3[:, 0:n], AF.Copy, scale=-1.0), r=['g3'], w=['g3'])
            P.dma('sp', glsg[:, s:s + n], g3[:, 0:n], r=['g3'])

        tmi = [0]
        tm_groups = [(A_TM, 256, mkv, 0, AF.Copy), (A_TM + 256, 512, mkv, 256, AF.Copy),
                     (A_TM + 768, 512, spo, 0, AF.Sigmoid)] + \
                    [(A_TM + 1280 + q * 512, 512, spm, q * 512, AF.Sigmoid) for q in range(4)]
        for i in range(NTILE):
            for (c0, ncl, dst, dc0, func) in tm_groups:
                bi = tmi[0] % 2
                tmi[0] += 1
                ps = pt[bi]
                for k in range(8):
                    P.op('pe', lambda e, k=k, ps=ps, c0=c0, ncl=ncl, i=i: e.matmul(
                        ps[:, 0:ncl], hT[:, k, i * 128:(i + 1) * 128], win[:, k, c0:c0 + ncl],
                        start=(k == 0), stop=(k == 7)), r=['win', 'hT%d' % i], w=['pt%d' % bi])
                stage_out(ps, 'pt%d' % bi, 128, ncl, dst[i * 128:(i + 1) * 128, dc0:dc0 + ncl], func=func)
        P.finish()
    return nc


ROPE_PERM = np.concatenate([np.arange(8, 16), np.arange(0, 8), np.arange(24, 32), np.arange(16, 24)])
ROPE_SIGN = np.concatenate([-np.ones(8), np.ones(8), -np.ones(8), np.ones(8)]).astype(np.float32)


def rope_tables_np():
    rows = SEQ // 64
    row, col = np.meshgrid(np.arange(rows, dtype=np.float32), np.arange(64, dtype=np.float32), indexing='ij')
    row, col = row.reshape(-1), col.reshape(-1)
    half = 16
    inv = (np.float32(10000.0) ** (-np.arange(0, half, 2, dtype=np.float32) / np.float32(half))).astype(np.float32)
    ar, ac = row[:, None] * inv, col[:, None] * inv
    ang = np.concatenate([ar, ar, ac, ac], axis=-1)
    ang = np.concatenate([np.zeros((CTX, 32), np.float32), ang], axis=0).astype(np.float32)
    return np.cos(ang).astype(np.float32), (np.sin(ang) * ROPE_SIGN).astype(np.float32)


def core_tokens(i):
    return np.concatenate([np.arange(CTX), CTX + np.arange(i * LAT_C, (i + 1) * LAT_C)])


def kp(a, nk):
    return np.ascontiguousarray(a.reshape(nk, 128, -1).transpose(1, 0, 2))


def prep_A_weights(w_in, w_uq, w_uk, w_uv, g_qn, g_kvn, b_gates):
    kr = 640 + ROPE_PERM
    colsA = np.concatenate([np.arange(0, 384), np.arange(384, 640), np.arange(640, 672), kr,
                            np.arange(672, 928), np.arange(928, 1184), np.arange(2208, 2224),
                            np.arange(928, 1184), np.arange(1184, 1696), np.arange(1696, 2208),
                            np.arange(2224, 4272)])
    assert len(colsA) == A_NCOL
    hq = np.arange(8)[:, None] * 96
    nope = (hq + np.arange(64)[None, :]).reshape(-1)
    rope = (hq + 64 + np.arange(32)[None, :]).reshape(-1)
    ropep = (hq + 64 + ROPE_PERM[None, :]).reshape(-1)
    colsq = np.concatenate([nope, rope, ropep])
    return {
        "w_in": kp(w_in[:, colsA], 8),
        "w_uq": kp(w_uq[:, colsq], 3),
        "w_uk": kp(w_uk, 2),
        "w_uv": kp(w_uv, 2),
        "gq": np.ascontiguousarray(g_qn.reshape(3, 128).T),
        "gkv": np.ascontiguousarray(g_kvn.reshape(2, 128).T),
        "bg": np.ascontiguousarray(b_gates.reshape(16, 1)),
    }


NKT = T // 128
QGROUPS = [(0, 256, 2)] + [(CTX + g * 512, 512, NKT) for g in range(SEQ // 512)]


def build_B1():
    nc = bass.Bass("TRN2", target_bir_lowering=False)
    Qd = dram_in(nc, "Q", [96, T])
    Kd = dram_in(nc, "K", [96, T])
    Vd = dram_in(nc, "V", [128, NKT, 65])
    Od = dram_out(nc, "O", [T, 65])
    with ExitStack() as es:
        P = Prog(nc, es)
        Qs = P.sb([96, T], BF16)
        Ks = P.sb([96, T], BF16)
        Vs = P.sb([128, NKT, 65], BF16)
        pTs = [P.sb([128, 512], BF16) for _ in range(3)]
        ost = [P.sb([128, 4, 65], F32) for _ in range(2)]
        pss = [P.ps([128, 512], F32) for _ in range(3)]
        pso = [P.ps([128, 512], F32) for _ in range(4)]
        CW = 1280
        for c0 in range(0, T, CW):
            P.dma('pool', Ks[:, c0:c0 + CW], Kd[:, c0:c0 + CW], w=['K%d' % (c0 // CW)])
            P.dma('pool', Qs[:, c0:c0 + CW], Qd[:, c0:c0 + CW], w=['Q%d' % (c0 // CW)])
        for t0 in range(0, NKT, 26):
            P.dma('pool', Vs[:, t0:t0 + 26, :], Vd[:, t0:t0 + 26, :], w=['V%d' % (t0 // 26)])
        steps = [(gi, kt) for gi, (q0, nq, nkt) in enumerate(QGROUPS) for kt in range(nkt)]

        def mm1(si):
            gi, kt = steps[si]
            q0, nq, nkt = QGROUPS[gi]
            sb_ = si % 3
            qkeys = ['Q%d' % j for j in range(q0 // CW, (q0 + nq - 1) // CW + 1)]
            P.op('pe', lambda e: e.matmul(pss[sb_][:, 0:nq], Ks[:, kt * 128:(kt + 1) * 128], Qs[:, q0:q0 + nq],
                                          start=True, stop=True),
                 r=['K%d' % ((kt * 128) // CW)] + qkeys, w=['pss%d' % sb_])

        LA = 2
        for si in range(min(LA, len(steps))):
            mm1(si)
        for si, (gi, kt) in enumerate(steps):
            q0, nq, nkt = QGROUPS[gi]
            sb_ = si % 3
            if si + LA < len(steps):
                mm1(si + LA)
            P.op('act', lambda e, sb_=sb_, nq=nq: e.activation(pTs[sb_][:, 0:nq], pss[sb_][:, 0:nq], AF.Exp, scale=MLA_SCALE),
                 r=['pss%d' % sb_], w=['pT%d' % sb_])
            for qb in range(nq // 128):
                P.op('pe', lambda e, sb_=sb_, kt=kt, qb=qb, nkt=nkt: e.matmul(
                    pso[qb][:, 0:65], pTs[sb_][:, qb * 128:(qb + 1) * 128], Vs[:, kt, :],
                    start=(kt == 0), stop=(kt == nkt - 1)),
                    r=['V%d' % (kt // 26), 'pT%d' % sb_], w=['pso%d' % qb])
            if kt == nkt - 1:
                ob = gi % 2
                nb = nq // 128
                for qb in range(nb):
                    P.op('dve', lambda e, ob=ob, qb=qb: e.tensor_copy(ost[ob][:, qb, :], pso[qb][:, 0:65]),
                         r=['pso%d' % qb], w=['ost%d' % ob])
                P.dma('sp', Od[q0:q0 + nq, :].rearrange("(b p) c -> p b c", p=128), ost[ob][:, 0:nb, :], r=['ost%d' % ob])
        P.finish()
    return nc


CB = 20
NBLK = NCH // CB
RING = 8


def build_B2():
    nc = bass.Bass("TRN2", target_bir_lowering=False)
    qTd = dram_in(nc, "qT", [64, T])
    kTd = dram_in(nc, "kT", [64, T])
    ktd = dram_in(nc, "kt", [64, NCH, 64])
    vxd = dram_in(nc, "vx", [64, NCH, 129])
    igd = dram_in(nc, "ig", [64, NCH])
    lfd = dram_in(nc, "lf", [64, NCH])
    trid = dram_in(nc, "tri", [64, 64])
    oned = dram_in(nc, "ones", [64, 64])
    H = dram_out(nc, "H", [64, NCH, 128])
    with ExitStack() as es:
        P = Prog(nc, es)
        qT = P.sb([64, T], BF16)
        kT = P.sb([64, T], BF16)
        kt = P.sb([64, NCH, 64], BF16)
        ig = P.sb([64, NCH], F32)
        lf = P.sb([64, NCH], F32)
        tri = P.sb([64, 64], F32)
        ones = P.sb([64, 64], F32)
        bb = P.sb([64, NCH], F32)
        ebl = P.sb([64, NCH], F32)
        ek = P.sb([64, NCH], F32)
        ek2 = P.sb([64, NCH], F32)
        eb = P.sb([64, NCH], F32)
        tmp = P.sb([64, NCH], F32)
        vx = [P.sb([64, CB, 129], F32)] * 2
        v1 = [P.sb([64, CB, 129], BF16) for _ in range(2)]
        v2 = [P.sb([64, CB, 129], BF16) for _ in range(2)]
        Us = [P.sb([64, CB, 129], F32) for _ in range(2)]
        MTb = [P.sb([64, CB, 64], BF16) for _ in range(2)]
        hr = [P.sb([64, CB, 129], F32) for _ in range(2)]
        ho = [P.sb([64, CB, 128], F32)] * 2
        tn = [P.sb([64, CB], F32) for _ in range(2)]
        Cst = [P.sb([64, 129], F32) for _ in range(3)]
        Cbf = [P.sb([64, 129], BF16) for _ in range(RING)]
        psS = [P.ps([64, 512], F32) for _ in range(2)]
        psU = [P.ps([64, 512], F32) for _ in range(2)]
        ps_h = [P.ps([64, 512], F32) for _ in range(3)]
        pb = psS[0]
        pbl = psU[0]
        CW = 1280
        P.dma('sp', ig[:], igd, w=['ig'])
        P.dma('sp', lf[:], lfd, w=['lf'])
        P.dma('sp', tri[:], trid, w=['tri'])
        P.dma('sp', ones[:], oned, w=['ones'])
        for b in range(NBLK):
            c0 = b * CW
            P.dma('pool', qT[:, c0:c0 + CW], qTd[:, c0:c0 + CW], w=['qT%d' % b])
            P.dma('pool', kT[:, c0:c0 + CW], kTd[:, c0:c0 + CW], w=['kT%d' % b])
            P.dma('pool', kt[:, b * CB:(b + 1) * CB, :], ktd[:, b * CB:(b + 1) * CB, :], w=['kt%d' % b])
        P.op('pe', lambda e: e.matmul(pb[:, 0:NCH], tri[:], lf[:], start=True, stop=True), r=['tri', 'lf'], w=['psS0'])
        P.op('pe', lambda e: e.matmul(pbl[:, 0:NCH], ones[:], lf[:], start=True, stop=True), r=['ones', 'lf'], w=['psU0'])
        P.op('dve', lambda e: e.tensor_copy(bb[:], pb[:, 0:NCH]), r=['psS0'], w=['bb'])
        P.op('act', lambda e: e.activation(ebl[:], pbl[:, 0:NCH], AF.Exp), r=['psU0'], w=['ebl'])
        P.op('act', lambda e: e.activation(eb[:], bb[:], AF.Exp), r=['bb'], w=['eb'])
        P.op('dve', lambda e: e.tensor_tensor(tmp[:], ig[:], bb[:], ALU.subtract), r=['ig', 'bb'], w=['tmp'])
        P.op('act', lambda e: e.activation(ek[:], tmp[:], AF.Exp), r=['tmp'], w=['ek'])
        P.op('dve', lambda e: e.tensor_tensor(tmp[:], tmp[:], pbl[:, 0:NCH], ALU.add), r=['tmp', 'psU0', 'ek'], w=['tmp2'])
        P.op('act', lambda e: e.activation(ek2[:], tmp[:], AF.Exp), r=['tmp2'], w=['ek2'])
        P.op('dve', lambda e: e.memset(Cst[0][:], 0.0), w=['Cst0'])
        P.op('dve', lambda e: e.memset(Cbf[0][:], 0.0), w=['Cbf0'])
        sct = [0]
        uct = [0]

        def batch(b):
            s = b % 2
            P.dma('sp', vx[s][:], vxd[:, b * CB:(b + 1) * CB, :], w=['vx'])
            P.op('dve', lambda e: e.tensor_tensor(v1[s][:], vx[s][:],
                                                  ek[:, b * CB:(b + 1) * CB].unsqueeze(2).to_broadcast([64, CB, 129]), ALU.mult),
                 r=['vx', 'ek'], w=['v1_%d' % s])
            P.op('dve', lambda e: e.tensor_tensor(v2[s][:], vx[s][:],
                                                  ek2[:, b * CB:(b + 1) * CB].unsqueeze(2).to_broadcast([64, CB, 129]), ALU.mult),
                 r=['vx', 'ek2'], w=['v2_%d' % s])
            for ci0 in range(0, CB, 8):
                nb = min(8, CB - ci0)
                bk = sct[0] % 2
                sct[0] += 1
                for j in range(nb):
                    c = b * CB + ci0 + j
                    P.op('pe', lambda e, c=c, j=j, bk=bk: e.matmul(psS[bk][:, j * 64:(j + 1) * 64], kT[:, c * 64:(c + 1) * 64],
                                                                 qT[:, c * 64:(c + 1) * 64], start=True, stop=True),
                         r=['qT%d' % ((c * 64) // CW), 'kT%d' % ((c * 64) // CW)], w=['psS%d' % bk])
                P.op('dve', lambda e, ci0=ci0, nb=nb, bk=bk: e.tensor_tensor(
                    MTb[s][:, ci0:ci0 + nb, :], psS[bk][:, 0:nb * 64].rearrange("p (c j) -> p c j", c=nb),
                    tri[:].unsqueeze(1).to_broadcast([64, nb, 64]), ALU.mult),
                    r=['psS%d' % bk, 'tri'], w=['MT%d' % s])
            for ci0 in range(0, CB, 3):
                nb = min(3, CB - ci0)
                bk = uct[0] % 2
                uct[0] += 1
                for j in range(nb):
                    c = b * CB + ci0 + j
                    P.op('pe', lambda e, c=c, j=j, bk=bk, ci0=ci0: e.matmul(psU[bk][:, j * 129:(j + 1) * 129], kt[:, c, :],
                                                                          v2[s][:, ci0 + j, :], start=True, stop=True),
                         r=['kt%d' % b, 'v2_%d' % s], w=['psU%d' % bk])
                P.op('act', lambda e, ci0=ci0, nb=nb, bk=bk: e.activation(
                    Us[s][:, ci0:ci0 + nb, :], psU[bk][:, 0:nb * 129].rearrange("p (c j) -> p c j", c=nb), AF.Copy),
                    r=['psU%d' % bk], w=['Us%d' % s])

        batch(0)
        for b in range(NBLK):
            s = b % 2
            if b + 1 < NBLK:
                batch(b + 1)
            for ci in range(CB):
                c = b * CB + ci
                hb_ = c % 3
                P.op('pe', lambda e, ci=ci, hb_=hb_, s=s: e.matmul(ps_h[hb_][:, 0:129], MTb[s][:, ci, :], v1[s][:, ci, :], start=True, stop=False),
                     r=['MT%d' % s, 'v1_%d' % s], w=['ps_h%d' % hb_])
                P.op('pe', lambda e, c=c, hb_=hb_: e.matmul(ps_h[hb_][:, 0:129], qT[:, c * 64:(c + 1) * 64], Cbf[c % RING][:], start=False, stop=True),
                     r=['qT%d' % ((c * 64) // CW), 'Cbf%d' % (c % RING)], w=['ps_h%d' % hb_])
                P.op('act', lambda e, ci=ci, hb_=hb_, s=s: e.activation(hr[s][:, ci, :], ps_h[hb_][:, 0:129], AF.Copy),
                     r=['ps_h%d' % hb_], w=['hr%d' % s])
                if c + 1 < NCH:
                    P.op('dve', lambda e, c=c, ci=ci, s=s: e.scalar_tensor_tensor(Cst[(c + 1) % 3][:], Cst[c % 3][:], ebl[:, c:c + 1],
                                                                         Us[s][:, ci, :], ALU.mult, ALU.add),
                         r=['Cst%d' % (c % 3), 'ebl', 'Us%d' % s], w=['Cst%d' % ((c + 1) % 3)])
                    P.op('dve', lambda e, c=c, ci=ci, s=s: e.scalar_tensor_tensor(Cbf[(c + 1) % RING][:], Cst[c % 3][:], ebl[:, c:c + 1],
                                                                              Us[s][:, ci, :], ALU.mult, ALU.add),
                         r=['Cst%d' % (c % 3), 'ebl', 'Us%d' % s], w=['Cbf%d' % ((c + 1) % RING)])
            sl = slice(b * CB, (b + 1) * CB)
            P.op('dve', lambda e, sl=sl, s=s: e.tensor_tensor(tn[s][:], hr[s][:, :, 128], eb[:, sl], ALU.mult),
                 r=['hr%d' % s, 'eb'], w=['tn%d' % s])
            P.op('act', lambda e, s=s: e.activation(tn[s][:], tn[s][:], AF.Abs), r=['tn%d' % s], w=['tn%d' % s])
            P.op('dve', lambda e, s=s: e.tensor_scalar(tn[s][:], tn[s][:], 1.0, None, ALU.max), r=['tn%d' % s], w=['tn%d' % s])
            P.op('dve', lambda e, s=s: e.reciprocal(tn[s][:], tn[s][:]), r=['tn%d' % s], w=['tn%d' % s])
            P.op('dve', lambda e, sl=sl, s=s: e.tensor_tensor(tn[s][:], tn[s][:], eb[:, sl], ALU.mult),
                 r=['tn%d' % s, 'eb'], w=['tn%d' % s])
            P.op('dve', lambda e, s=s: e.tensor_tensor(ho[s][:], hr[s][:, :, 0:128],
                                                  tn[s][:].unsqueeze(2).to_broadcast([64, CB, 128]), ALU.mult),
                 r=['hr%d' % s, 'tn%d' % s], w=['ho'])
            P.dma('sp', H[:, sl, :], ho[s][:], r=['ho'])
        P.finish()
    return nc


def ln_tile(P, z, zkey, st, mv, rs, sfx, gB=None, bB=None, gkeys=()):
    for hh in range(2):
        P.op('dve', lambda e, hh=hh: e.bn_stats(st[:, hh * 6:(hh + 1) * 6], z[:, hh * 512:(hh + 1) * 512]),
             r=[zkey], w=['st' + sfx])
    P.op('dve', lambda e: e.bn_aggr(mv[:], st[:]), r=['st' + sfx], w=['mv' + sfx])
    ln_rstd(P, mv[:, 1:2], rs[:], ['mv' + sfx], 'rs' + sfx)
    P.op('dve', lambda e: e.tensor_scalar(z[:], z[:], mv[:, 0:1], rs[:, 0:1], ALU.subtract, ALU.mult),
         r=[zkey, 'mv' + sfx, 'rs' + sfx], w=[zkey])
    if gB is not None:
        P.op('dve', lambda e: e.tensor_tensor(z[:], z[:], gB[:], ALU.mult), r=[zkey] + list(gkeys), w=[zkey])
        P.op('dve', lambda e: e.tensor_tensor(z[:], z[:], bB[:], ALU.add), r=[zkey] + list(gkeys), w=[zkey])


def transpose_to(P, src, skey, nch, tp, idt, dst_fn, dkeys, evac):
    for k in range(nch):
        P.op('pe', lambda e, k=k: e.transpose(tp[:, k, :], src[:, k * 128:(k + 1) * 128], idt[:]),
             r=[skey, 'idt'], w=['tp%d' % (k // 4)])
    for k in range(nch):
        evac(k, tp[:, k, :], 'tp%d' % (k // 4))


def build_C1():
    nc = bass.Bass("TRN2", target_bir_lowering=False)
    xd = dram_in(nc, "x", [TC, D])
    od = dram_in(nc, "o", [TC, 8, 65])
    hfd = dram_in(nc, "hf", [TC, 512])
    hbd = dram_in(nc, "hb", [TC, 512])
    spod = dram_in(nc, "spo", [TC, 512])
    spmd = dram_in(nc, "spm", [TC, 2048])
    ident = dram_in(nc, "ident", [128, 128])
    wmla_d = dram_in(nc, "wmla", [128, 4, D])
    wmls_d = dram_in(nc, "wmls", [128, 4, D])
    wout_d = dram_in(nc, "wout", [128, 8, D])
    gmh_d = dram_in(nc, "gmhB", [128, 512])
    g1_d = dram_in(nc, "g1B", [128, 2, D])
    lng_d = dram_in(nc, "lngB", [128, D])
    lnb_d = dram_in(nc, "lnbB", [128, D])
    xo = dram_out(nc, "xo", [TC, D])
    with ExitStack() as es:
        P = Prog(nc, es)
        idt = P.sb([128, 128], F32)
        wmla = P.sb([128, 4, D], BF16)
        wmls = P.sb([128, 4, D], BF16)
        wout = P.sb([128, 8, D], BF16)
        gmh = P.sb([128, 512], F32)
        g1B = P.sb([128, 2, D], F32)
        lng = P.sb([128, D], F32)
        lnb = P.sb([128, D], F32)
        xs = [P.sb([128, D], F32) for _ in range(2)]
        os_ = [P.sb([128, 8, 65], F32) for _ in range(2)]
        hf = [P.sb([128, 512], F32) for _ in range(2)]
        hb = [P.sb([128, 512], F32) for _ in range(2)]
        po = [P.sb([128, 512], F32) for _ in range(2)]
        pm = [P.sb([128, 2048], F32) for _ in range(2)]
        rec = [P.sb([128, 8], F32) for _ in range(2)]
        on = [P.sb([128, 512], F32) for _ in range(2)]
        onT = [P.sb([128, 4, 128], BF16) for _ in range(2)]
        hnT = [P.sb([128, 4, 128], BF16) for _ in range(2)]
        ymT = P.sb([128, 8, 128], BF16)
        s4 = [P.sb([128, 4], F32) for _ in range(2)]
        v4 = [P.sb([128, 4], F32) for _ in range(2)]
        cen = [P.sb([128, 512], F32) for _ in range(2)]
        sqv = [P.sb([128, 512], F32) for _ in range(2)]
        ta = P.sb([128, D], F32)
        tb = P.sb([128, D], F32)
        st = P.sb([128, 12], F32)
        mv = P.sb([128, 2], F32)
        rs = P.sb([128, 1], F32)
        tp = P.ps([128, 8, 128], F32)
        pA = P.ps([128, 2, 512], F32)
        pB = P.ps([128, 2, 512], F32)
        pY = P.ps([128, 2, 512], F32)
        P.dma('sp', idt[:], ident, w=['idt'])
        P.dma('sp', gmh[:], gmh_d, w=['gmh'])
        P.dma('sp', g1B[:], g1_d, w=['g1B'])
        P.dma('sp', lng[:], lng_d, w=['lng'])
        P.dma('sp', lnb[:], lnb_d, w=['lng'])
        for k in range(4):
            P.dma('pool', wmla[:, k, :], wmla_d[:, k, :], w=['wmla'])
            P.dma('pool', wmls[:, k, :], wmls_d[:, k, :], w=['wmls'])
        for k in range(8):
            P.dma('pool', wout[:, k, :], wout_d[:, k, :], w=['wout'])

        def evac_to(dstT, dkey):
            def f(k, src, skey):
                P.op('act', lambda e: e.activation(dstT[:, k, :], src, AF.Copy), r=[skey], w=[dkey])
            return f

        def proj(ps, pskey, lT, lkey, w, wkey, nk):
            for half in range(2):
                for k in range(nk):
                    P.op('pe', lambda e, half=half, k=k: e.matmul(ps[:, half, :], lT[:, k, :], w[:, k, half * 512:(half + 1) * 512],
                                                                 start=(k == 0), stop=(k == nk - 1)),
                         r=[lkey, wkey], w=[pskey + str(half)])

        def stageX(i):
            b = i % 2
            rows = slice(i * 128, (i + 1) * 128)
            P.dma('sp', os_[b][:], od[rows], w=['o%d' % b])
            P.dma('sp', hf[b][:], hfd[rows], w=['hf%d' % b])
            P.dma('sp', hb[b][:], hbd[rows], w=['hb%d' % b])
            P.dma('sp', po[b][:], spod[rows], w=['po%d' % b])
            P.dma('sp', pm[b][:], spmd[rows], w=['pm%d' % b])
            P.dma('sp', xs[b][:], xd[rows], w=['xs%d' % b])
            on_, onT_, hnT_, cen_, sqv_, rec_, s4_, v4_ = on[b], onT[b], hnT[b], cen[b], sqv[b], rec[b], s4[b], v4[b]
            kb = str(b)
            P.op('dve', lambda e: e.reciprocal(rec_[:], os_[b][:, :, 64]), r=['o' + kb], w=['rec' + kb])
            P.op('dve', lambda e: e.tensor_tensor(on_[:].rearrange("p (h d) -> p h d", h=8), os_[b][:, :, 0:64],
                                                  rec_[:].unsqueeze(2).to_broadcast([128, 8, 64]), ALU.mult),
                 r=['o' + kb, 'rec' + kb], w=['on' + kb])
            transpose_to(P, on_, 'on' + kb, 4, tp, idt, None, None, evac_to(onT_, 'onT' + kb))
            P.op('dve', lambda e: e.tensor_tensor(cen_[:], hf[b][:], hb[b][:], ALU.add), r=['hf' + kb, 'hb' + kb], w=['cen' + kb])
            c3 = cen_[:].rearrange("p (h d) -> p h d", h=4)
            P.op('dve', lambda e: e.tensor_reduce(s4_[:], c3, AX.X, ALU.add), r=['cen' + kb], w=['s4' + kb])
            P.op('dve', lambda e: e.tensor_scalar(s4_[:], s4_[:], -1.0 / 128, None, ALU.mult), r=['s4' + kb], w=['s4' + kb])
            P.op('dve', lambda e: e.tensor_tensor(c3, c3, s4_[:].unsqueeze(2).to_broadcast([128, 4, 128]), ALU.add),
                 r=['cen' + kb, 's4' + kb], w=['cen' + kb])
            P.op('dve', lambda e: e.tensor_tensor(sqv_[:], cen_[:], cen_[:], ALU.mult), r=['cen' + kb], w=['sqv' + kb])
            P.op('dve', lambda e: e.tensor_reduce(v4_[:], sqv_[:].rearrange("p (h d) -> p h d", h=4), AX.X, ALU.add),
                 r=['sqv' + kb], w=['v4' + kb])
            ln_rstd(P, v4_[:], v4_[:], ['v4' + kb], 'v4' + kb, scale=1.0 / 128)
            P.op('dve', lambda e: e.tensor_tensor(c3, c3, v4_[:].unsqueeze(2).to_broadcast([128, 4, 128]), ALU.mult),
                 r=['cen' + kb, 'v4' + kb], w=['cen' + kb])
            P.op('dve', lambda e: e.tensor_tensor(cen_[:], cen_[:], gmh[:], ALU.mult), r=['cen' + kb, 'gmh'], w=['cen' + kb])
            P.op('dve', lambda e: e.tensor_tensor(cen_[:], cen_[:], po[b][:], ALU.mult), r=['cen' + kb, 'po' + kb], w=['cen' + kb])
            transpose_to(P, cen_, 'cen' + kb, 4, tp, idt, None, None, evac_to(hnT_, 'hnT' + kb))

        def stageY(i):
            b = i % 2
            kb = str(b)
            sel = 1 if i < 2 else 0
            rows = slice(i * 128, (i + 1) * 128)
            proj(pA, 'pA', onT[b], 'onT' + kb, wmla, 'wmla', 4)
            proj(pB, 'pB', hnT[b], 'hnT' + kb, wmls, 'wmls', 4)
            for half in range(2):
                hs = slice(half * 512, (half + 1) * 512)
                P.op('dve', lambda e, half=half, hs=hs: e.tensor_tensor(ta[:, hs], pA[:, half, :], pm[b][:, hs], ALU.mult),
                     r=['pA%d' % half, 'pm' + kb], w=['ta'])
                P.op('dve', lambda e, half=half, hs=hs: e.tensor_tensor(
                    tb[:, hs], pB[:, half, :], pm[b][:, 1024 + half * 512:1024 + (half + 1) * 512], ALU.mult),
                    r=['pB%d' % half, 'pm' + kb], w=['tb'])
            P.op('dve', lambda e: e.tensor_tensor(ta[:], ta[:], tb[:], ALU.add), r=['ta', 'tb'], w=['ta'])
            transpose_to(P, ta, 'ta', 8, tp, idt, None, None, evac_to(ymT, 'ymT'))
            proj(pY, 'pY', ymT, 'ymT', wout, 'wout', 8)
            for half in range(2):
                hs = slice(half * 512, (half + 1) * 512)
                P.op('dve', lambda e, half=half, hs=hs: e.tensor_tensor(tb[:, hs], pY[:, half, :], g1B[:, sel, hs], ALU.mult),
                     r=['pY%d' % half, 'g1B'], w=['tb'])
            P.op('dve', lambda e: e.scalar_tensor_tensor(tb[:], xs[b][:], ALPHA, tb[:], ALU.mult, ALU.add),
                 r=['xs' + kb, 'tb'], w=['tb'])
            ln_tile(P, tb, 'tb', st, mv, rs, '', lng, lnb, ['lng'])
            P.dma('sp', xo[rows], tb[:], r=['tb'])

        stageX(0)
        for i in range(NTILE):
            if i + 1 < NTILE:
                stageX(i + 1)
            stageY(i)
        P.finish()
    return nc


def bcast(v, p=128):
    return np.ascontiguousarray(np.broadcast_to(np.asarray(v, np.float32).reshape(1, -1), (p, np.asarray(v).size)))


def prep_C1_weights(w_bo_mla, w_bo_mlstm, w_out, g_mh, ln1_g, ln1_b, g1_lat, g1_ctx):
    return {"wmla": kp(w_bo_mla, 4), "wmls": kp(w_bo_mlstm, 4), "wout": kp(w_out, 8),
            "gmhB": bcast(g_mh), "lngB": bcast(ln1_g), "lnbB": bcast(ln1_b),
            "g1B": np.ascontiguousarray(np.stack([bcast(g1_lat), bcast(g1_ctx)], axis=1)),
            "ident": np.eye(128, dtype=np.float32)}


NEXP = 32
DEXP = 256


def build_C2():
    nc = bass.Bass("TRN2", target_bir_lowering=False)
    xd = dram_in(nc, "x", [TC, D])
    ident = dram_in(nc, "ident", [128, 128])
    mod2 = dram_in(nc, "mod2", [128, 8, 4])
    wr_d = dram_in(nc, "wr", [128, 8, 36])
    br_d = dram_in(nc, "brB", [128, 36])
    g2_d = dram_in(nc, "g2B", [128, 2, D])
    lng_d = dram_in(nc, "lngB", [128, D])
    lnb_d = dram_in(nc, "lnbB", [128, D])
    wg_d = dram_in(nc, "wg", [NEXP, 128, 8, DEXP])
    wu_d = dram_in(nc, "wu", [NEXP, 128, 8, DEXP])
    wd_d = dram_in(nc, "wd", [NEXP, 128, 2, D])
    xo = dram_out(nc, "xo", [TC, D])
    with ExitStack() as es:
        P = Prog(nc, es)
        idt = P.sb([128, 128], F32)
        mods = P.sb([128, 8, 4], F32)
        wr = P.sb([128, 8, 36], F32)
        brB = P.sb([128, 36], F32)
        g2B = P.sb([128, 2, D], F32)
        lng = P.sb([128, D], F32)
        lnb = P.sb([128, D], F32)
        h2T = P.sb([128, 8, TC], BF16)
        hTf = P.sb([128, 8, 128], F32)
        yacc = P.sb([128, NTILE, D], F32)
        comb_all = P.sb([128, NTILE, 32], F32)
        wg = [P.sb([128, 8, DEXP], BF16) for _ in range(2)]
        wu = [P.sb([128, 8, DEXP], BF16) for _ in range(2)]
        wd = [P.sb([128, 2, D], BF16) for _ in range(2)]
        xs = [P.sb([128, D], F32) for _ in range(2)]
        st = P.sb([128, 12], F32)
        mv = P.sb([128, 2], F32)
        rs = P.sb([128, 1], F32)
        lg = P.sb([128, 36], F32)
        sm = [P.sb([128, 8], F32, name="sm%d" % j) for j in range(12)]
        selg = P.sb([128, 4, 8], F32)
        sa = [P.sb([128, 512], F32) for _ in range(2)]
        hid = [P.sb([128, 2, 512], BF16) for _ in range(2)]
        tp = P.ps([128, 8, 128], F32)
        pA = [P.ps([128, 512], F32) for _ in range(2)]
        pU = [P.ps([128, 512], F32) for _ in range(2)]
        pY = [P.ps([128, 512], F32) for _ in range(2)]
        pC = pY[0]
        P.dma('sp', idt[:], ident, w=['idt'])
        P.dma('sp', mods[:], mod2, w=['mods'])
        P.dma('sp', wr[:], wr_d, w=['wr'])
        P.dma('sp', brB[:], br_d, w=['brB'])
        P.dma('sp', g2B[:], g2_d, w=['g2B'])
        P.dma('sp', lng[:], lng_d, w=['lng'])
        P.dma('sp', lnb[:], lnb_d, w=['lng'])
        P.op('dve', lambda e: e.tensor_scalar(mods[:, :, 0:1], mods[:, :, 0:1], 1.0, None, ALU.add), r=['mods'], w=['mods'])
        P.op('dve', lambda e: e.tensor_scalar(mods[:, :, 2:3], mods[:, :, 2:3], 1.0, None, ALU.add), r=['mods'], w=['mods'])

        def load_expert(e_):
            s = e_ % 2
            for k0 in range(0, 8, 4):
                P.dma('pool', wg[s][:, k0:k0 + 4, :], wg_d[e_, :, k0:k0 + 4, :], w=['wg%d' % s])
                P.dma('pool', wu[s][:, k0:k0 + 4, :], wu_d[e_, :, k0:k0 + 4, :], w=['wu%d' % s])
            for f in range(2):
                P.dma('pool', wd[s][:, f, :], wd_d[e_, :, f, :], w=['wd%d' % s])

        load_expert(0)
        for i in range(NTILE):
            b = i % 2
            msel = 2 if i < 2 else 0
            rows = slice(i * 128, (i + 1) * 128)
            P.dma('sp', xs[b][:], xd[rows], w=['xs%d' % b])
            ln_tile(P, xs[b], 'xs%d' % b, st, mv, rs, '')

            def evac(k, src, skey, i=i, msel=msel):
                P.op('act', lambda e: e.activation(h2T[:, k, i * 128:(i + 1) * 128], src, AF.Identity,
                                                   bias=mods[:, k, msel + 1:msel + 2], scale=mods[:, k, msel:msel + 1]),
                     r=[skey, 'mods'], w=['h2T%d' % i])
                P.op('act', lambda e: e.activation(hTf[:, k, :], src, AF.Identity,
                                                   bias=mods[:, k, msel + 1:msel + 2], scale=mods[:, k, msel:msel + 1]),
                     r=[skey, 'mods'], w=['hTf'])
            transpose_to(P, xs[b], 'xs%d' % b, 8, tp, idt, None, None, evac)
            for k in range(8):
                P.op('pe', lambda e, k=k: e.matmul(pC[:, 0:36], hTf[:, k, :], wr[:, k, :], start=(k == 0), stop=(k == 7)),
                     r=['hTf', 'wr'], w=['pY0'])
            P.op('dve', lambda e: e.tensor_tensor(lg[:], pC[:, 0:36], brB[:], ALU.add), r=['pY0', 'brB'], w=['lg'])
            gmax, ohg, negm, eg, sume, lsel, m1, mk1 = [sm[j] for j in range(8)]
            l2, m2, mk2, tt = sm[8], sm[9], sm[10], sm[11]
            lgG = lg[:, 0:4]
            lgE = lg[:, 4:36].rearrange("p (g e) -> p g e", g=4)

            def dv(fn, r, w):
                P.op('dve', fn, r=r, w=w)
            dv(lambda e: e.tensor_reduce(gmax[:, 0:1], lgG, AX.X, ALU.max), ['lg'], ['gmax'])
            dv(lambda e: e.tensor_scalar(ohg[:, 0:4], lgG, gmax[:, 0:1], None, ALU.is_equal), ['lg', 'gmax'], ['ohg'])
            dv(lambda e: e.tensor_scalar(negm[:, 0:1], gmax[:, 0:1], -1.0, None, ALU.mult), ['gmax'], ['negm'])
            P.op('act', lambda e: e.activation(eg[:, 0:4], lgG, AF.Exp, bias=negm[:, 0:1], scale=1.0), r=['lg', 'negm'], w=['eg'])
            dv(lambda e: e.tensor_reduce(sume[:, 0:1], eg[:, 0:4], AX.X, ALU.add), ['eg'], ['sume'])
            dv(lambda e: e.reciprocal(sume[:, 0:1], sume[:, 0:1]), ['sume'], ['sume'])
            dv(lambda e: e.tensor_tensor(selg[:], lgE, ohg[:, 0:4].unsqueeze(2).to_broadcast([128, 4, 8]), ALU.mult),
               ['lg', 'ohg'], ['selg'])
            dv(lambda e: e.tensor_reduce(lsel[:], selg[:].rearrange("p g e -> p e g"), AX.X, ALU.add), ['selg'], ['lsel'])
            dv(lambda e: e.tensor_reduce(m1[:, 0:1], lsel[:], AX.X, ALU.max), ['lsel'], ['m1'])
            dv(lambda e: e.tensor_scalar(mk1[:], lsel[:], m1[:, 0:1], None, ALU.is_equal), ['lsel', 'm1'], ['mk1'])
            dv(lambda e: e.scalar_tensor_tensor(l2[:], mk1[:], -1e30, lsel[:], ALU.mult, ALU.add), ['mk1', 'lsel'], ['l2'])
            dv(lambda e: e.tensor_reduce(m2[:, 0:1], l2[:], AX.X, ALU.max), ['l2'], ['m2'])
            dv(lambda e: e.tensor_scalar(mk2[:], l2[:], m2[:, 0:1], None, ALU.is_equal), ['l2', 'm2'], ['mk2'])
            dv(lambda e: e.tensor_tensor(tt[:, 0:1], m2[:, 0:1], m1[:, 0:1], ALU.subtract), ['m2', 'm1'], ['tt'])
            P.op('act', lambda e: e.activation(tt[:, 1:2], tt[:, 0:1], AF.Exp), r=['tt'], w=['tt'])
            dv(lambda e: e.tensor_scalar(tt[:, 2:3], tt[:, 1:2], 1.0, None, ALU.add), ['tt'], ['tt'])
            dv(lambda e: e.reciprocal(tt[:, 2:3], tt[:, 2:3]), ['tt'], ['tt'])
            dv(lambda e: e.tensor_tensor(tt[:, 3:4], tt[:, 2:3], sume[:, 0:1], ALU.mult), ['tt', 'sume'], ['tt'])
            dv(lambda e: e.tensor_tensor(tt[:, 4:5], tt[:, 3:4], tt[:, 1:2], ALU.mult), ['tt'], ['tt'])
            dv(lambda e: e.tensor_scalar(mk1[:], mk1[:], tt[:, 3:4], None, ALU.mult), ['mk1', 'tt'], ['mk1'])
            dv(lambda e: e.scalar_tensor_tensor(mk2[:], mk2[:], tt[:, 4:5], mk1[:], ALU.mult, ALU.add), ['mk2', 'tt', 'mk1'], ['mk2'])
            dv(lambda e, i=i: e.tensor_tensor(comb_all[:, i, :].rearrange("p (g e) -> p g e", g=4),
                                              ohg[:, 0:4].unsqueeze(2).to_broadcast([128, 4, 8]),
                                              mk2[:].unsqueeze(1).to_broadcast([128, 4, 8]), ALU.mult), ['ohg', 'mk2'], ['comb%d' % i])

        stepn = [0]
        for e_ in range(NEXP):
            s = e_ % 2
            if e_ + 1 < NEXP:
                load_expert(e_ + 1)
            for (t0, n) in GROUPS:
                hb_ = stepn[0] % 2
                stepn[0] += 1
                tiles = ['h2T%d' % t for t in range(t0 // 128, (t0 + n) // 128)]
                for fc in range(2):
                    ab = (2 * stepn[0] + fc) % 2
                    for k in range(8):
                        P.op('pe', lambda e, k=k, fc=fc, ab=ab, s=s, t0=t0, n=n: e.matmul(
                            pA[ab][:, 0:n], wg[s][:, k, fc * 128:(fc + 1) * 128], h2T[:, k, t0:t0 + n],
                            start=(k == 0), stop=(k == 7)), r=['wg%d' % s] + tiles, w=['pA%d' % ab])
                    for k in range(8):
                        P.op('pe', lambda e, k=k, fc=fc, ab=ab, s=s, t0=t0, n=n: e.matmul(
                            pU[ab][:, 0:n], wu[s][:, k, fc * 128:(fc + 1) * 128], h2T[:, k, t0:t0 + n],
                            start=(k == 0), stop=(k == 7)), r=['wu%d' % s] + tiles, w=['pU%d' % ab])
                    P.op('act', lambda e, ab=ab, n=n: e.activation(sa[ab][:, 0:n], pA[ab][:, 0:n], AF.Silu),
                         r=['pA%d' % ab], w=['sa%d' % ab])
                    P.op('dve', lambda e, ab=ab, n=n, fc=fc, hb_=hb_: e.tensor_tensor(hid[hb_][:, fc, 0:n], sa[ab][:, 0:n], pU[ab][:, 0:n], ALU.mult),
                         r=['sa%d' % ab, 'pU%d' % ab], w=['hid%d' % hb_])
                for tt_ in range(n // 128):
                    ti = t0 // 128 + tt_
                    for half in range(2):
                        yb = (2 * ti + half) % 2
                        for fc in range(2):
                            P.op('pe', lambda e, fc=fc, half=half, yb=yb, hb_=hb_, tt_=tt_, s=s: e.matmul(
                                pY[yb][:, :], hid[hb_][:, fc, tt_ * 128:(tt_ + 1) * 128], wd[s][:, fc, half * 512:(half + 1) * 512],
                                start=(fc == 0), stop=(fc == 1)), r=['hid%d' % hb_, 'wd%d' % s], w=['pY%d' % yb])
                        ysl = yacc[:, ti, half * 512:(half + 1) * 512]
                        cw = comb_all[:, ti, e_:e_ + 1]
                        if e_ == 0:
                            P.op('dve', lambda e, yb=yb, ysl=ysl, cw=cw: e.tensor_scalar(ysl, pY[yb][:, :], cw, None, ALU.mult),
                                 r=['pY%d' % yb, 'comb%d' % ti], w=['yacc%d' % ti])
                        else:
                            P.op('dve', lambda e, yb=yb, ysl=ysl, cw=cw: e.scalar_tensor_tensor(ysl, pY[yb][:, :], cw, ysl, ALU.mult, ALU.add),
                                 r=['pY%d' % yb, 'yacc%d' % ti, 'comb%d' % ti], w=['yacc%d' % ti])
        for i in range(NTILE):
            b = i % 2
            gs = 1 if i < 2 else 0
            rows = slice(i * 128, (i + 1) * 128)
            P.dma('sp', xs[b][:], xd[rows], w=['xs%d' % b])
            P.op('dve', lambda e, i=i, gs=gs: e.tensor_tensor(yacc[:, i, :], yacc[:, i, :], g2B[:, gs, :], ALU.mult),
                 r=['yacc%d' % i, 'g2B'], w=['yacc%d' % i])
            P.op('dve', lambda e, i=i, b=b: e.scalar_tensor_tensor(xs[b][:], xs[b][:], ALPHA, yacc[:, i, :], ALU.mult, ALU.add),
                 r=['xs%d' % b, 'yacc%d' % i], w=['xs%d' % b])
            ln_tile(P, xs[b], 'xs%d' % b, st, mv, rs, '', lng, lnb, ['lng'])
            P.dma('sp', xo[rows], xs[b][:], r=['xs%d' % b])
        P.finish()
    return nc


def prep_C2_weights(w_rg, b_rg, w_re, b_re, w_e_gate, w_e_up, w_e_down, ln2_g, ln2_b, g2_lat, g2_ctx):
    wr = np.concatenate([w_rg, w_re], axis=1)
    br = np.concatenate([b_rg, b_re], axis=0)
    return {"wr": kp(wr, 8), "brB": bcast(br),
            "wg": np.ascontiguousarray(w_e_gate.reshape(NEXP, 8, 128, DEXP).transpose(0, 2, 1, 3)),
            "wu": np.ascontiguousarray(w_e_up.reshape(NEXP, 8, 128, DEXP).transpose(0, 2, 1, 3)),
            "wd": np.ascontiguousarray(w_e_down.reshape(NEXP, 2, 128, D).transpose(0, 2, 1, 3)),
            "lngB": bcast(ln2_g), "lnbB": bcast(ln2_b),
            "g2B": np.ascontiguousarray(np.stack([bcast(g2_lat), bcast(g2_ctx)], axis=1)),
            "ident": np.eye(128, dtype=np.float32)}


_PROGS = {}


def _prog(name):
    if name not in _PROGS:
        _PROGS[name] = {"M": build_M, "A": build_A, "B1": build_B1, "B2": build_B2, "C1": build_C1, "C2": build_C2}[name]()
    return _PROGS[name]


def _run(name, in_maps):
    res = run_bass_kernel_spmd(_prog(name), in_maps, core_ids=list(range(NCORE)))
    return res.results


def _gather_tok(outs, key, axis):
    parts = [np.take(outs[0][key], np.arange(CTX), axis=axis)]
    for i in range(NCORE):
        parts.append(np.take(outs[i][key], np.arange(CTX, TC), axis=axis))
    return np.concatenate(parts, axis=axis)


def kernel(x, c, ctx, c_ctx, w_ada, b_ada, w_in, b_gates, w_uq, w_uk, w_uv, g_qn, g_kvn, g_mh,
           w_bo_mla, w_bo_mlstm, w_out, ln1_g, ln1_b, w_rg, b_rg, w_re, b_re,
           w_e_gate, w_e_up, w_e_down, ln2_g, ln2_b):
    f32 = np.float32
    x = np.asarray(x, f32)
    ctx = np.asarray(ctx, f32)
    ident = np.eye(128, dtype=f32)
    ones128 = np.ones((128, 128), f32)
    cc = np.ascontiguousarray(np.stack([np.asarray(c, f32)[0].reshape(8, 128).T, np.asarray(c_ctx, f32).reshape(8, 128).T], axis=-1))
    wall = np.concatenate([np.asarray(w_ada[l], f32) for l in range(DEPTH)], axis=1)
    ball = np.concatenate([np.asarray(b_ada[l], f32) for l in range(DEPTH)], axis=0)
    ins = []
    for i in range(NCORE):
        ins.append({"cc": cc, "wa": kp(wall[:, i * 3072:(i + 1) * 3072], 8),
                    "ba": np.ascontiguousarray(ball[i * 3072:(i + 1) * 3072].reshape(24, 128).T)})
    outs = _run("M", ins)
    mod = np.concatenate([o["mo"].transpose(1, 0, 2).reshape(3072, 2) for o in outs], axis=0).reshape(DEPTH, 6 * D, 2)
    del wall, ins

    cosT, ssinT = rope_tables_np()
    toks = [core_tokens(i) for i in range(NCORE)]
    cos4 = [np.ascontiguousarray(np.tile(cosT[t].T, (4, 1))) for t in toks]
    sin4 = [np.ascontiguousarray(np.tile(ssinT[t].T, (4, 1))) for t in toks]
    xc = [np.ascontiguousarray(np.concatenate([ctx[0], x[0, i * LAT_C:(i + 1) * LAT_C]], axis=0)) for i in range(NCORE)]
    perm_f = np.arange(T)
    perm_b = np.concatenate([np.arange(CTX - 1, -1, -1), np.arange(T - 1, CTX - 1, -1)])
    tri = np.triu(np.ones((64, 64), f32))
    ones64 = np.ones((64, 64), f32)
    onescol = np.ones((T, 1), f32)

    for l in range(DEPTH):
        m = mod[l]
        sh1, sc1, g1, sh2, sc2, g2 = [m[j * D:(j + 1) * D] for j in range(6)]
        mod1 = kp(np.stack([sc1[:, 0], sh1[:, 0], sc1[:, 1], sh1[:, 1]], axis=-1), 8)
        mod2 = kp(np.stack([sc2[:, 0], sh2[:, 0], sc2[:, 1], sh2[:, 1]], axis=-1), 8)
        wA = prep_A_weights(np.asarray(w_in[l], f32), np.asarray(w_uq[l], f32), np.asarray(w_uk[l], f32),
                            np.asarray(w_uv[l], f32), np.asarray(g_qn[l], f32), np.asarray(g_kvn[l], f32),
                            np.asarray(b_gates[l], f32))
        ins = []
        for i in range(NCORE):
            d_ = {"x": xc[i], "mod1": mod1, "ident": ident, "ones": ones128, "cos4": cos4[i], "ssin4": sin4[i]}
            d_.update(wA)
            ins.append(d_)
        oA = _run("A", ins)
        del ins, wA
        QT = _gather_tok(oA, "QT", 2).reshape(768, T)
        KnT = _gather_tok(oA, "KnT", 2).reshape(512, T)
        KrT = _gather_tok(oA, "KrT", 1)
        Vall = _gather_tok(oA, "V", 0)
        mqT = _gather_tok(oA, "mqT", 2).reshape(256, T)
        mkT = _gather_tok(oA, "mkT", 2).reshape(256, T)
        mkv = _gather_tok(oA, "mkv", 0)
        graw = _gather_tok(oA, "graw", 1)
        glsg = _gather_tok(oA, "glsg", 1)
        ins = []
        for h in range(8):
            Q = np.concatenate([QT[h * 64:(h + 1) * 64], QT[512 + h * 32:512 + (h + 1) * 32]], axis=0)
            Kk = np.concatenate([KnT[h * 64:(h + 1) * 64], KrT], axis=0)
            Vx = np.concatenate([Vall[:, h * 64:(h + 1) * 64], onescol], axis=1).reshape(NKT, 128, 65).transpose(1, 0, 2)
            ins.append({"Q": np.ascontiguousarray(Q), "K": np.ascontiguousarray(Kk), "V": np.ascontiguousarray(Vx)})
        oB1 = _run("B1", ins)
        ins = []
        for cidx in range(8):
            hd, dr = cidx // 2, cidx % 2
            perm = perm_b if dr else perm_f
            q_ = mqT[hd * 64:(hd + 1) * 64][:, perm]
            k_ = mkT[hd * 64:(hd + 1) * 64][:, perm]
            kt_ = mkv[perm, hd * 64:(hd + 1) * 64].reshape(NCH, 64, 64).transpose(1, 0, 2)
            vx_ = np.concatenate([mkv[perm, 256 + hd * 128:256 + (hd + 1) * 128], onescol], axis=1).reshape(NCH, 64, 129).transpose(1, 0, 2)
            ig_ = graw[(2 * dr) * 4 + hd][perm].reshape(NCH, 64).T
            lf_ = glsg[(2 * dr + 1) * 4 + hd][perm].reshape(NCH, 64).T
            ins.append({"qT": np.ascontiguousarray(q_), "kT": np.ascontiguousarray(k_), "kt": np.ascontiguousarray(kt_),
                        "vx": np.ascontiguousarray(vx_), "ig": np.ascontiguousarray(ig_), "lf": np.ascontiguousarray(lf_),
                        "tri": tri, "ones": ones64})
        oB2 = _run("B2", ins)
        del ins
        hf_all = np.empty((T, 512), f32)
        hb_all = np.empty((T, 512), f32)
        for cidx in range(8):
            hd, dr = cidx // 2, cidx % 2
            hp = oB2[cidx]["H"].transpose(1, 0, 2).reshape(T, 128)
            if dr:
                hb_all[perm_b, hd * 128:(hd + 1) * 128] = hp
            else:
                hf_all[:, hd * 128:(hd + 1) * 128] = hp
        o_all = np.stack([oB1[h]["O"] for h in range(8)], axis=1)
        wC1 = prep_C1_weights(np.asarray(w_bo_mla[l], f32), np.asarray(w_bo_mlstm[l], f32), np.asarray(w_out[l], f32),
                              np.asarray(g_mh[l], f32), np.asarray(ln1_g[l], f32), np.asarray(ln1_b[l], f32), g1[:, 0], g1[:, 1])
        ins = []
        for i in range(NCORE):
            d_ = {"x": xc[i], "o": np.ascontiguousarray(o_all[toks[i]]), "hf": np.ascontiguousarray(hf_all[toks[i]]),
                  "hb": np.ascontiguousarray(hb_all[toks[i]]), "spo": oA[i]["spo"], "spm": oA[i]["spm"]}
            d_.update(wC1)
            ins.append(d_)
        oC1 = _run("C1", ins)
        del ins, oA, o_all, hf_all, hb_all
        wC2 = prep_C2_weights(np.asarray(w_rg[l], f32), np.asarray(b_rg[l], f32), np.asarray(w_re[l], f32), np.asarray(b_re[l], f32),
                              np.asarray(w_e_gate[l], f32), np.asarray(w_e_up[l], f32), np.asarray(w_e_down[l], f32),
                              np.asarray(ln2_g[l], f32), np.asarray(ln2_b[l], f32), g2[:, 0], g2[:, 1])
        wC2["mod2"] = mod2
        ins = []
        for i in range(NCORE):
            d_ = {"x": oC1[i]["xo"]}
            d_.update(wC2)
            ins.append(d_)
        oC2 = _run("C2", ins)
        del ins, wC2
        xc = [np.ascontiguousarray(oC2[i]["xo"]) for i in range(NCORE)]
    out = np.concatenate([xc[i][CTX:] for i in range(NCORE)], axis=0)[None]
    return np.ascontiguousarray(out.astype(np.float32))
```

```python
import numpy as np
from contextlib import ExitStack
import concourse.bass as bass
import concourse.mybir as mybir
from concourse.bass_utils import run_bass_kernel_spmd

F32 = mybir.dt.float32
BF16 = mybir.dt.bfloat16
AF = mybir.ActivationFunctionType
ALU = mybir.AluOpType
AX = mybir.AxisListType

EPOCH = 16000
NDS = 12
ENGS = ('pe', 'act', 'dve', 'pool', 'sp')


class Prog:
    def __init__(self, nc, es):
        self.nc = nc
        self.es = es
        self.ops = {e: [] for e in ENGS}
        self.cnt = {e: 0 for e in ENGS}
        self.known = {e: {} for e in ENGS}
        self.lastw = {}
        self.readers = {}
        self.ndma = 0
        self.esems = {e: [] for e in ENGS}
        self.dsems = [es.enter_context(nc.semaphore("dsem%d" % j)) for j in range(NDS)]
        self.nt = 0

    def sb(self, shape, dt, name=None):
        self.nt += 1
        return self.es.enter_context(self.nc.sbuf_tensor(name or ("sb%d" % self.nt), list(shape), dt))

    def ps(self, shape, dt, name=None):
        self.nt += 1
        return self.es.enter_context(self.nc.psum_tensor(name or ("ps%d" % self.nt), list(shape), dt))

    def _esem(self, eng, idx):
        while len(self.esems[eng]) <= idx:
            self.esems[eng].append(self.es.enter_context(
                self.nc.semaphore("s_%s_%d" % (eng, len(self.esems[eng])))))
        return self.esems[eng][idx]

    def _deps(self, eng, r, w, extra=()):
        need = {}

        def add(p, v):
            if p is None:
                return
            if need.get(p, 0) < v:
                need[p] = v
        for k in r:
            lw = self.lastw.get(k)
            if lw:
                add(*lw)
        for k in w:
            lw = self.lastw.get(k)
            if lw:
                add(*lw)
            for p, v in self.readers.get(k, {}).items():
                add(p, v)
        for p, v in extra:
            add(p, v)
        waits = []
        kn = self.known[eng]
        for p, v in need.items():
            if p == eng and eng in ('pe', 'sp'):
                continue
            if kn.get(p, 0) >= v:
                continue
            kn[p] = v
            waits.append((p, v))
        return waits

    def op(self, eng, fn, r=(), w=()):
        waits = self._deps(eng, r, w)
        self.cnt[eng] += 1
        c = self.cnt[eng]
        self.ops[eng].append(('op', fn, waits, c))
        for k in r:
            d = self.readers.setdefault(k, {})
            d[eng] = c
        for k in w:
            self.lastw[k] = (eng, c)
            self.readers[k] = {}
        return c

    def dma(self, q, out, in_, r=(), w=(), **kw):
        j = self.ndma % NDS
        n = self.ndma // NDS + 1
        self.ndma += 1
        prod = ('d', j)
        waits = self._deps(q, r, w, extra=([(prod, n - 1)] if n > 1 else []))
        self.ops[q].append(('dma', (out, in_, kw), waits, (j, n)))
        for k in r:
            d = self.readers.setdefault(k, {})
            d[prod] = n
        for k in w:
            self.lastw[k] = (prod, n)
            self.readers[k] = {}

    def _emit_wait(self, e, p, v):
        if isinstance(p, tuple):
            e.wait_ge(self.dsems[p[1]], 16 * v)
        else:
            idx = (v - 1) // EPOCH
            e.wait_ge(self._esem(p, idx), (v - 1) % EPOCH + 1)

    def _run(self, name, e):
        for kind, payload, waits, c in self.ops[name]:
            for p, v in waits:
                self._emit_wait(e, p, v)
            if kind == 'op':
                ins = payload(e)
                idx = (c - 1) // EPOCH
                ins.then_inc(self._esem(name, idx), 1)
            else:
                out, in_, kw = payload
                j, n = c
                e.dma_start(out=out, in_=in_, **kw).then_inc(self.dsems[j], 16)
        if name == 'sp':
            tot = {}
            for j in range(NDS):
                n = (self.ndma - j + NDS - 1) // NDS if self.ndma > j else 0
                if n > 0:
                    e.wait_ge(self.dsems[j], 16 * n)

    def finish(self):
        nc = self.nc
        for e in ENGS:
            if self.cnt[e] > 0:
                self._esem(e, (self.cnt[e] - 1) // EPOCH)
        with nc.Block() as block:
            @block.sync
            def _(sync):
                self._run('sp', sync)

            @block.tensor
            def _(tensor):
                self._run('pe', tensor)

            @block.scalar
            def _(scalar):
                self._run('act', scalar)

            @block.vector
            def _(vector):
                self._run('dve', vector)

            @block.gpsimd
            def _(gpsimd):
                self._run('pool', gpsimd)


D = 1024
SEQ = 16384
CTX = 256
T = SEQ + CTX
NCORE = 8
LAT_C = SEQ // NCORE
TC = CTX + LAT_C
NTILE = TC // 128
GROUPS = [(0, 256), (256, 512), (768, 512), (1280, 512), (1792, 512)]
DEPTH = 4
EPS = 1e-6
ALPHA = (2 * DEPTH) ** 0.25
MLA_SCALE = 96 ** -0.5
NCH = T // 64


def dram_in(nc, name, shape, dt=F32):
    return nc.dram_tensor(name, list(shape), dt, kind="ExternalInput").ap()


def dram_out(nc, name, shape, dt=F32):
    return nc.dram_tensor(name, list(shape), dt, kind="ExternalOutput").ap()


def build_M():
    nc = bass.Bass("TRN2", target_bir_lowering=False)
    cc = dram_in(nc, "cc", [128, 8, 2])
    wa = dram_in(nc, "wa", [128, 8, 3072])
    ba = dram_in(nc, "ba", [128, 24])
    mo = dram_out(nc, "mo", [128, 24, 2])
    with ExitStack() as es:
        P = Prog(nc, es)
        ccs = P.sb([128, 8, 2], F32)
        was = P.sb([128, 8, 3072], F32)
        bas = P.sb([128, 24], F32)
        mos = P.sb([128, 24, 2], F32)
        pm = P.ps([128, 24, 2], F32)
        P.dma('sp', ccs[:], cc, w=['cc'])
        P.dma('sp', bas[:], ba, w=['ba'])
        for k in range(8):
            P.dma('sp', was[:, k, :], wa[:, k, :], w=['wa%d' % k])
        P.op('act', lambda e: e.activation(ccs[:], ccs[:], AF.Silu), r=['cc'], w=['cc'])
        for j in range(24):
            for k in range(8):
                P.op('pe', lambda e, j=j, k=k: e.matmul(pm[:, j, :], was[:, k, j * 128:(j + 1) * 128], ccs[:, k, :],
                                                       start=(k == 0), stop=(k == 7)),
                     r=['cc', 'wa%d' % k], w=['pm'])
        P.op('dve', lambda e: e.tensor_tensor(mos[:], pm[:], bas[:].unsqueeze(2).to_broadcast([128, 24, 2]), ALU.add),
             r=['pm', 'ba'], w=['mo'])
        P.dma('sp', mo, mos[:], r=['mo'])
        P.finish()
    return nc


A_QD, A_KVD, A_KR, A_KRP, A_MQ, A_MK, A_PG = 0, 384, 640, 672, 704, 960, 1216
A_TM = 1232
A_NCOL = A_TM + 256 + 512 + 512 + 2048


class Common:
    def __init__(self, P, nc):
        self.P = P
        self.nc = nc

    def load(self, q, dst, src, key, maxcols=None):
        self.P.dma(q, dst, src, w=[key])


def ln_rstd(P, var_ap, out_ap, rkeys, wkey, scale=1.0):
    P.op('act', lambda e: e.activation(out_ap, var_ap, AF.Ln, bias=EPS, scale=scale), r=rkeys, w=[wkey])
    P.op('act', lambda e: e.activation(out_ap, out_ap, AF.Exp, scale=-0.5), r=[wkey], w=[wkey])


def build_A():
    nc = bass.Bass("TRN2", target_bir_lowering=False)
    x = dram_in(nc, "x", [TC, D])
    mod1 = dram_in(nc, "mod1", [128, 8, 4])
    ident = dram_in(nc, "ident", [128, 128])
    ones_d = dram_in(nc, "ones", [128, 128])
    w_in = dram_in(nc, "w_in", [128, 8, A_NCOL])
    w_uq = dram_in(nc, "w_uq", [128, 3, 1024])
    w_uk = dram_in(nc, "w_uk", [128, 2, 512])
    w_uv = dram_in(nc, "w_uv", [128, 2, 512])
    gq_d = dram_in(nc, "gq", [128, 3])
    gkv_d = dram_in(nc, "gkv", [128, 2])
    bg_d = dram_in(nc, "bg", [16, 1])
    cos_d = dram_in(nc, "cos4", [128, TC])
    sin_d = dram_in(nc, "ssin4", [128, TC])
    QT = dram_out(nc, "QT", [6, 128, TC], BF16)
    KnT = dram_out(nc, "KnT", [4, 128, TC], BF16)
    KrT = dram_out(nc, "KrT", [32, TC], BF16)
    V = dram_out(nc, "V", [TC, 512], BF16)
    mqT = dram_out(nc, "mqT", [2, 128, TC], BF16)
    mkT = dram_out(nc, "mkT", [2, 128, TC], BF16)
    mkv = dram_out(nc, "mkv", [TC, 768], BF16)
    graw = dram_out(nc, "graw", [16, TC])
    glsg = dram_out(nc, "glsg", [16, TC])
    spo = dram_out(nc, "spo", [TC, 512], BF16)
    spm = dram_out(nc, "spm", [TC, 2048], BF16)
    with ExitStack() as es:
        P = Prog(nc, es)
        idt = P.sb([128, 128], F32)
        ones = P.sb([128, 128], F32)
        mods = P.sb([128, 8, 4], F32)
        win = P.sb([128, 8, A_NCOL], BF16)
        wuq = P.sb([128, 3, 1024], BF16)
        wuk = P.sb([128, 2, 512], BF16)
        wuv = P.sb([128, 2, 512], BF16)
        gq = P.sb([128, 3], F32)
        gkv = P.sb([128, 2], F32)
        bg = P.sb([16, 1], F32)
        cos4 = P.sb([128, TC], F32)
        sin4 = P.sb([128, TC], F32)
        hT = P.sb([128, 8, TC], BF16)
        xs = [P.sb([128, D], F32) for _ in range(2)]
        xn = [P.sb([128, D], F32) for _ in range(2)]
        st = [P.sb([128, 12], F32) for _ in range(2)]
        mv = [P.sb([128, 2], F32) for _ in range(2)]
        rs = [P.sb([128, 1], F32) for _ in range(2)]
        tp = P.ps([128, 8, 128], F32)
        pf = [P.ps([128, 512], F32) for _ in range(2)]
        pt = [P.ps([128, 512], F32) for _ in range(2)]
        pst = P.ps([128, 512], F32)
        dn = P.sb([128, 3, 512], F32)
        sq = P.sb([128, 3, 512], F32)
        rq = P.sb([128, 512], F32)
        dnn = P.sb([128, 3, 512], BF16)
        stg = [P.sb([128, 512], BF16) for _ in range(3)]
        t1b = P.sb([128, 512], BF16)
        t1 = P.sb([128, 512], F32)
        t2 = P.sb([128, 512], F32)
        g1 = P.sb([16, 512], F32)
        g2 = P.sb([16, 512], F32)
        g3 = P.sb([16, 512], F32)

        P.dma('sp', idt[:], ident, w=['idt'])
        P.dma('sp', ones[:], ones_d, w=['ones'])
        P.dma('sp', mods[:], mod1, w=['mods'])
        P.dma('sp', gq[:], gq_d, w=['gq'])
        P.dma('sp', gkv[:], gkv_d, w=['gkv'])
        P.dma('sp', bg[:], bg_d, w=['bg'])
        P.dma('sp', cos4[:], cos_d, w=['cos'])
        P.dma('sp', sin4[:], sin_d, w=['sin'])
        for k in range(8):
            for c0 in range(0, A_NCOL, 1520):
                P.dma('pool', win[:, k, c0:c0 + 1520], w_in[:, k, c0:c0 + 1520], w=['win'])
        for j in range(3):
            P.dma('pool', wuq[:, j, :], w_uq[:, j, :], w=['wuq'])
        for j in range(2):
            P.dma('pool', wuk[:, j, :], w_uk[:, j, :], w=['wuk'])
            P.dma('pool', wuv[:, j, :], w_uv[:, j, :], w=['wuv'])
        P.op('dve', lambda e: e.tensor_scalar(mods[:, :, 0:1], mods[:, :, 0:1], 1.0, None, ALU.add), r=['mods'], w=['mods'])
        P.op('dve', lambda e: e.tensor_scalar(mods[:, :, 2:3], mods[:, :, 2:3], 1.0, None, ALU.add), r=['mods'], w=['mods'])

        for i in range(NTILE):
            b = i % 2
            sel = 2 if i < 2 else 0
            P.dma('sp', xs[b][:], x[i * 128:(i + 1) * 128, :], w=['xs%d' % b])
            for hh in range(2):
                P.op('dve', lambda e, b=b, hh=hh: e.bn_stats(st[b][:, hh * 6:(hh + 1) * 6], xs[b][:, hh * 512:(hh + 1) * 512]),
                     r=['xs%d' % b], w=['st%d' % b])
            P.op('dve', lambda e, b=b: e.bn_aggr(mv[b][:], st[b][:]), r=['st%d' % b], w=['mv%d' % b])
            ln_rstd(P, mv[b][:, 1:2], rs[b][:], ['mv%d' % b], 'rs%d' % b)
            P.op('dve', lambda e, b=b: e.tensor_scalar(xn[b][:], xs[b][:], mv[b][:, 0:1], rs[b][:, 0:1], ALU.subtract, ALU.mult),
                 r=['xs%d' % b, 'mv%d' % b, 'rs%d' % b], w=['xn%d' % b])
            for k in range(8):
                P.op('pe', lambda e, b=b, k=k: e.transpose(tp[:, k, :], xn[b][:, k * 128:(k + 1) * 128], idt[:]),
                     r=['xn%d' % b, 'idt'], w=['tp%d' % (k // 4)])
            for k in range(8):
                P.op('act', lambda e, i=i, k=k, sel=sel: e.activation(
                    hT[:, k, i * 128:(i + 1) * 128], tp[:, k, :], AF.Identity,
                    bias=mods[:, k, sel + 1:sel + 2], scale=mods[:, k, sel:sel + 1]),
                    r=['tp%d' % (k // 4), 'mods'], w=['hT%d' % i])

        fmi = [0]

        def fm_mm(col0, m, s, n):
            bi = fmi[0] % 2
            fmi[0] += 1
            ps = pf[bi]
            tiles = ['hT%d' % t for t in range(s // 128, (s + n) // 128)]
            for k in range(8):
                P.op('pe', lambda e, k=k, ps=ps: e.matmul(ps[0:m, 0:n], win[:, k, col0:col0 + m], hT[:, k, s:s + n],
                                                          start=(k == 0), stop=(k == 7)),
                     r=['win'] + tiles, w=['pf%d' % bi])
            return ps, 'pf%d' % bi

        sgi = [0]

        def stage_out(ps, pkey, m, n, dst, func=AF.Copy, scale=1.0, bias=None):
            si = sgi[0] % 3
            sgi[0] += 1
            sg = stg[si]
            if bias is None:
                P.op('act', lambda e: e.activation(sg[0:m, 0:n], ps[0:m, 0:n], func, scale=scale),
                     r=[pkey], w=['stg%d' % si])
            else:
                P.op('act', lambda e: e.activation(sg[0:m, 0:n], ps[0:m, 0:n], func, bias=bias, scale=scale),
                     r=[pkey, 'bg'], w=['stg%d' % si])
            P.dma('sp', dst, sg[0:m, 0:n], r=['stg%d' % si])

        def rms_block(col0, nch, gvec, gkey, dim, s, n):
            for j in range(nch):
                ps, pk = fm_mm(col0 + j * 128, 128, s, n)
                P.op('act', lambda e, j=j, ps=ps: e.activation(dn[:, j, 0:n], ps[:, 0:n], AF.Copy), r=[pk], w=['dn%d' % j])
                P.op('act', lambda e, j=j, ps=ps: e.activation(sq[:, j, 0:n], ps[:, 0:n], AF.Square), r=[pk], w=['sq%d' % j])
            for j in range(nch):
                P.op('pe', lambda e, j=j: e.matmul(pst[:, 0:n], ones[:], sq[:, j, 0:n], start=(j == 0), stop=(j == nch - 1)),
                     r=['ones', 'sq%d' % j], w=['pst'])
            ln_rstd(P, pst[:, 0:n], rq[:, 0:n], ['pst'], 'rq', scale=1.0 / dim)
            for j in range(nch):
                P.op('dve', lambda e, j=j: e.scalar_tensor_tensor(dnn[:, j, 0:n], dn[:, j, 0:n], gvec[:, j:j + 1], rq[:, 0:n],
                                                                 ALU.mult, ALU.mult),
                     r=['dn%d' % j, gkey, 'rq'], w=['dnn%d' % j])

        def up_mm(wt, wkey, nch, col0, m, n, tok0=None, tm=False):
            bi = fmi[0] % 2
            fmi[0] += 1
            ps = pf[bi]
            for j in range(nch):
                if not tm:
                    P.op('pe', lambda e, j=j, ps=ps: e.matmul(ps[0:m, 0:n], wt[:, j, col0:col0 + m], dnn[:, j, 0:n],
                                                              start=(j == 0), stop=(j == nch - 1)),
                         r=[wkey, 'dnn%d' % j], w=['pf%d' % bi])
                else:
                    P.op('pe', lambda e, j=j, ps=ps: e.matmul(ps[:, 0:m], dnn[:, j, tok0:tok0 + 128], wt[:, j, col0:col0 + m],
                                                              start=(j == 0), stop=(j == nch - 1)),
                         r=[wkey, 'dnn%d' % j], w=['pf%d' % bi])
            return ps, 'pf%d' % bi

        def rope_out(psA, kA, psP, kP, m, s, n, dst):
            P.op('dve', lambda e: e.tensor_tensor(t1[0:m, 0:n], psA[0:m, 0:n], cos4[0:m, s:s + n], ALU.mult),
                 r=[kA, 'cos'], w=['t1'])
            P.op('dve', lambda e: e.tensor_tensor(t2[0:m, 0:n], psP[0:m, 0:n], sin4[0:m, s:s + n], ALU.mult),
                 r=[kP, 'sin'], w=['t2'])
            P.op('dve', lambda e: e.tensor_tensor(t1b[0:m, 0:n], t1[0:m, 0:n], t2[0:m, 0:n], ALU.add),
                 r=['t1', 't2'], w=['t1b'])
            P.dma('sp', dst, t1b[0:m, 0:n], r=['t1b'])

        for (s, n) in GROUPS:
            rms_block(A_QD, 3, gq, 'gq', 384, s, n)
            for oc in range(4):
                ps, pk = up_mm(wuq, 'wuq', 3, oc * 128, 128, n)
                stage_out(ps, pk, 128, n, QT[oc, :, s:s + n])
            for c in range(2):
                psA, kA = up_mm(wuq, 'wuq', 3, 512 + c * 128, 128, n)
                psP, kP = up_mm(wuq, 'wuq', 3, 768 + c * 128, 128, n)
                rope_out(psA, kA, psP, kP, 128, s, n, QT[4 + c, :, s:s + n])
            rms_block(A_KVD, 2, gkv, 'gkv', 256, s, n)
            for oc in range(4):
                ps, pk = up_mm(wuk, 'wuk', 2, oc * 128, 128, n)
                stage_out(ps, pk, 128, n, KnT[oc, :, s:s + n])
            for tt in range(n // 128):
                ps, pk = up_mm(wuv, 'wuv', 2, 0, 512, n, tok0=tt * 128, tm=True)
                stage_out(ps, pk, 128, 512, V[s + tt * 128:s + (tt + 1) * 128, :])
            psA, kA = fm_mm(A_KR, 32, s, n)
            psP, kP = fm_mm(A_KRP, 32, s, n)
            rope_out(psA, kA, psP, kP, 32, s, n, KrT[:, s:s + n])
            for c in range(2):
                ps, pk = fm_mm(A_MQ + c * 128, 128, s, n)
                stage_out(ps, pk, 128, n, mqT[c, :, s:s + n], scale=0.125)
            for c in range(2):
                ps, pk = fm_mm(A_MK + c * 128, 128, s, n)
                stage_out(ps, pk, 128, n, mkT[c, :, s:s + n])
            ps, pk = fm_mm(A_PG, 16, s, n)
            P.op('act', lambda e, ps=ps: e.activation(g1[:, 0:n], ps[0:16, 0:n], AF.Identity, bias=bg[:, 0:1], scale=1.0),
                 r=[pk, 'bg'], w=['g1'])
            P.dma('sp', graw[:, s:s + n], g1[:, 0:n], r=['g1'])
            P.op('act', lambda e: e.activation(g2[:, 0:n], g1[:, 0:n], AF.Exp, scale=-1.0), r=['g1'], w=['g2'])
            P.op('act', lambda e: e.activation(g3[:, 0:n], g2[:, 0:n], AF.Ln, bias=1.0, scale=1.0), r=['g2'], w=['g3'])
            P.op('act', lambda e: e.activation(g3[:, 0:n], g3[:, 0:n], AF.Copy, scale=-1.0), r=['g3'], w=['g3'])
            P.dma('sp', glsg[:, s:s + n], g3[:, 0:n], r=['g3'])

        tmi = [0]
        tm_groups = [(A_TM, 256, mkv, 0, AF.Copy), (A_TM + 256, 512, mkv, 256, AF.Copy),
                     (A_TM + 768, 512, spo, 0, AF.Sigmoid)] + \
                    [(A_TM + 1280 + q * 512, 512, spm, q * 512, AF.Sigmoid) for q in range(4)]
        for i in range(NTILE):
            for (c0, ncl, dst, dc0, func) in tm_groups:
                bi = tmi[0] % 2
                tmi[0] += 1
                ps = pt[bi]
                for k in range(8):
                    P.op('pe', lambda e, k=k, ps=ps, c0=c0, ncl=ncl, i=i: e.matmul(
                        ps[:, 0:ncl], hT[:, k, i * 128:(i + 1) * 128], win[:, k, c0:c0 + ncl],
                        start=(k == 0), stop=(k == 7)), r=['win', 'hT%d' % i], w=['pt%d' % bi])
                stage_out(ps, 'pt%d' % bi, 128, ncl, dst[i * 128:(i + 1) * 128, dc0:dc0 + ncl], func=func)
        P.finish()
    return nc


ROPE_PERM = np.concatenate([np.arange(8, 16), np.arange(0, 8), np.arange(24, 32), np.arange(16, 24)])
ROPE_SIGN = np.concatenate([-np.ones(8), np.ones(8), -np.ones(8), np.ones(8)]).astype(np.float32)


def rope_tables_np():
    rows = SEQ // 64
    row, col = np.meshgrid(np.arange(rows, dtype=np.float32), np.arange(64, dtype=np.float32), indexing='ij')
    row, col = row.reshape(-1), col.reshape(-1)
    half = 16
    inv = (np.float32(10000.0) ** (-np.arange(0, half, 2, dtype=np.float32) / np.float32(half))).astype(np.float32)
    ar, ac = row[:, None] * inv, col[:, None] * inv
    ang = np.concatenate([ar, ar, ac, ac], axis=-1)
    ang = np.concatenate([np.zeros((CTX, 32), np.float32), ang], axis=0).astype(np.float32)
    return np.cos(ang).astype(np.float32), (np.sin(ang) * ROPE_SIGN).astype(np.float32)


def core_tokens(i):
    return np.concatenate([np.arange(CTX), CTX + np.arange(i * LAT_C, (i + 1) * LAT_C)])


def kp(a, nk):
    return np.ascontiguousarray(a.reshape(nk, 128, -1).transpose(1, 0, 2))


def prep_A_weights(w_in, w_uq, w_uk, w_uv, g_qn, g_kvn, b_gates):
    kr = 640 + ROPE_PERM
    colsA = np.concatenate([np.arange(0, 384), np.arange(384, 640), np.arange(640, 672), kr,
                            np.arange(672, 928), np.arange(928, 1184), np.arange(2208, 2224),
                            np.arange(928, 1184), np.arange(1184, 1696), np.arange(1696, 2208),
                            np.arange(2224, 4272)])
    assert len(colsA) == A_NCOL
    hq = np.arange(8)[:, None] * 96
    nope = (hq + np.arange(64)[None, :]).reshape(-1)
    rope = (hq + 64 + np.arange(32)[None, :]).reshape(-1)
    ropep = (hq + 64 + ROPE_PERM[None, :]).reshape(-1)
    colsq = np.concatenate([nope, rope, ropep])
    return {
        "w_in": kp(w_in[:, colsA], 8),
        "w_uq": kp(w_uq[:, colsq], 3),
        "w_uk": kp(w_uk, 2),
        "w_uv": kp(w_uv, 2),
        "gq": np.ascontiguousarray(g_qn.reshape(3, 128).T),
        "gkv": np.ascontiguousarray(g_kvn.reshape(2, 128).T),
        "bg": np.ascontiguousarray(b_gates.reshape(16, 1)),
    }


NKT = T // 128
QGROUPS = [(0, 256, 2)] + [(CTX + g * 512, 512, NKT) for g in range(SEQ // 512)]


def build_B1():
    nc = bass.Bass("TRN2", target_bir_lowering=False)
    Qd = dram_in(nc, "Q", [96, T], BF16)
    Kd = dram_in(nc, "K", [96, T], BF16)
    Vd = dram_in(nc, "V", [128, NKT, 65], BF16)
    Od = dram_out(nc, "O", [T, 65])
    with ExitStack() as es:
        P = Prog(nc, es)
        Qs = P.sb([96, T], BF16)
        Ks = P.sb([96, T], BF16)
        Vs = P.sb([128, NKT, 65], BF16)
        pTs = [P.sb([128, 512], BF16) for _ in range(3)]
        ost = [P.sb([128, 4, 65], F32) for _ in range(2)]
        pss = [P.ps([128, 512], F32) for _ in range(3)]
        pso = [P.ps([128, 512], F32) for _ in range(4)]
        CW = 1280
        for c0 in range(0, T, CW):
            P.dma('sp', Ks[:, c0:c0 + CW], Kd[:, c0:c0 + CW], w=['K%d' % (c0 // CW)])
            P.dma('sp', Qs[:, c0:c0 + CW], Qd[:, c0:c0 + CW], w=['Q%d' % (c0 // CW)])
        for t0 in range(0, NKT, 26):
            P.dma('sp', Vs[:, t0:t0 + 26, :], Vd[:, t0:t0 + 26, :], w=['V%d' % (t0 // 26)])
        steps = [(gi, kt) for gi, (q0, nq, nkt) in enumerate(QGROUPS) for kt in range(nkt)]

        def mm1(si):
            gi, kt = steps[si]
            q0, nq, nkt = QGROUPS[gi]
            sb_ = si % 3
            qkeys = ['Q%d' % j for j in range(q0 // CW, (q0 + nq - 1) // CW + 1)]
            P.op('pe', lambda e: e.matmul(pss[sb_][:, 0:nq], Ks[:, kt * 128:(kt + 1) * 128], Qs[:, q0:q0 + nq],
                                          start=True, stop=True),
                 r=['K%d' % ((kt * 128) // CW)] + qkeys, w=['pss%d' % sb_])

        LA = 2
        for si in range(min(LA, len(steps))):
            mm1(si)
        for si, (gi, kt) in enumerate(steps):
            q0, nq, nkt = QGROUPS[gi]
            sb_ = si % 3
            if si + LA < len(steps):
                mm1(si + LA)
            P.op('act', lambda e, sb_=sb_, nq=nq: e.activation(pTs[sb_][:, 0:nq], pss[sb_][:, 0:nq], AF.Exp, scale=MLA_SCALE),
                 r=['pss%d' % sb_], w=['pT%d' % sb_])
            for qb in range(nq // 128):
                P.op('pe', lambda e, sb_=sb_, kt=kt, qb=qb, nkt=nkt: e.matmul(
                    pso[qb][:, 0:65], pTs[sb_][:, qb * 128:(qb + 1) * 128], Vs[:, kt, :],
                    start=(kt == 0), stop=(kt == nkt - 1)),
                    r=['V%d' % (kt // 26), 'pT%d' % sb_], w=['pso%d' % qb])
            if kt == nkt - 1:
                ob = gi % 2
                nb = nq // 128
                for qb in range(nb):
                    P.op('dve', lambda e, ob=ob, qb=qb: e.tensor_copy(ost[ob][:, qb, :], pso[qb][:, 0:65]),
                         r=['pso%d' % qb], w=['ost%d' % ob])
                P.dma('sp', Od[q0:q0 + nq, :].rearrange("(b p) c -> p b c", p=128), ost[ob][:, 0:nb, :], r=['ost%d' % ob])
        P.finish()
    return nc


CB = 20
NBLK = NCH // CB
RING = 8


def build_B2():
    nc = bass.Bass("TRN2", target_bir_lowering=False)
    qTd = dram_in(nc, "qT", [64, T], BF16)
    kTd = dram_in(nc, "kT", [64, T], BF16)
    ktd = dram_in(nc, "kt", [64, NCH, 64], BF16)
    vxd = dram_in(nc, "vx", [64, NCH, 129], BF16)
    igd = dram_in(nc, "ig", [64, NCH])
    lfd = dram_in(nc, "lf", [64, NCH])
    trid = dram_in(nc, "tri", [64, 64])
    oned = dram_in(nc, "ones", [64, 64])
    H = dram_out(nc, "H", [64, NCH, 128])
    with ExitStack() as es:
        P = Prog(nc, es)
        qT = P.sb([64, T], BF16)
        kT = P.sb([64, T], BF16)
        kt = P.sb([64, NCH, 64], BF16)
        ig = P.sb([64, NCH], F32)
        lf = P.sb([64, NCH], F32)
        tri = P.sb([64, 64], F32)
        ones = P.sb([64, 64], F32)
        bb = P.sb([64, NCH], F32)
        ebl = P.sb([64, NCH], F32)
        ek = P.sb([64, NCH], F32)
        ek2 = P.sb([64, NCH], F32)
        eb = P.sb([64, NCH], F32)
        tmp = P.sb([64, NCH], F32)
        vx = [P.sb([64, CB, 129], BF16)] * 2
        v1 = [P.sb([64, CB, 129], BF16) for _ in range(2)]
        v2 = [P.sb([64, CB, 129], BF16) for _ in range(2)]
        Us = [P.sb([64, CB, 129], F32) for _ in range(2)]
        MTb = [P.sb([64, CB, 64], BF16) for _ in range(2)]
        hr = [P.sb([64, CB, 129], F32) for _ in range(2)]
        ho = [P.sb([64, CB, 128], F32)] * 2
        tn = [P.sb([64, CB], F32) for _ in range(2)]
        Cst = [P.sb([64, 129], F32) for _ in range(3)]
        Cbf = [P.sb([64, 129], BF16) for _ in range(RING)]
        psS = [P.ps([64, 512], F32) for _ in range(2)]
        psU = [P.ps([64, 512], F32) for _ in range(2)]
        ps_h = [P.ps([64, 512], F32) for _ in range(3)]
        pb = psS[0]
        pbl = psU[0]
        CW = 1280
        P.dma('sp', ig[:], igd, w=['ig'])
        P.dma('sp', lf[:], lfd, w=['lf'])
        P.dma('sp', tri[:], trid, w=['tri'])
        P.dma('sp', ones[:], oned, w=['ones'])
        for b in range(NBLK):
            c0 = b * CW
            P.dma('sp', qT[:, c0:c0 + CW], qTd[:, c0:c0 + CW], w=['qT%d' % b])
            P.dma('sp', kT[:, c0:c0 + CW], kTd[:, c0:c0 + CW], w=['kT%d' % b])
            P.dma('sp', kt[:, b * CB:(b + 1) * CB, :], ktd[:, b * CB:(b + 1) * CB, :], w=['kt%d' % b])
        P.op('pe', lambda e: e.matmul(pb[:, 0:NCH], tri[:], lf[:], start=True, stop=True), r=['tri', 'lf'], w=['psS0'])
        P.op('pe', lambda e: e.matmul(pbl[:, 0:NCH], ones[:], lf[:], start=True, stop=True), r=['ones', 'lf'], w=['psU0'])
        P.op('dve', lambda e: e.tensor_copy(bb[:], pb[:, 0:NCH]), r=['psS0'], w=['bb'])
        P.op('act', lambda e: e.activation(ebl[:], pbl[:, 0:NCH], AF.Exp), r=['psU0'], w=['ebl'])
        P.op('act', lambda e: e.activation(eb[:], bb[:], AF.Exp), r=['bb'], w=['eb'])
        P.op('dve', lambda e: e.tensor_tensor(tmp[:], ig[:], bb[:], ALU.subtract), r=['ig', 'bb'], w=['tmp'])
        P.op('act', lambda e: e.activation(ek[:], tmp[:], AF.Exp), r=['tmp'], w=['ek'])
        P.op('dve', lambda e: e.tensor_tensor(tmp[:], tmp[:], pbl[:, 0:NCH], ALU.add), r=['tmp', 'psU0', 'ek'], w=['tmp2'])
        P.op('act', lambda e: e.activation(ek2[:], tmp[:], AF.Exp), r=['tmp2'], w=['ek2'])
        P.op('dve', lambda e: e.memset(Cst[0][:], 0.0), w=['Cst0'])
        P.op('dve', lambda e: e.memset(Cbf[0][:], 0.0), w=['Cbf0'])
        sct = [0]
        uct = [0]

        def batch(b):
            s = b % 2
            P.dma('sp', vx[s][:], vxd[:, b * CB:(b + 1) * CB, :], w=['vx'])
            P.op('dve', lambda e: e.tensor_tensor(v1[s][:], vx[s][:],
                                                  ek[:, b * CB:(b + 1) * CB].unsqueeze(2).to_broadcast([64, CB, 129]), ALU.mult),
                 r=['vx', 'ek'], w=['v1_%d' % s])
            P.op('dve', lambda e: e.tensor_tensor(v2[s][:], vx[s][:],
                                                  ek2[:, b * CB:(b + 1) * CB].unsqueeze(2).to_broadcast([64, CB, 129]), ALU.mult),
                 r=['vx', 'ek2'], w=['v2_%d' % s])
            for ci0 in range(0, CB, 8):
                nb = min(8, CB - ci0)
                bk = sct[0] % 2
                sct[0] += 1
                for j in range(nb):
                    c = b * CB + ci0 + j
                    P.op('pe', lambda e, c=c, j=j, bk=bk: e.matmul(psS[bk][:, j * 64:(j + 1) * 64], kT[:, c * 64:(c + 1) * 64],
                                                                 qT[:, c * 64:(c + 1) * 64], start=True, stop=True),
                         r=['qT%d' % ((c * 64) // CW), 'kT%d' % ((c * 64) // CW)], w=['psS%d' % bk])
                P.op('dve', lambda e, ci0=ci0, nb=nb, bk=bk: e.tensor_tensor(
                    MTb[s][:, ci0:ci0 + nb, :], psS[bk][:, 0:nb * 64].rearrange("p (c j) -> p c j", c=nb),
                    tri[:].unsqueeze(1).to_broadcast([64, nb, 64]), ALU.mult),
                    r=['psS%d' % bk, 'tri'], w=['MT%d' % s])
            for ci0 in range(0, CB, 3):
                nb = min(3, CB - ci0)
                bk = uct[0] % 2
                uct[0] += 1
                for j in range(nb):
                    c = b * CB + ci0 + j
                    P.op('pe', lambda e, c=c, j=j, bk=bk, ci0=ci0: e.matmul(psU[bk][:, j * 129:(j + 1) * 129], kt[:, c, :],
                                                                          v2[s][:, ci0 + j, :], start=True, stop=True),
                         r=['kt%d' % b, 'v2_%d' % s], w=['psU%d' % bk])
                P.op('act', lambda e, ci0=ci0, nb=nb, bk=bk: e.activation(
                    Us[s][:, ci0:ci0 + nb, :], psU[bk][:, 0:nb * 129].rearrange("p (c j) -> p c j", c=nb), AF.Copy),
                    r=['psU%d' % bk], w=['Us%d' % s])

        batch(0)
        for b in range(NBLK):
            s = b % 2
            if b + 1 < NBLK:
                batch(b + 1)
            for ci in range(CB):
                c = b * CB + ci
                hb_ = c % 3
                P.op('pe', lambda e, ci=ci, hb_=hb_, s=s: e.matmul(ps_h[hb_][:, 0:129], MTb[s][:, ci, :], v1[s][:, ci, :], start=True, stop=False),
                     r=['MT%d' % s, 'v1_%d' % s], w=['ps_h%d' % hb_])
                P.op('pe', lambda e, c=c, hb_=hb_: e.matmul(ps_h[hb_][:, 0:129], qT[:, c * 64:(c + 1) * 64], Cbf[c % RING][:], start=False, stop=True),
                     r=['qT%d' % ((c * 64) // CW), 'Cbf%d' % (c % RING)], w=['ps_h%d' % hb_])
                P.op('act', lambda e, ci=ci, hb_=hb_, s=s: e.activation(hr[s][:, ci, :], ps_h[hb_][:, 0:129], AF.Copy),
                     r=['ps_h%d' % hb_], w=['hr%d' % s])
                if c + 1 < NCH:
                    P.op('dve', lambda e, c=c, ci=ci, s=s: e.scalar_tensor_tensor(Cst[(c + 1) % 3][:], Cst[c % 3][:], ebl[:, c:c + 1],
                                                                         Us[s][:, ci, :], ALU.mult, ALU.add),
                         r=['Cst%d' % (c % 3), 'ebl', 'Us%d' % s], w=['Cst%d' % ((c + 1) % 3)])
                    P.op('dve', lambda e, c=c, ci=ci, s=s: e.scalar_tensor_tensor(Cbf[(c + 1) % RING][:], Cst[c % 3][:], ebl[:, c:c + 1],
                                                                              Us[s][:, ci, :], ALU.mult, ALU.add),
                         r=['Cst%d' % (c % 3), 'ebl', 'Us%d' % s], w=['Cbf%d' % ((c + 1) % RING)])
            sl = slice(b * CB, (b + 1) * CB)
            P.op('dve', lambda e, sl=sl, s=s: e.tensor_tensor(tn[s][:], hr[s][:, :, 128], eb[:, sl], ALU.mult),
                 r=['hr%d' % s, 'eb'], w=['tn%d' % s])
            P.op('act', lambda e, s=s: e.activation(tn[s][:], tn[s][:], AF.Abs), r=['tn%d' % s], w=['tn%d' % s])
            P.op('dve', lambda e, s=s: e.tensor_scalar(tn[s][:], tn[s][:], 1.0, None, ALU.max), r=['tn%d' % s], w=['tn%d' % s])
            P.op('dve', lambda e, s=s: e.reciprocal(tn[s][:], tn[s][:]), r=['tn%d' % s], w=['tn%d' % s])
            P.op('dve', lambda e, sl=sl, s=s: e.tensor_tensor(tn[s][:], tn[s][:], eb[:, sl], ALU.mult),
                 r=['tn%d' % s, 'eb'], w=['tn%d' % s])
            P.op('dve', lambda e, s=s: e.tensor_tensor(ho[s][:], hr[s][:, :, 0:128],
                                                  tn[s][:].unsqueeze(2).to_broadcast([64, CB, 128]), ALU.mult),
                 r=['hr%d' % s, 'tn%d' % s], w=['ho'])
            P.dma('sp', H[:, sl, :], ho[s][:], r=['ho'])
        P.finish()
    return nc


def ln_tile(P, z, zkey, st, mv, rs, sfx, gB=None, bB=None, gkeys=()):
    for hh in range(2):
        P.op('dve', lambda e, hh=hh: e.bn_stats(st[:, hh * 6:(hh + 1) * 6], z[:, hh * 512:(hh + 1) * 512]),
             r=[zkey], w=['st' + sfx])
    P.op('dve', lambda e: e.bn_aggr(mv[:], st[:]), r=['st' + sfx], w=['mv' + sfx])
    ln_rstd(P, mv[:, 1:2], rs[:], ['mv' + sfx], 'rs' + sfx)
    P.op('dve', lambda e: e.tensor_scalar(z[:], z[:], mv[:, 0:1], rs[:, 0:1], ALU.subtract, ALU.mult),
         r=[zkey, 'mv' + sfx, 'rs' + sfx], w=[zkey])
    if gB is not None:
        P.op('dve', lambda e: e.tensor_tensor(z[:], z[:], gB[:], ALU.mult), r=[zkey] + list(gkeys), w=[zkey])
        P.op('dve', lambda e: e.tensor_tensor(z[:], z[:], bB[:], ALU.add), r=[zkey] + list(gkeys), w=[zkey])


def transpose_to(P, src, skey, nch, tp, idt, dst_fn, dkeys, evac):
    for k in range(nch):
        P.op('pe', lambda e, k=k: e.transpose(tp[:, k, :], src[:, k * 128:(k + 1) * 128], idt[:]),
             r=[skey, 'idt'], w=['tp%d' % (k // 4)])
    for k in range(nch):
        evac(k, tp[:, k, :], 'tp%d' % (k // 4))


def build_C1():
    nc = bass.Bass("TRN2", target_bir_lowering=False)
    xd = dram_in(nc, "x", [TC, D])
    od = dram_in(nc, "o", [TC, 8, 65])
    hfd = dram_in(nc, "hf", [TC, 512])
    hbd = dram_in(nc, "hb", [TC, 512])
    spod = dram_in(nc, "spo", [TC, 512], BF16)
    spmd = dram_in(nc, "spm", [TC, 2048], BF16)
    ident = dram_in(nc, "ident", [128, 128])
    wmla_d = dram_in(nc, "wmla", [128, 4, D])
    wmls_d = dram_in(nc, "wmls", [128, 4, D])
    wout_d = dram_in(nc, "wout", [128, 8, D])
    gmh_d = dram_in(nc, "gmhB", [128, 512])
    g1_d = dram_in(nc, "g1B", [128, 2, D])
    lng_d = dram_in(nc, "lngB", [128, D])
    lnb_d = dram_in(nc, "lnbB", [128, D])
    xo = dram_out(nc, "xo", [TC, D])
    with ExitStack() as es:
        P = Prog(nc, es)
        idt = P.sb([128, 128], F32)
        wmla = P.sb([128, 4, D], BF16)
        wmls = P.sb([128, 4, D], BF16)
        wout = P.sb([128, 8, D], BF16)
        gmh = P.sb([128, 512], F32)
        g1B = P.sb([128, 2, D], F32)
        lng = P.sb([128, D], F32)
        lnb = P.sb([128, D], F32)
        xs = [P.sb([128, D], F32) for _ in range(2)]
        os_ = [P.sb([128, 8, 65], F32) for _ in range(2)]
        hf = [P.sb([128, 512], F32) for _ in range(2)]
        hb = [P.sb([128, 512], F32) for _ in range(2)]
        po = [P.sb([128, 512], BF16) for _ in range(2)]
        pm = [P.sb([128, 2048], BF16) for _ in range(2)]
        rec = [P.sb([128, 8], F32) for _ in range(2)]
        on = [P.sb([128, 512], F32) for _ in range(2)]
        onT = [P.sb([128, 4, 128], BF16) for _ in range(2)]
        hnT = [P.sb([128, 4, 128], BF16) for _ in range(2)]
        ymT = P.sb([128, 8, 128], BF16)
        s4 = [P.sb([128, 4], F32) for _ in range(2)]
        v4 = [P.sb([128, 4], F32) for _ in range(2)]
        cen = [P.sb([128, 512], F32) for _ in range(2)]
        sqv = [P.sb([128, 512], F32) for _ in range(2)]
        ta = P.sb([128, D], F32)
        tb = P.sb([128, D], F32)
        st = P.sb([128, 12], F32)
        mv = P.sb([128, 2], F32)
        rs = P.sb([128, 1], F32)
        tp = P.ps([128, 8, 128], F32)
        pA = P.ps([128, 2, 512], F32)
        pB = P.ps([128, 2, 512], F32)
        pY = P.ps([128, 2, 512], F32)
        P.dma('sp', idt[:], ident, w=['idt'])
        P.dma('sp', gmh[:], gmh_d, w=['gmh'])
        P.dma('sp', g1B[:], g1_d, w=['g1B'])
        P.dma('sp', lng[:], lng_d, w=['lng'])
        P.dma('sp', lnb[:], lnb_d, w=['lng'])
        for k in range(4):
            P.dma('pool', wmla[:, k, :], wmla_d[:, k, :], w=['wmla'])
            P.dma('pool', wmls[:, k, :], wmls_d[:, k, :], w=['wmls'])
        for k in range(8):
            P.dma('pool', wout[:, k, :], wout_d[:, k, :], w=['wout'])

        def evac_to(dstT, dkey):
            def f(k, src, skey):
                P.op('act', lambda e: e.activation(dstT[:, k, :], src, AF.Copy), r=[skey], w=[dkey])
            return f

        def proj(ps, pskey, lT, lkey, w, wkey, nk):
            for half in range(2):
                for k in range(nk):
                    P.op('pe', lambda e, half=half, k=k: e.matmul(ps[:, half, :], lT[:, k, :], w[:, k, half * 512:(half + 1) * 512],
                                                                 start=(k == 0), stop=(k == nk - 1)),
                         r=[lkey, wkey], w=[pskey + str(half)])

        def stageX(i):
            b = i % 2
            rows = slice(i * 128, (i + 1) * 128)
            P.dma('sp', os_[b][:], od[rows], w=['o%d' % b])
            P.dma('sp', hf[b][:], hfd[rows], w=['hf%d' % b])
            P.dma('sp', hb[b][:], hbd[rows], w=['hb%d' % b])
            P.dma('sp', po[b][:], spod[rows], w=['po%d' % b])
            P.dma('sp', pm[b][:], spmd[rows], w=['pm%d' % b])
            P.dma('sp', xs[b][:], xd[rows], w=['xs%d' % b])
            on_, onT_, hnT_, cen_, sqv_, rec_, s4_, v4_ = on[b], onT[b], hnT[b], cen[b], sqv[b], rec[b], s4[b], v4[b]
            kb = str(b)
            P.op('dve', lambda e: e.reciprocal(rec_[:], os_[b][:, :, 64]), r=['o' + kb], w=['rec' + kb])
            P.op('dve', lambda e: e.tensor_tensor(on_[:].rearrange("p (h d) -> p h d", h=8), os_[b][:, :, 0:64],
                                                  rec_[:].unsqueeze(2).to_broadcast([128, 8, 64]), ALU.mult),
                 r=['o' + kb, 'rec' + kb], w=['on' + kb])
            transpose_to(P, on_, 'on' + kb, 4, tp, idt, None, None, evac_to(onT_, 'onT' + kb))
            P.op('dve', lambda e: e.tensor_tensor(cen_[:], hf[b][:], hb[b][:], ALU.add), r=['hf' + kb, 'hb' + kb], w=['cen' + kb])
            c3 = cen_[:].rearrange("p (h d) -> p h d", h=4)
            P.op('dve', lambda e: e.tensor_reduce(s4_[:], c3, AX.X, ALU.add), r=['cen' + kb], w=['s4' + kb])
            P.op('dve', lambda e: e.tensor_scalar(s4_[:], s4_[:], -1.0 / 128, None, ALU.mult), r=['s4' + kb], w=['s4' + kb])
            P.op('dve', lambda e: e.tensor_tensor(c3, c3, s4_[:].unsqueeze(2).to_broadcast([128, 4, 128]), ALU.add),
                 r=['cen' + kb, 's4' + kb], w=['cen' + kb])
            P.op('dve', lambda e: e.tensor_tensor(sqv_[:], cen_[:], cen_[:], ALU.mult), r=['cen' + kb], w=['sqv' + kb])
            P.op('dve', lambda e: e.tensor_reduce(v4_[:], sqv_[:].rearrange("p (h d) -> p h d", h=4), AX.X, ALU.add),
                 r=['sqv' + kb], w=['v4' + kb])
            ln_rstd(P, v4_[:], v4_[:], ['v4' + kb], 'v4' + kb, scale=1.0 / 128)
            P.op('dve', lambda e: e.tensor_tensor(c3, c3, v4_[:].unsqueeze(2).to_broadcast([128, 4, 128]), ALU.mult),
                 r=['cen' + kb, 'v4' + kb], w=['cen' + kb])
            P.op('dve', lambda e: e.tensor_tensor(cen_[:], cen_[:], gmh[:], ALU.mult), r=['cen' + kb, 'gmh'], w=['cen' + kb])
            P.op('dve', lambda e: e.tensor_tensor(cen_[:], cen_[:], po[b][:], ALU.mult), r=['cen' + kb, 'po' + kb], w=['cen' + kb])
            transpose_to(P, cen_, 'cen' + kb, 4, tp, idt, None, None, evac_to(hnT_, 'hnT' + kb))

        def stageY(i):
            b = i % 2
            kb = str(b)
            sel = 1 if i < 2 else 0
            rows = slice(i * 128, (i + 1) * 128)
            proj(pA, 'pA', onT[b], 'onT' + kb, wmla, 'wmla', 4)
            proj(pB, 'pB', hnT[b], 'hnT' + kb, wmls, 'wmls', 4)
            for half in range(2):
                hs = slice(half * 512, (half + 1) * 512)
                P.op('dve', lambda e, half=half, hs=hs: e.tensor_tensor(ta[:, hs], pA[:, half, :], pm[b][:, hs], ALU.mult),
                     r=['pA%d' % half, 'pm' + kb], w=['ta'])
                P.op('dve', lambda e, half=half, hs=hs: e.tensor_tensor(
                    tb[:, hs], pB[:, half, :], pm[b][:, 1024 + half * 512:1024 + (half + 1) * 512], ALU.mult),
                    r=['pB%d' % half, 'pm' + kb], w=['tb'])
            P.op('dve', lambda e: e.tensor_tensor(ta[:], ta[:], tb[:], ALU.add), r=['ta', 'tb'], w=['ta'])
            transpose_to(P, ta, 'ta', 8, tp, idt, None, None, evac_to(ymT, 'ymT'))
            proj(pY, 'pY', ymT, 'ymT', wout, 'wout', 8)
            for half in range(2):
                hs = slice(half * 512, (half + 1) * 512)
                P.op('dve', lambda e, half=half, hs=hs: e.tensor_tensor(tb[:, hs], pY[:, half, :], g1B[:, sel, hs], ALU.mult),
                     r=['pY%d' % half, 'g1B'], w=['tb'])
            P.op('dve', lambda e: e.scalar_tensor_tensor(tb[:], xs[b][:], ALPHA, tb[:], ALU.mult, ALU.add),
                 r=['xs' + kb, 'tb'], w=['tb'])
            ln_tile(P, tb, 'tb', st, mv, rs, '', lng, lnb, ['lng'])
            P.dma('sp', xo[rows], tb[:], r=['tb'])

        stageX(0)
        for i in range(NTILE):
            if i + 1 < NTILE:
                stageX(i + 1)
            stageY(i)
        P.finish()
    return nc


def bcast(v, p=128):
    return np.ascontiguousarray(np.broadcast_to(np.asarray(v, np.float32).reshape(1, -1), (p, np.asarray(v).size)))


def prep_C1_weights(w_bo_mla, w_bo_mlstm, w_out, g_mh, ln1_g, ln1_b, g1_lat, g1_ctx):
    return {"wmla": kp(w_bo_mla, 4), "wmls": kp(w_bo_mlstm, 4), "wout": kp(w_out, 8),
            "gmhB": bcast(g_mh), "lngB": bcast(ln1_g), "lnbB": bcast(ln1_b),
            "g1B": np.ascontiguousarray(np.stack([bcast(g1_lat), bcast(g1_ctx)], axis=1)),
            "ident": np.eye(128, dtype=np.float32)}


NEXP = 32
DEXP = 256


def build_C2():
    nc = bass.Bass("TRN2", target_bir_lowering=False)
    xd = dram_in(nc, "x", [TC, D])
    ident = dram_in(nc, "ident", [128, 128])
    mod2 = dram_in(nc, "mod2", [128, 8, 4])
    wr_d = dram_in(nc, "wr", [128, 8, 36])
    br_d = dram_in(nc, "brB", [128, 36])
    g2_d = dram_in(nc, "g2B", [128, 2, D])
    lng_d = dram_in(nc, "lngB", [128, D])
    lnb_d = dram_in(nc, "lnbB", [128, D])
    wg_d = dram_in(nc, "wg", [NEXP, 128, 8, DEXP])
    wu_d = dram_in(nc, "wu", [NEXP, 128, 8, DEXP])
    wd_d = dram_in(nc, "wd", [NEXP, 128, 2, D])
    xo = dram_out(nc, "xo", [TC, D])
    with ExitStack() as es:
        P = Prog(nc, es)
        idt = P.sb([128, 128], F32)
        mods = P.sb([128, 8, 4], F32)
        wr = P.sb([128, 8, 36], F32)
        brB = P.sb([128, 36], F32)
        g2B = P.sb([128, 2, D], F32)
        lng = P.sb([128, D], F32)
        lnb = P.sb([128, D], F32)
        h2T = P.sb([128, 8, TC], BF16)
        hTf = P.sb([128, 8, 128], F32)
        yacc = P.sb([128, NTILE, D], F32)
        comb_all = P.sb([128, NTILE, 32], F32)
        wg = [P.sb([128, 8, DEXP], BF16) for _ in range(2)]
        wu = [P.sb([128, 8, DEXP], BF16) for _ in range(2)]
        wd = [P.sb([128, 2, D], BF16) for _ in range(2)]
        xs = [P.sb([128, D], F32) for _ in range(2)]
        st = P.sb([128, 12], F32)
        mv = P.sb([128, 2], F32)
        rs = P.sb([128, 1], F32)
        lg = P.sb([128, 36], F32)
        sm = [P.sb([128, 8], F32, name="sm%d" % j) for j in range(12)]
        selg = P.sb([128, 4, 8], F32)
        sa = [P.sb([128, 512], F32) for _ in range(2)]
        hid = [P.sb([128, 2, 512], BF16) for _ in range(2)]
        tp = P.ps([128, 8, 128], F32)
        pA = [P.ps([128, 512], F32) for _ in range(2)]
        pU = [P.ps([128, 512], F32) for _ in range(2)]
        pY = [P.ps([128, 512], F32) for _ in range(2)]
        pC = pY[0]
        P.dma('sp', idt[:], ident, w=['idt'])
        P.dma('sp', mods[:], mod2, w=['mods'])
        P.dma('sp', wr[:], wr_d, w=['wr'])
        P.dma('sp', brB[:], br_d, w=['brB'])
        P.dma('sp', g2B[:], g2_d, w=['g2B'])
        P.dma('sp', lng[:], lng_d, w=['lng'])
        P.dma('sp', lnb[:], lnb_d, w=['lng'])
        P.op('dve', lambda e: e.tensor_scalar(mods[:, :, 0:1], mods[:, :, 0:1], 1.0, None, ALU.add), r=['mods'], w=['mods'])
        P.op('dve', lambda e: e.tensor_scalar(mods[:, :, 2:3], mods[:, :, 2:3], 1.0, None, ALU.add), r=['mods'], w=['mods'])

        def load_expert(e_):
            s = e_ % 2
            for k0 in range(0, 8, 4):
                P.dma('pool', wg[s][:, k0:k0 + 4, :], wg_d[e_, :, k0:k0 + 4, :], w=['wg%d' % s])
                P.dma('pool', wu[s][:, k0:k0 + 4, :], wu_d[e_, :, k0:k0 + 4, :], w=['wu%d' % s])
            for f in range(2):
                P.dma('pool', wd[s][:, f, :], wd_d[e_, :, f, :], w=['wd%d' % s])

        load_expert(0)
        for i in range(NTILE):
            b = i % 2
            msel = 2 if i < 2 else 0
            rows = slice(i * 128, (i + 1) * 128)
            P.dma('sp', xs[b][:], xd[rows], w=['xs%d' % b])
            ln_tile(P, xs[b], 'xs%d' % b, st, mv, rs, '')

            def evac(k, src, skey, i=i, msel=msel):
                P.op('act', lambda e: e.activation(h2T[:, k, i * 128:(i + 1) * 128], src, AF.Identity,
                                                   bias=mods[:, k, msel + 1:msel + 2], scale=mods[:, k, msel:msel + 1]),
                     r=[skey, 'mods'], w=['h2T%d' % i])
                P.op('act', lambda e: e.activation(hTf[:, k, :], src, AF.Identity,
                                                   bias=mods[:, k, msel + 1:msel + 2], scale=mods[:, k, msel:msel + 1]),
                     r=[skey, 'mods'], w=['hTf'])
            transpose_to(P, xs[b], 'xs%d' % b, 8, tp, idt, None, None, evac)
            for k in range(8):
                P.op('pe', lambda e, k=k: e.matmul(pC[:, 0:36], hTf[:, k, :], wr[:, k, :], start=(k == 0), stop=(k == 7)),
                     r=['hTf', 'wr'], w=['pY0'])
            P.op('dve', lambda e: e.tensor_tensor(lg[:], pC[:, 0:36], brB[:], ALU.add), r=['pY0', 'brB'], w=['lg'])
            gmax, ohg, negm, eg, sume, lsel, m1, mk1 = [sm[j] for j in range(8)]
            l2, m2, mk2, tt = sm[8], sm[9], sm[10], sm[11]
            lgG = lg[:, 0:4]
            lgE = lg[:, 4:36].rearrange("p (g e) -> p g e", g=4)

            def dv(fn, r, w):
                P.op('dve', fn, r=r, w=w)
            dv(lambda e: e.tensor_reduce(gmax[:, 0:1], lgG, AX.X, ALU.max), ['lg'], ['gmax'])
            dv(lambda e: e.tensor_scalar(ohg[:, 0:4], lgG, gmax[:, 0:1], None, ALU.is_equal), ['lg', 'gmax'], ['ohg'])
            dv(lambda e: e.tensor_scalar(negm[:, 0:1], gmax[:, 0:1], -1.0, None, ALU.mult), ['gmax'], ['negm'])
            P.op('act', lambda e: e.activation(eg[:, 0:4], lgG, AF.Exp, bias=negm[:, 0:1], scale=1.0), r=['lg', 'negm'], w=['eg'])
            dv(lambda e: e.tensor_reduce(sume[:, 0:1], eg[:, 0:4], AX.X, ALU.add), ['eg'], ['sume'])
            dv(lambda e: e.reciprocal(sume[:, 0:1], sume[:, 0:1]), ['sume'], ['sume'])
            dv(lambda e: e.tensor_tensor(selg[:], lgE, ohg[:, 0:4].unsqueeze(2).to_broadcast([128, 4, 8]), ALU.mult),
               ['lg', 'ohg'], ['selg'])
            dv(lambda e: e.tensor_reduce(lsel[:], selg[:].rearrange("p g e -> p e g"), AX.X, ALU.add), ['selg'], ['lsel'])
            dv(lambda e: e.tensor_reduce(m1[:, 0:1], lsel[:], AX.X, ALU.max), ['lsel'], ['m1'])
            dv(lambda e: e.tensor_scalar(mk1[:], lsel[:], m1[:, 0:1], None, ALU.is_equal), ['lsel', 'm1'], ['mk1'])
            dv(lambda e: e.scalar_tensor_tensor(l2[:], mk1[:], -1e30, lsel[:], ALU.mult, ALU.add), ['mk1', 'lsel'], ['l2'])
            dv(lambda e: e.tensor_reduce(m2[:, 0:1], l2[:], AX.X, ALU.max), ['l2'], ['m2'])
            dv(lambda e: e.tensor_scalar(mk2[:], l2[:], m2[:, 0:1], None, ALU.is_equal), ['l2', 'm2'], ['mk2'])
            dv(lambda e: e.tensor_tensor(tt[:, 0:1], m2[:, 0:1], m1[:, 0:1], ALU.subtract), ['m2', 'm1'], ['tt'])
            P.op('act', lambda e: e.activation(tt[:, 1:2], tt[:, 0:1], AF.Exp), r=['tt'], w=['tt'])
            dv(lambda e: e.tensor_scalar(tt[:, 2:3], tt[:, 1:2], 1.0, None, ALU.add), ['tt'], ['tt'])
            dv(lambda e: e.reciprocal(tt[:, 2:3], tt[:, 2:3]), ['tt'], ['tt'])
            dv(lambda e: e.tensor_tensor(tt[:, 3:4], tt[:, 2:3], sume[:, 0:1], ALU.mult), ['tt', 'sume'], ['tt'])
            dv(lambda e: e.tensor_tensor(tt[:, 4:5], tt[:, 3:4], tt[:, 1:2], ALU.mult), ['tt'], ['tt'])
            dv(lambda e: e.tensor_scalar(mk1[:], mk1[:], tt[:, 3:4], None, ALU.mult), ['mk1', 'tt'], ['mk1'])
            dv(lambda e: e.scalar_tensor_tensor(mk2[:], mk2[:], tt[:, 4:5], mk1[:], ALU.mult, ALU.add), ['mk2', 'tt', 'mk1'], ['mk2'])
            dv(lambda e, i=i: e.tensor_tensor(comb_all[:, i, :].rearrange("p (g e) -> p g e", g=4),
                                              ohg[:, 0:4].unsqueeze(2).to_broadcast([128, 4, 8]),
                                              mk2[:].unsqueeze(1).to_broadcast([128, 4, 8]), ALU.mult), ['ohg', 'mk2'], ['comb%d' % i])

        stepn = [0]
        pending = [None]
        for e_ in range(NEXP):
            s = e_ % 2
            if pending[0] is not None:
                pending[0]()
                pending[0] = None
            if e_ + 1 < NEXP:
                load_expert(e_ + 1)
            for (t0, n) in GROUPS:
                hb_ = stepn[0] % 2
                stepn[0] += 1
                tiles = ['h2T%d' % t for t in range(t0 // 128, (t0 + n) // 128)]
                for fc in range(2):
                    ab = (2 * stepn[0] + fc) % 2
                    for k in range(8):
                        P.op('pe', lambda e, k=k, fc=fc, ab=ab, s=s, t0=t0, n=n: e.matmul(
                            pA[ab][:, 0:n], wg[s][:, k, fc * 128:(fc + 1) * 128], h2T[:, k, t0:t0 + n],
                            start=(k == 0), stop=(k == 7)), r=['wg%d' % s] + tiles, w=['pA%d' % ab])
                    for k in range(8):
                        P.op('pe', lambda e, k=k, fc=fc, ab=ab, s=s, t0=t0, n=n: e.matmul(
                            pU[ab][:, 0:n], wu[s][:, k, fc * 128:(fc + 1) * 128], h2T[:, k, t0:t0 + n],
                            start=(k == 0), stop=(k == 7)), r=['wu%d' % s] + tiles, w=['pU%d' % ab])
                    P.op('act', lambda e, ab=ab, n=n: e.activation(sa[ab][:, 0:n], pA[ab][:, 0:n], AF.Silu),
                         r=['pA%d' % ab], w=['sa%d' % ab])
                    P.op('dve', lambda e, ab=ab, n=n, fc=fc, hb_=hb_: e.tensor_tensor(hid[hb_][:, fc, 0:n], sa[ab][:, 0:n], pU[ab][:, 0:n], ALU.mult),
                         r=['sa%d' % ab, 'pU%d' % ab], w=['hid%d' % hb_])
                def down(e_=e_, s=s, t0=t0, n=n, hb_=hb_):
                    for tt_ in range(n // 128):
                        ti = t0 // 128 + tt_
                        for half in range(2):
                            yb = (2 * ti + half) % 2
                            for fc in range(2):
                                P.op('pe', lambda e, fc=fc, half=half, yb=yb, tt_=tt_: e.matmul(
                                    pY[yb][:, :], hid[hb_][:, fc, tt_ * 128:(tt_ + 1) * 128], wd[s][:, fc, half * 512:(half + 1) * 512],
                                    start=(fc == 0), stop=(fc == 1)), r=['hid%d' % hb_, 'wd%d' % s], w=['pY%d' % yb])
                            ysl = yacc[:, ti, half * 512:(half + 1) * 512]
                            cw = comb_all[:, ti, e_:e_ + 1]
                            if e_ == 0:
                                P.op('dve', lambda e, yb=yb, ysl=ysl, cw=cw: e.tensor_scalar(ysl, pY[yb][:, :], cw, None, ALU.mult),
                                     r=['pY%d' % yb, 'comb%d' % ti], w=['yacc%d' % ti])
                            else:
                                P.op('dve', lambda e, yb=yb, ysl=ysl, cw=cw: e.scalar_tensor_tensor(ysl, pY[yb][:, :], cw, ysl, ALU.mult, ALU.add),
                                     r=['pY%d' % yb, 'yacc%d' % ti, 'comb%d' % ti], w=['yacc%d' % ti])
                if pending[0] is not None:
                    pending[0]()
                pending[0] = down
        pending[0]()
        for i in range(NTILE):
            b = i % 2
            gs = 1 if i < 2 else 0
            rows = slice(i * 128, (i + 1) * 128)
            P.dma('sp', xs[b][:], xd[rows], w=['xs%d' % b])
            P.op('dve', lambda e, i=i, gs=gs: e.tensor_tensor(yacc[:, i, :], yacc[:, i, :], g2B[:, gs, :], ALU.mult),
                 r=['yacc%d' % i, 'g2B'], w=['yacc%d' % i])
            P.op('dve', lambda e, i=i, b=b: e.scalar_tensor_tensor(xs[b][:], xs[b][:], ALPHA, yacc[:, i, :], ALU.mult, ALU.add),
                 r=['xs%d' % b, 'yacc%d' % i], w=['xs%d' % b])
            ln_tile(P, xs[b], 'xs%d' % b, st, mv, rs, '', lng, lnb, ['lng'])
            P.dma('sp', xo[rows], xs[b][:], r=['xs%d' % b])
        P.finish()
    return nc


def prep_C2_weights(w_rg, b_rg, w_re, b_re, w_e_gate, w_e_up, w_e_down, ln2_g, ln2_b, g2_lat, g2_ctx):
    wr = np.concatenate([w_rg, w_re], axis=1)
    br = np.concatenate([b_rg, b_re], axis=0)
    return {"wr": kp(wr, 8), "brB": bcast(br),
            "wg": np.ascontiguousarray(w_e_gate.reshape(NEXP, 8, 128, DEXP).transpose(0, 2, 1, 3)),
            "wu": np.ascontiguousarray(w_e_up.reshape(NEXP, 8, 128, DEXP).transpose(0, 2, 1, 3)),
            "wd": np.ascontiguousarray(w_e_down.reshape(NEXP, 2, 128, D).transpose(0, 2, 1, 3)),
            "lngB": bcast(ln2_g), "lnbB": bcast(ln2_b),
            "g2B": np.ascontiguousarray(np.stack([bcast(g2_lat), bcast(g2_ctx)], axis=1)),
            "ident": np.eye(128, dtype=np.float32)}


_PROGS = {}


def _prog(name):
    if name not in _PROGS:
        _PROGS[name] = {"M": build_M, "A": build_A, "B1": build_B1, "B2": build_B2, "C1": build_C1, "C2": build_C2}[name]()
    return _PROGS[name]


def _run(name, in_maps):
    res = run_bass_kernel_spmd(_prog(name), in_maps, core_ids=list(range(NCORE)))
    return res.results


def _gather_tok(outs, key, axis):
    parts = [np.take(outs[0][key], np.arange(CTX), axis=axis)]
    for i in range(NCORE):
        parts.append(np.take(outs[i][key], np.arange(CTX, TC), axis=axis))
    return np.concatenate(parts, axis=axis)


def kernel(x, c, ctx, c_ctx, w_ada, b_ada, w_in, b_gates, w_uq, w_uk, w_uv, g_qn, g_kvn, g_mh,
           w_bo_mla, w_bo_mlstm, w_out, ln1_g, ln1_b, w_rg, b_rg, w_re, b_re,
           w_e_gate, w_e_up, w_e_down, ln2_g, ln2_b):
    f32 = np.float32
    x = np.asarray(x, f32)
    ctx = np.asarray(ctx, f32)
    ident = np.eye(128, dtype=f32)
    ones128 = np.ones((128, 128), f32)
    cc = np.ascontiguousarray(np.stack([np.asarray(c, f32)[0].reshape(8, 128).T, np.asarray(c_ctx, f32).reshape(8, 128).T], axis=-1))
    wall = np.concatenate([np.asarray(w_ada[l], f32) for l in range(DEPTH)], axis=1)
    ball = np.concatenate([np.asarray(b_ada[l], f32) for l in range(DEPTH)], axis=0)
    ins = []
    for i in range(NCORE):
        ins.append({"cc": cc, "wa": kp(wall[:, i * 3072:(i + 1) * 3072], 8),
                    "ba": np.ascontiguousarray(ball[i * 3072:(i + 1) * 3072].reshape(24, 128).T)})
    outs = _run("M", ins)
    mod = np.concatenate([o["mo"].transpose(1, 0, 2).reshape(3072, 2) for o in outs], axis=0).reshape(DEPTH, 6 * D, 2)
    del wall, ins

    cosT, ssinT = rope_tables_np()
    toks = [core_tokens(i) for i in range(NCORE)]
    cos4 = [np.ascontiguousarray(np.tile(cosT[t].T, (4, 1))) for t in toks]
    sin4 = [np.ascontiguousarray(np.tile(ssinT[t].T, (4, 1))) for t in toks]
    xc = [np.ascontiguousarray(np.concatenate([ctx[0], x[0, i * LAT_C:(i + 1) * LAT_C]], axis=0)) for i in range(NCORE)]
    perm_f = np.arange(T)
    perm_b = np.concatenate([np.arange(CTX - 1, -1, -1), np.arange(T - 1, CTX - 1, -1)])
    tri = np.triu(np.ones((64, 64), f32))
    ones64 = np.ones((64, 64), f32)
    onescol = np.ones((T, 1), f32)

    for l in range(DEPTH):
        m = mod[l]
        sh1, sc1, g1, sh2, sc2, g2 = [m[j * D:(j + 1) * D] for j in range(6)]
        mod1 = kp(np.stack([sc1[:, 0], sh1[:, 0], sc1[:, 1], sh1[:, 1]], axis=-1), 8)
        mod2 = kp(np.stack([sc2[:, 0], sh2[:, 0], sc2[:, 1], sh2[:, 1]], axis=-1), 8)
        wA = prep_A_weights(np.asarray(w_in[l], f32), np.asarray(w_uq[l], f32), np.asarray(w_uk[l], f32),
                            np.asarray(w_uv[l], f32), np.asarray(g_qn[l], f32), np.asarray(g_kvn[l], f32),
                            np.asarray(b_gates[l], f32))
        ins = []
        for i in range(NCORE):
            d_ = {"x": xc[i], "mod1": mod1, "ident": ident, "ones": ones128, "cos4": cos4[i], "ssin4": sin4[i]}
            d_.update(wA)
            ins.append(d_)
        oA = _run("A", ins)
        del ins, wA
        QT = _gather_tok(oA, "QT", 2).reshape(768, T)
        KnT = _gather_tok(oA, "KnT", 2).reshape(512, T)
        KrT = _gather_tok(oA, "KrT", 1)
        Vall = _gather_tok(oA, "V", 0)
        mqT = _gather_tok(oA, "mqT", 2).reshape(256, T)
        mkT = _gather_tok(oA, "mkT", 2).reshape(256, T)
        mkv = _gather_tok(oA, "mkv", 0)
        graw = _gather_tok(oA, "graw", 1)
        glsg = _gather_tok(oA, "glsg", 1)
        ins = []
        for h in range(8):
            Q = np.concatenate([QT[h * 64:(h + 1) * 64], QT[512 + h * 32:512 + (h + 1) * 32]], axis=0)
            Kk = np.concatenate([KnT[h * 64:(h + 1) * 64], KrT], axis=0)
            Vx = np.concatenate([Vall[:, h * 64:(h + 1) * 64], onescol.astype(Vall.dtype)], axis=1).reshape(NKT, 128, 65).transpose(1, 0, 2)
            ins.append({"Q": np.ascontiguousarray(Q), "K": np.ascontiguousarray(Kk), "V": np.ascontiguousarray(Vx)})
        oB1 = _run("B1", ins)
        ins = []
        for cidx in range(8):
            hd, dr = cidx // 2, cidx % 2
            perm = perm_b if dr else perm_f
            q_ = mqT[hd * 64:(hd + 1) * 64][:, perm]
            k_ = mkT[hd * 64:(hd + 1) * 64][:, perm]
            kt_ = mkv[perm, hd * 64:(hd + 1) * 64].reshape(NCH, 64, 64).transpose(1, 0, 2)
            vx_ = np.concatenate([mkv[perm, 256 + hd * 128:256 + (hd + 1) * 128], onescol.astype(mkv.dtype)], axis=1).reshape(NCH, 64, 129).transpose(1, 0, 2)
            ig_ = graw[(2 * dr) * 4 + hd][perm].reshape(NCH, 64).T
            lf_ = glsg[(2 * dr + 1) * 4 + hd][perm].reshape(NCH, 64).T
            ins.append({"qT": np.ascontiguousarray(q_), "kT": np.ascontiguousarray(k_), "kt": np.ascontiguousarray(kt_),
                        "vx": np.ascontiguousarray(vx_), "ig": np.ascontiguousarray(ig_), "lf": np.ascontiguousarray(lf_),
                        "tri": tri, "ones": ones64})
        oB2 = _run("B2", ins)
        del ins
        hf_all = np.empty((T, 512), f32)
        hb_all = np.empty((T, 512), f32)
        for cidx in range(8):
            hd, dr = cidx // 2, cidx % 2
            hp = oB2[cidx]["H"].transpose(1, 0, 2).reshape(T, 128)
            if dr:
                hb_all[perm_b, hd * 128:(hd + 1) * 128] = hp
            else:
                hf_all[:, hd * 128:(hd + 1) * 128] = hp
        o_all = np.stack([oB1[h]["O"] for h in range(8)], axis=1)
        wC1 = prep_C1_weights(np.asarray(w_bo_mla[l], f32), np.asarray(w_bo_mlstm[l], f32), np.asarray(w_out[l], f32),
                              np.asarray(g_mh[l], f32), np.asarray(ln1_g[l], f32), np.asarray(ln1_b[l], f32), g1[:, 0], g1[:, 1])
        ins = []
        for i in range(NCORE):
            d_ = {"x": xc[i], "o": np.ascontiguousarray(o_all[toks[i]]), "hf": np.ascontiguousarray(hf_all[toks[i]]),
                  "hb": np.ascontiguousarray(hb_all[toks[i]]), "spo": oA[i]["spo"], "spm": oA[i]["spm"]}
            d_.update(wC1)
            ins.append(d_)
        oC1 = _run("C1", ins)
        del ins, oA, o_all, hf_all, hb_all
        wC2 = prep_C2_weights(np.asarray(w_rg[l], f32), np.asarray(b_rg[l], f32), np.asarray(w_re[l], f32), np.asarray(b_re[l], f32),
                              np.asarray(w_e_gate[l], f32), np.asarray(w_e_up[l], f32), np.asarray(w_e_down[l], f32),
                              np.asarray(ln2_g[l], f32), np.asarray(ln2_b[l], f32), g2[:, 0], g2[:, 1])
        wC2["mod2"] = mod2
        ins = []
        for i in range(NCORE):
            d_ = {"x": oC1[i]["xo"]}
            d_.update(wC2)
            ins.append(d_)
        oC2 = _run("C2", ins)
        del ins, wC2
        xc = [np.ascontiguousarray(oC2[i]["xo"]) for i in range(NCORE)]
    out = np.concatenate([xc[i][CTX:] for i in range(NCORE)], axis=0)[None]
    return np.ascontiguousarray(out.astype(np.float32))
```

```python
import numpy as np
from contextlib import ExitStack
import concourse.bass as bass
import concourse.mybir as mybir
from concourse.bass_utils import run_bass_kernel_spmd

F32 = mybir.dt.float32
BF16 = mybir.dt.bfloat16
AF = mybir.ActivationFunctionType
ALU = mybir.AluOpType
AX = mybir.AxisListType

EPOCH = 16000
NDS = 32
ENGS = ('pe', 'act', 'dve', 'pool', 'sp')


class Prog:
    def __init__(self, nc, es):
        self.nc = nc
        self.es = es
        self.ops = {e: [] for e in ENGS}
        self.cnt = {e: 0 for e in ENGS}
        self.known = {e: {} for e in ENGS}
        self.lastw = {}
        self.readers = {}
        self.ndma = 0
        self.esems = {e: [] for e in ENGS}
        self.dsems = [es.enter_context(nc.semaphore("dsem%d" % j)) for j in range(NDS)]
        self.nt = 0

    def sb(self, shape, dt, name=None):
        self.nt += 1
        return self.es.enter_context(self.nc.sbuf_tensor(name or ("sb%d" % self.nt), list(shape), dt))

    def ps(self, shape, dt, name=None):
        self.nt += 1
        return self.es.enter_context(self.nc.psum_tensor(name or ("ps%d" % self.nt), list(shape), dt))

    def _esem(self, eng, idx):
        while len(self.esems[eng]) <= idx:
            self.esems[eng].append(self.es.enter_context(
                self.nc.semaphore("s_%s_%d" % (eng, len(self.esems[eng])))))
        return self.esems[eng][idx]

    def _deps(self, eng, r, w, extra=()):
        need = {}

        def add(p, v):
            if p is None:
                return
            if need.get(p, 0) < v:
                need[p] = v
        for k in r:
            lw = self.lastw.get(k)
            if lw:
                add(*lw)
        for k in w:
            lw = self.lastw.get(k)
            if lw:
                add(*lw)
            for p, v in self.readers.get(k, {}).items():
                add(p, v)
        for p, v in extra:
            add(p, v)
        waits = []
        kn = self.known[eng]
        for p, v in need.items():
            if p == eng and eng in ('pe', 'sp'):
                continue
            if kn.get(p, 0) >= v:
                continue
            kn[p] = v
            waits.append((p, v))
        return waits

    def op(self, eng, fn, r=(), w=()):
        waits = self._deps(eng, r, w)
        self.cnt[eng] += 1
        c = self.cnt[eng]
        self.ops[eng].append(('op', fn, waits, c))
        for k in r:
            d = self.readers.setdefault(k, {})
            d[eng] = c
        for k in w:
            self.lastw[k] = (eng, c)
            self.readers[k] = {}
        return c

    def dma(self, q, out, in_, r=(), w=(), **kw):
        j = self.ndma % NDS
        n = self.ndma // NDS + 1
        self.ndma += 1
        prod = ('d', j)
        waits = self._deps(q, r, w, extra=([(prod, n - 1)] if n > 1 else []))
        self.ops[q].append(('dma', (out, in_, kw), waits, (j, n)))
        for k in r:
            d = self.readers.setdefault(k, {})
            d[prod] = n
        for k in w:
            self.lastw[k] = (prod, n)
            self.readers[k] = {}

    def _emit_wait(self, e, p, v):
        if isinstance(p, tuple):
            e.wait_ge(self.dsems[p[1]], 16 * v)
        else:
            idx = (v - 1) // EPOCH
            e.wait_ge(self._esem(p, idx), (v - 1) % EPOCH + 1)

    def _run(self, name, e):
        for kind, payload, waits, c in self.ops[name]:
            for p, v in waits:
                self._emit_wait(e, p, v)
            if kind == 'op':
                ins = payload(e)
                idx = (c - 1) // EPOCH
                ins.then_inc(self._esem(name, idx), 1)
            else:
                out, in_, kw = payload
                j, n = c
                e.dma_start(out=out, in_=in_, **kw).then_inc(self.dsems[j], 16)
        if name == 'sp':
            tot = {}
            for j in range(NDS):
                n = (self.ndma - j + NDS - 1) // NDS if self.ndma > j else 0
                if n > 0:
                    e.wait_ge(self.dsems[j], 16 * n)

    def finish(self):
        nc = self.nc
        for e in ENGS:
            if self.cnt[e] > 0:
                self._esem(e, (self.cnt[e] - 1) // EPOCH)
        with nc.Block() as block:
            @block.sync
            def _(sync):
                self._run('sp', sync)

            @block.tensor
            def _(tensor):
                self._run('pe', tensor)

            @block.scalar
            def _(scalar):
                self._run('act', scalar)

            @block.vector
            def _(vector):
                self._run('dve', vector)

            @block.gpsimd
            def _(gpsimd):
                self._run('pool', gpsimd)


D = 1024
SEQ = 16384
CTX = 256
T = SEQ + CTX
NCORE = 8
LAT_C = SEQ // NCORE
TC = CTX + LAT_C
NTILE = TC // 128
GROUPS = [(0, 256), (256, 512), (768, 512), (1280, 512), (1792, 512)]
DEPTH = 4
EPS = 1e-6
ALPHA = (2 * DEPTH) ** 0.25
MLA_SCALE = 96 ** -0.5
NCH = T // 64


def dram_in(nc, name, shape, dt=F32):
    return nc.dram_tensor(name, list(shape), dt, kind="ExternalInput").ap()


def dram_out(nc, name, shape, dt=F32):
    return nc.dram_tensor(name, list(shape), dt, kind="ExternalOutput").ap()


def build_M():
    nc = bass.Bass("TRN2", target_bir_lowering=False)
    cc = dram_in(nc, "cc", [128, 8, 2])
    wa = dram_in(nc, "wa", [128, 8, 3072])
    ba = dram_in(nc, "ba", [128, 24])
    mo = dram_out(nc, "mo", [128, 24, 2])
    with ExitStack() as es:
        P = Prog(nc, es)
        ccs = P.sb([128, 8, 2], F32)
        was = P.sb([128, 8, 3072], F32)
        bas = P.sb([128, 24], F32)
        mos = P.sb([128, 24, 2], F32)
        pm = P.ps([128, 24, 2], F32)
        P.dma('sp', ccs[:], cc, w=['cc'])
        P.dma('sp', bas[:], ba, w=['ba'])
        for k in range(8):
            P.dma('sp', was[:, k, :], wa[:, k, :], w=['wa%d' % k])
        P.op('act', lambda e: e.activation(ccs[:], ccs[:], AF.Silu), r=['cc'], w=['cc'])
        for j in range(24):
            for k in range(8):
                P.op('pe', lambda e, j=j, k=k: e.matmul(pm[:, j, :], was[:, k, j * 128:(j + 1) * 128], ccs[:, k, :],
                                                       start=(k == 0), stop=(k == 7)),
                     r=['cc', 'wa%d' % k], w=['pm'])
        P.op('dve', lambda e: e.tensor_tensor(mos[:], pm[:], bas[:].unsqueeze(2).to_broadcast([128, 24, 2]), ALU.add),
             r=['pm', 'ba'], w=['mo'])
        P.dma('sp', mo, mos[:], r=['mo'])
        P.finish()
    return nc


A_QD, A_KVD, A_KR, A_KRP, A_MQ, A_MK, A_PG = 0, 384, 640, 672, 704, 960, 1216
A_TM = 1232
A_NCOL = A_TM + 256 + 512 + 512 + 2048


class Common:
    def __init__(self, P, nc):
        self.P = P
        self.nc = nc

    def load(self, q, dst, src, key, maxcols=None):
        self.P.dma(q, dst, src, w=[key])


def ln_rstd(P, var_ap, out_ap, rkeys, wkey, scale=1.0):
    P.op('act', lambda e: e.activation(out_ap, var_ap, AF.Ln, bias=EPS, scale=scale), r=rkeys, w=[wkey])
    P.op('act', lambda e: e.activation(out_ap, out_ap, AF.Exp, scale=-0.5), r=[wkey], w=[wkey])


def build_A():
    nc = bass.Bass("TRN2", target_bir_lowering=False)
    x = dram_in(nc, "x", [TC, D])
    mod1 = dram_in(nc, "mod1", [128, 8, 4])
    ident = dram_in(nc, "ident", [128, 128])
    ones_d = dram_in(nc, "ones", [128, 128])
    w_in = dram_in(nc, "w_in", [128, 8, A_NCOL])
    w_uq = dram_in(nc, "w_uq", [128, 3, 1024])
    w_uk = dram_in(nc, "w_uk", [128, 2, 512])
    w_uv = dram_in(nc, "w_uv", [128, 2, 512])
    gq_d = dram_in(nc, "gq", [128, 3])
    gkv_d = dram_in(nc, "gkv", [128, 2])
    bg_d = dram_in(nc, "bg", [16, 1])
    cos_d = dram_in(nc, "cos4", [128, TC])
    sin_d = dram_in(nc, "ssin4", [128, TC])
    QT = dram_out(nc, "QT", [6, 128, TC], BF16)
    KnT = dram_out(nc, "KnT", [4, 128, TC], BF16)
    KrT = dram_out(nc, "KrT", [32, TC], BF16)
    V = dram_out(nc, "V", [TC, 512], BF16)
    mqT = dram_out(nc, "mqT", [2, 128, TC], BF16)
    mkT = dram_out(nc, "mkT", [2, 128, TC], BF16)
    mkv = dram_out(nc, "mkv", [TC, 768], BF16)
    graw = dram_out(nc, "graw", [16, TC])
    glsg = dram_out(nc, "glsg", [16, TC])
    spo = dram_out(nc, "spo", [TC, 512], BF16)
    spm = dram_out(nc, "spm", [TC, 2048], BF16)
    with ExitStack() as es:
        P = Prog(nc, es)
        idt = P.sb([128, 128], F32)
        ones = P.sb([128, 128], F32)
        mods = P.sb([128, 8, 4], F32)
        win = P.sb([128, 8, A_NCOL], BF16)
        wuq = P.sb([128, 3, 1024], BF16)
        wuk = P.sb([128, 2, 512], BF16)
        wuv = P.sb([128, 2, 512], BF16)
        gq = P.sb([128, 3], F32)
        gkv = P.sb([128, 2], F32)
        bg = P.sb([16, 1], F32)
        cos4 = P.sb([128, TC], F32)
        sin4 = P.sb([128, TC], F32)
        hT = P.sb([128, 8, TC], BF16)
        xs = [P.sb([128, D], F32) for _ in range(2)]
        xn = [P.sb([128, D], F32) for _ in range(2)]
        st = [P.sb([128, 12], F32) for _ in range(2)]
        mv = [P.sb([128, 2], F32) for _ in range(2)]
        rs = [P.sb([128, 1], F32) for _ in range(2)]
        tp = P.ps([128, 8, 128], F32)
        pf = [P.ps([128, 512], F32) for _ in range(2)]
        pt = [P.ps([128, 512], F32) for _ in range(2)]
        pst = P.ps([128, 512], F32)
        dn = P.sb([128, 3, 512], F32)
        sq = P.sb([128, 3, 512], F32)
        rq = P.sb([128, 512], F32)
        dnn = P.sb([128, 3, 512], BF16)
        stg = [P.sb([128, 512], BF16) for _ in range(3)]
        t1b = P.sb([128, 512], BF16)
        t1 = P.sb([128, 512], F32)
        t2 = P.sb([128, 512], F32)
        g1 = P.sb([16, 512], F32)
        g2 = P.sb([16, 512], F32)
        g3 = P.sb([16, 512], F32)

        P.dma('sp', idt[:], ident, w=['idt'])
        P.dma('sp', ones[:], ones_d, w=['ones'])
        P.dma('sp', mods[:], mod1, w=['mods'])
        P.dma('sp', gq[:], gq_d, w=['gq'])
        P.dma('sp', gkv[:], gkv_d, w=['gkv'])
        P.dma('sp', bg[:], bg_d, w=['bg'])
        P.dma('sp', cos4[:], cos_d, w=['cos'])
        P.dma('sp', sin4[:], sin_d, w=['sin'])
        for k in range(8):
            for c0 in range(0, A_NCOL, 1520):
                P.dma('pool', win[:, k, c0:c0 + 1520], w_in[:, k, c0:c0 + 1520], w=['win'])
        for j in range(3):
            P.dma('pool', wuq[:, j, :], w_uq[:, j, :], w=['wuq'])
        for j in range(2):
            P.dma('pool', wuk[:, j, :], w_uk[:, j, :], w=['wuk'])
            P.dma('pool', wuv[:, j, :], w_uv[:, j, :], w=['wuv'])
        P.op('dve', lambda e: e.tensor_scalar(mods[:, :, 0:1], mods[:, :, 0:1], 1.0, None, ALU.add), r=['mods'], w=['mods'])
        P.op('dve', lambda e: e.tensor_scalar(mods[:, :, 2:3], mods[:, :, 2:3], 1.0, None, ALU.add), r=['mods'], w=['mods'])

        for i in range(NTILE):
            b = i % 2
            sel = 2 if i < 2 else 0
            P.dma('sp', xs[b][:], x[i * 128:(i + 1) * 128, :], w=['xs%d' % b])
            for hh in range(2):
                P.op('dve', lambda e, b=b, hh=hh: e.bn_stats(st[b][:, hh * 6:(hh + 1) * 6], xs[b][:, hh * 512:(hh + 1) * 512]),
                     r=['xs%d' % b], w=['st%d' % b])
            P.op('dve', lambda e, b=b: e.bn_aggr(mv[b][:], st[b][:]), r=['st%d' % b], w=['mv%d' % b])
            ln_rstd(P, mv[b][:, 1:2], rs[b][:], ['mv%d' % b], 'rs%d' % b)
            P.op('dve', lambda e, b=b: e.tensor_scalar(xn[b][:], xs[b][:], mv[b][:, 0:1], rs[b][:, 0:1], ALU.subtract, ALU.mult),
                 r=['xs%d' % b, 'mv%d' % b, 'rs%d' % b], w=['xn%d' % b])
            for k in range(8):
                P.op('pe', lambda e, b=b, k=k: e.transpose(tp[:, k, :], xn[b][:, k * 128:(k + 1) * 128], idt[:]),
                     r=['xn%d' % b, 'idt'], w=['tp%d' % (k // 4)])
            for k in range(8):
                P.op('act', lambda e, i=i, k=k, sel=sel: e.activation(
                    hT[:, k, i * 128:(i + 1) * 128], tp[:, k, :], AF.Identity,
                    bias=mods[:, k, sel + 1:sel + 2], scale=mods[:, k, sel:sel + 1]),
                    r=['tp%d' % (k // 4), 'mods'], w=['hT%d' % i])

        fmi = [0]

        def fm_mm(col0, m, s, n):
            bi = fmi[0] % 2
            fmi[0] += 1
            ps = pf[bi]
            tiles = ['hT%d' % t for t in range(s // 128, (s + n) // 128)]
            for k in range(8):
                P.op('pe', lambda e, k=k, ps=ps: e.matmul(ps[0:m, 0:n], win[:, k, col0:col0 + m], hT[:, k, s:s + n],
                                                          start=(k == 0), stop=(k == 7)),
                     r=['win'] + tiles, w=['pf%d' % bi])
            return ps, 'pf%d' % bi

        sgi = [0]

        def stage_out(ps, pkey, m, n, dst, func=AF.Copy, scale=1.0, bias=None):
            si = sgi[0] % 3
            sgi[0] += 1
            sg = stg[si]
            if bias is None:
                P.op('act', lambda e: e.activation(sg[0:m, 0:n], ps[0:m, 0:n], func, scale=scale),
                     r=[pkey], w=['stg%d' % si])
            else:
                P.op('act', lambda e: e.activation(sg[0:m, 0:n], ps[0:m, 0:n], func, bias=bias, scale=scale),
                     r=[pkey, 'bg'], w=['stg%d' % si])
            P.dma('sp', dst, sg[0:m, 0:n], r=['stg%d' % si])

        def rms_block(col0, nch, gvec, gkey, dim, s, n):
            for j in range(nch):
                ps, pk = fm_mm(col0 + j * 128, 128, s, n)
                P.op('act', lambda e, j=j, ps=ps: e.activation(dn[:, j, 0:n], ps[:, 0:n], AF.Copy), r=[pk], w=['dn%d' % j])
                P.op('act', lambda e, j=j, ps=ps: e.activation(sq[:, j, 0:n], ps[:, 0:n], AF.Square), r=[pk], w=['sq%d' % j])
            for j in range(nch):
                P.op('pe', lambda e, j=j: e.matmul(pst[:, 0:n], ones[:], sq[:, j, 0:n], start=(j == 0), stop=(j == nch - 1)),
                     r=['ones', 'sq%d' % j], w=['pst'])
            ln_rstd(P, pst[:, 0:n], rq[:, 0:n], ['pst'], 'rq', scale=1.0 / dim)
            for j in range(nch):
                P.op('dve', lambda e, j=j: e.scalar_tensor_tensor(dnn[:, j, 0:n], dn[:, j, 0:n], gvec[:, j:j + 1], rq[:, 0:n],
                                                                 ALU.mult, ALU.mult),
                     r=['dn%d' % j, gkey, 'rq'], w=['dnn%d' % j])

        def up_mm(wt, wkey, nch, col0, m, n, tok0=None, tm=False):
            bi = fmi[0] % 2
            fmi[0] += 1
            ps = pf[bi]
            for j in range(nch):
                if not tm:
                    P.op('pe', lambda e, j=j, ps=ps: e.matmul(ps[0:m, 0:n], wt[:, j, col0:col0 + m], dnn[:, j, 0:n],
                                                              start=(j == 0), stop=(j == nch - 1)),
                         r=[wkey, 'dnn%d' % j], w=['pf%d' % bi])
                else:
                    P.op('pe', lambda e, j=j, ps=ps: e.matmul(ps[:, 0:m], dnn[:, j, tok0:tok0 + 128], wt[:, j, col0:col0 + m],
                                                              start=(j == 0), stop=(j == nch - 1)),
                         r=[wkey, 'dnn%d' % j], w=['pf%d' % bi])
            return ps, 'pf%d' % bi

        def rope_out(psA, kA, psP, kP, m, s, n, dst):
            P.op('dve', lambda e: e.tensor_tensor(t1[0:m, 0:n], psA[0:m, 0:n], cos4[0:m, s:s + n], ALU.mult),
                 r=[kA, 'cos'], w=['t1'])
            P.op('dve', lambda e: e.tensor_tensor(t2[0:m, 0:n], psP[0:m, 0:n], sin4[0:m, s:s + n], ALU.mult),
                 r=[kP, 'sin'], w=['t2'])
            P.op('dve', lambda e: e.tensor_tensor(t1b[0:m, 0:n], t1[0:m, 0:n], t2[0:m, 0:n], ALU.add),
                 r=['t1', 't2'], w=['t1b'])
            P.dma('sp', dst, t1b[0:m, 0:n], r=['t1b'])

        for (s, n) in GROUPS:
            rms_block(A_QD, 3, gq, 'gq', 384, s, n)
            for oc in range(4):
                ps, pk = up_mm(wuq, 'wuq', 3, oc * 128, 128, n)
                stage_out(ps, pk, 128, n, QT[oc, :, s:s + n])
            for c in range(2):
                psA, kA = up_mm(wuq, 'wuq', 3, 512 + c * 128, 128, n)
                psP, kP = up_mm(wuq, 'wuq', 3, 768 + c * 128, 128, n)
                rope_out(psA, kA, psP, kP, 128, s, n, QT[4 + c, :, s:s + n])
            rms_block(A_KVD, 2, gkv, 'gkv', 256, s, n)
            for oc in range(4):
                ps, pk = up_mm(wuk, 'wuk', 2, oc * 128, 128, n)
                stage_out(ps, pk, 128, n, KnT[oc, :, s:s + n])
            for tt in range(n // 128):
                ps, pk = up_mm(wuv, 'wuv', 2, 0, 512, n, tok0=tt * 128, tm=True)
                stage_out(ps, pk, 128, 512, V[s + tt * 128:s + (tt + 1) * 128, :])
            psA, kA = fm_mm(A_KR, 32, s, n)
            psP, kP = fm_mm(A_KRP, 32, s, n)
            rope_out(psA, kA, psP, kP, 32, s, n, KrT[:, s:s + n])
            for c in range(2):
                ps, pk = fm_mm(A_MQ + c * 128, 128, s, n)
                stage_out(ps, pk, 128, n, mqT[c, :, s:s + n], scale=0.125)
            for c in range(2):
                ps, pk = fm_mm(A_MK + c * 128, 128, s, n)
                stage_out(ps, pk, 128, n, mkT[c, :, s:s + n])
            ps, pk = fm_mm(A_PG, 16, s, n)
            P.op('act', lambda e, ps=ps: e.activation(g1[:, 0:n], ps[0:16, 0:n], AF.Identity, bias=bg[:, 0:1], scale=1.0),
                 r=[pk, 'bg'], w=['g1'])
            P.dma('sp', graw[:, s:s + n], g1[:, 0:n], r=['g1'])
            P.op('act', lambda e: e.activation(g2[:, 0:n], g1[:, 0:n], AF.Exp, scale=-1.0), r=['g1'], w=['g2'])
            P.op('act', lambda e: e.activation(g3[:, 0:n], g2[:, 0:n], AF.Ln, bias=1.0, scale=1.0), r=['g2'], w=['g3'])
            P.op('act', lambda e: e.activation(g3[:, 0:n], g3[:, 0:n], AF.Copy, scale=-1.0), r=['g3'], w=['g3'])
            P.dma('sp', glsg[:, s:s + n], g3[:, 0:n], r=['g3'])

        tmi = [0]
        tm_groups = [(A_TM, 256, mkv, 0, AF.Copy), (A_TM + 256, 512, mkv, 256, AF.Copy),
                     (A_TM + 768, 512, spo, 0, AF.Sigmoid)] + \
                    [(A_TM + 1280 + q * 512, 512, spm, q * 512, AF.Sigmoid) for q in range(4)]
        for i in range(NTILE):
            for (c0, ncl, dst, dc0, func) in tm_groups:
                bi = tmi[0] % 2
                tmi[0] += 1
                ps = pt[bi]
                for k in range(8):
                    P.op('pe', lambda e, k=k, ps=ps, c0=c0, ncl=ncl, i=i: e.matmul(
                        ps[:, 0:ncl], hT[:, k, i * 128:(i + 1) * 128], win[:, k, c0:c0 + ncl],
                        start=(k == 0), stop=(k == 7)), r=['win', 'hT%d' % i], w=['pt%d' % bi])
                stage_out(ps, 'pt%d' % bi, 128, ncl, dst[i * 128:(i + 1) * 128, dc0:dc0 + ncl], func=func)
        P.finish()
    return nc


ROPE_PERM = np.concatenate([np.arange(8, 16), np.arange(0, 8), np.arange(24, 32), np.arange(16, 24)])
ROPE_SIGN = np.concatenate([-np.ones(8), np.ones(8), -np.ones(8), np.ones(8)]).astype(np.float32)


def rope_tables_np():
    rows = SEQ // 64
    row, col = np.meshgrid(np.arange(rows, dtype=np.float32), np.arange(64, dtype=np.float32), indexing='ij')
    row, col = row.reshape(-1), col.reshape(-1)
    half = 16
    inv = (np.float32(10000.0) ** (-np.arange(0, half, 2, dtype=np.float32) / np.float32(half))).astype(np.float32)
    ar, ac = row[:, None] * inv, col[:, None] * inv
    ang = np.concatenate([ar, ar, ac, ac], axis=-1)
    ang = np.concatenate([np.zeros((CTX, 32), np.float32), ang], axis=0).astype(np.float32)
    return np.cos(ang).astype(np.float32), (np.sin(ang) * ROPE_SIGN).astype(np.float32)


def core_tokens(i):
    return np.concatenate([np.arange(CTX), CTX + np.arange(i * LAT_C, (i + 1) * LAT_C)])


def kp(a, nk):
    return np.ascontiguousarray(a.reshape(nk, 128, -1).transpose(1, 0, 2))


def prep_A_weights(w_in, w_uq, w_uk, w_uv, g_qn, g_kvn, b_gates):
    kr = 640 + ROPE_PERM
    colsA = np.concatenate([np.arange(0, 384), np.arange(384, 640), np.arange(640, 672), kr,
                            np.arange(672, 928), np.arange(928, 1184), np.arange(2208, 2224),
                            np.arange(928, 1184), np.arange(1184, 1696), np.arange(1696, 2208),
                            np.arange(2224, 4272)])
    assert len(colsA) == A_NCOL
    hq = np.arange(8)[:, None] * 96
    nope = (hq + np.arange(64)[None, :]).reshape(-1)
    rope = (hq + 64 + np.arange(32)[None, :]).reshape(-1)
    ropep = (hq + 64 + ROPE_PERM[None, :]).reshape(-1)
    colsq = np.concatenate([nope, rope, ropep])
    return {
        "w_in": kp(w_in[:, colsA], 8),
        "w_uq": kp(w_uq[:, colsq], 3),
        "w_uk": kp(w_uk, 2),
        "w_uv": kp(w_uv, 2),
        "gq": np.ascontiguousarray(g_qn.reshape(3, 128).T),
        "gkv": np.ascontiguousarray(g_kvn.reshape(2, 128).T),
        "bg": np.ascontiguousarray(b_gates.reshape(16, 1)),
    }


NKT = T // 128
QGROUPS = [(0, 256, 2)] + [(CTX + g * 512, 512, NKT) for g in range(SEQ // 512)]


def build_B1():
    nc = bass.Bass("TRN2", target_bir_lowering=False)
    Qd = dram_in(nc, "Q", [96, T], BF16)
    Kd = dram_in(nc, "K", [96, T], BF16)
    Vd = dram_in(nc, "V", [128, NKT, 65], BF16)
    Od = dram_out(nc, "O", [T, 65])
    with ExitStack() as es:
        P = Prog(nc, es)
        Qs = P.sb([96, T], BF16)
        Ks = P.sb([96, T], BF16)
        Vs = P.sb([128, NKT, 65], BF16)
        pTs = [P.sb([128, 512], BF16) for _ in range(3)]
        ost = [P.sb([128, 4, 65], F32) for _ in range(2)]
        pss = [P.ps([128, 512], F32) for _ in range(3)]
        pso = [P.ps([128, 512], F32) for _ in range(4)]
        CW = 1280
        for c0 in range(0, T, CW):
            P.dma('sp', Ks[:, c0:c0 + CW], Kd[:, c0:c0 + CW], w=['K%d' % (c0 // CW)])
            P.dma('sp', Qs[:, c0:c0 + CW], Qd[:, c0:c0 + CW], w=['Q%d' % (c0 // CW)])
        for t0 in range(0, NKT, 26):
            P.dma('sp', Vs[:, t0:t0 + 26, :], Vd[:, t0:t0 + 26, :], w=['V%d' % (t0 // 26)])
        steps = [(gi, kt) for gi, (q0, nq, nkt) in enumerate(QGROUPS) for kt in range(nkt)]

        def mm1(si):
            gi, kt = steps[si]
            q0, nq, nkt = QGROUPS[gi]
            sb_ = si % 3
            qkeys = ['Q%d' % j for j in range(q0 // CW, (q0 + nq - 1) // CW + 1)]
            P.op('pe', lambda e: e.matmul(pss[sb_][:, 0:nq], Ks[:, kt * 128:(kt + 1) * 128], Qs[:, q0:q0 + nq],
                                          start=True, stop=True),
                 r=['K%d' % ((kt * 128) // CW)] + qkeys, w=['pss%d' % sb_])

        LA = 2
        for si in range(min(LA, len(steps))):
            mm1(si)
        for si, (gi, kt) in enumerate(steps):
            q0, nq, nkt = QGROUPS[gi]
            sb_ = si % 3
            if si + LA < len(steps):
                mm1(si + LA)
            P.op('act', lambda e, sb_=sb_, nq=nq: e.activation(pTs[sb_][:, 0:nq], pss[sb_][:, 0:nq], AF.Exp, scale=MLA_SCALE),
                 r=['pss%d' % sb_], w=['pT%d' % sb_])
            for qb in range(nq // 128):
                P.op('pe', lambda e, sb_=sb_, kt=kt, qb=qb, nkt=nkt: e.matmul(
                    pso[qb][:, 0:65], pTs[sb_][:, qb * 128:(qb + 1) * 128], Vs[:, kt, :],
                    start=(kt == 0), stop=(kt == nkt - 1)),
                    r=['V%d' % (kt // 26), 'pT%d' % sb_], w=['pso%d' % qb])
            if kt == nkt - 1:
                ob = gi % 2
                nb = nq // 128
                for qb in range(nb):
                    P.op('dve', lambda e, ob=ob, qb=qb: e.tensor_copy(ost[ob][:, qb, :], pso[qb][:, 0:65]),
                         r=['pso%d' % qb], w=['ost%d' % ob])
                P.dma('sp', Od[q0:q0 + nq, :].rearrange("(b p) c -> p b c", p=128), ost[ob][:, 0:nb, :], r=['ost%d' % ob])
        P.finish()
    return nc


CB = 20
NBLK = NCH // CB
RING = 8


def build_B2():
    nc = bass.Bass("TRN2", target_bir_lowering=False)
    qTd = dram_in(nc, "qT", [64, T], BF16)
    kTd = dram_in(nc, "kT", [64, T], BF16)
    ktd = dram_in(nc, "kt", [64, NCH, 64], BF16)
    vxd = dram_in(nc, "vx", [64, NCH, 129], BF16)
    igd = dram_in(nc, "ig", [64, NCH])
    lfd = dram_in(nc, "lf", [64, NCH])
    trid = dram_in(nc, "tri", [64, 64])
    oned = dram_in(nc, "ones", [64, 64])
    H = dram_out(nc, "H", [64, NCH, 128])
    with ExitStack() as es:
        P = Prog(nc, es)
        qT = P.sb([64, T], BF16)
        kT = P.sb([64, T], BF16)
        kt = P.sb([64, NCH, 64], BF16)
        ig = P.sb([64, NCH], F32)
        lf = P.sb([64, NCH], F32)
        tri = P.sb([64, 64], F32)
        ones = P.sb([64, 64], F32)
        bb = P.sb([64, NCH], F32)
        ebl = P.sb([64, NCH], F32)
        ek = P.sb([64, NCH], F32)
        ek2 = P.sb([64, NCH], F32)
        eb = P.sb([64, NCH], F32)
        tmp = P.sb([64, NCH], F32)
        vx = [P.sb([64, CB, 129], BF16)] * 2
        v1 = [P.sb([64, CB, 129], BF16) for _ in range(2)]
        v2 = [P.sb([64, CB, 129], BF16) for _ in range(2)]
        Us = [P.sb([64, CB, 129], F32) for _ in range(2)]
        MTb = [P.sb([64, CB, 64], BF16) for _ in range(2)]
        hr = [P.sb([64, CB, 129], F32) for _ in range(2)]
        ho = [P.sb([64, CB, 128], F32)] * 2
        tn = [P.sb([64, CB], F32) for _ in range(2)]
        Cst = [P.sb([64, 129], F32) for _ in range(3)]
        Cbf = [P.sb([64, 129], BF16) for _ in range(RING)]
        psS = [P.ps([64, 512], F32) for _ in range(2)]
        psU = [P.ps([64, 512], F32) for _ in range(2)]
        ps_h = [P.ps([64, 512], F32) for _ in range(3)]
        pb = psS[0]
        pbl = psU[0]
        CW = 1280
        P.dma('sp', ig[:], igd, w=['ig'])
        P.dma('sp', lf[:], lfd, w=['lf'])
        P.dma('sp', tri[:], trid, w=['tri'])
        P.dma('sp', ones[:], oned, w=['ones'])
        for b in range(NBLK):
            c0 = b * CW
            P.dma('sp', qT[:, c0:c0 + CW], qTd[:, c0:c0 + CW], w=['qT%d' % b])
            P.dma('sp', kT[:, c0:c0 + CW], kTd[:, c0:c0 + CW], w=['kT%d' % b])
            P.dma('sp', kt[:, b * CB:(b + 1) * CB, :], ktd[:, b * CB:(b + 1) * CB, :], w=['kt%d' % b])
        P.op('pe', lambda e: e.matmul(pb[:, 0:NCH], tri[:], lf[:], start=True, stop=True), r=['tri', 'lf'], w=['psS0'])
        P.op('pe', lambda e: e.matmul(pbl[:, 0:NCH], ones[:], lf[:], start=True, stop=True), r=['ones', 'lf'], w=['psU0'])
        P.op('dve', lambda e: e.tensor_copy(bb[:], pb[:, 0:NCH]), r=['psS0'], w=['bb'])
        P.op('act', lambda e: e.activation(ebl[:], pbl[:, 0:NCH], AF.Exp), r=['psU0'], w=['ebl'])
        P.op('act', lambda e: e.activation(eb[:], bb[:], AF.Exp), r=['bb'], w=['eb'])
        P.op('dve', lambda e: e.tensor_tensor(tmp[:], ig[:], bb[:], ALU.subtract), r=['ig', 'bb'], w=['tmp'])
        P.op('act', lambda e: e.activation(ek[:], tmp[:], AF.Exp), r=['tmp'], w=['ek'])
        P.op('dve', lambda e: e.tensor_tensor(tmp[:], tmp[:], pbl[:, 0:NCH], ALU.add), r=['tmp', 'psU0', 'ek'], w=['tmp2'])
        P.op('act', lambda e: e.activation(ek2[:], tmp[:], AF.Exp), r=['tmp2'], w=['ek2'])
        P.op('dve', lambda e: e.memset(Cst[0][:], 0.0), w=['Cst0'])
        P.op('dve', lambda e: e.memset(Cbf[0][:], 0.0), w=['Cbf0'])
        sct = [0]
        uct = [0]

        def batch(b):
            s = b % 2
            P.dma('sp', vx[s][:], vxd[:, b * CB:(b + 1) * CB, :], w=['vx'])
            P.op('dve', lambda e: e.tensor_tensor(v1[s][:], vx[s][:],
                                                  ek[:, b * CB:(b + 1) * CB].unsqueeze(2).to_broadcast([64, CB, 129]), ALU.mult),
                 r=['vx', 'ek'], w=['v1_%d' % s])
            P.op('dve', lambda e: e.tensor_tensor(v2[s][:], vx[s][:],
                                                  ek2[:, b * CB:(b + 1) * CB].unsqueeze(2).to_broadcast([64, CB, 129]), ALU.mult),
                 r=['vx', 'ek2'], w=['v2_%d' % s])
            for ci0 in range(0, CB, 8):
                nb = min(8, CB - ci0)
                bk = sct[0] % 2
                sct[0] += 1
                for j in range(nb):
                    c = b * CB + ci0 + j
                    P.op('pe', lambda e, c=c, j=j, bk=bk: e.matmul(psS[bk][:, j * 64:(j + 1) * 64], kT[:, c * 64:(c + 1) * 64],
                                                                 qT[:, c * 64:(c + 1) * 64], start=True, stop=True),
                         r=['qT%d' % ((c * 64) // CW), 'kT%d' % ((c * 64) // CW)], w=['psS%d' % bk])
                P.op('dve', lambda e, ci0=ci0, nb=nb, bk=bk: e.tensor_tensor(
                    MTb[s][:, ci0:ci0 + nb, :], psS[bk][:, 0:nb * 64].rearrange("p (c j) -> p c j", c=nb),
                    tri[:].unsqueeze(1).to_broadcast([64, nb, 64]), ALU.mult),
                    r=['psS%d' % bk, 'tri'], w=['MT%d' % s])
            for ci0 in range(0, CB, 3):
                nb = min(3, CB - ci0)
                bk = uct[0] % 2
                uct[0] += 1
                for j in range(nb):
                    c = b * CB + ci0 + j
                    P.op('pe', lambda e, c=c, j=j, bk=bk, ci0=ci0: e.matmul(psU[bk][:, j * 129:(j + 1) * 129], kt[:, c, :],
                                                                          v2[s][:, ci0 + j, :], start=True, stop=True),
                         r=['kt%d' % b, 'v2_%d' % s], w=['psU%d' % bk])
                P.op('act', lambda e, ci0=ci0, nb=nb, bk=bk: e.activation(
                    Us[s][:, ci0:ci0 + nb, :], psU[bk][:, 0:nb * 129].rearrange("p (c j) -> p c j", c=nb), AF.Copy),
                    r=['psU%d' % bk], w=['Us%d' % s])

        batch(0)
        for b in range(NBLK):
            s = b % 2
            if b + 1 < NBLK:
                batch(b + 1)
            for ci in range(CB):
                c = b * CB + ci
                hb_ = c % 3
                P.op('pe', lambda e, ci=ci, hb_=hb_, s=s: e.matmul(ps_h[hb_][:, 0:129], MTb[s][:, ci, :], v1[s][:, ci, :], start=True, stop=False),
                     r=['MT%d' % s, 'v1_%d' % s], w=['ps_h%d' % hb_])
                P.op('pe', lambda e, c=c, hb_=hb_: e.matmul(ps_h[hb_][:, 0:129], qT[:, c * 64:(c + 1) * 64], Cbf[c % RING][:], start=False, stop=True),
                     r=['qT%d' % ((c * 64) // CW), 'Cbf%d' % (c % RING)], w=['ps_h%d' % hb_])
                P.op('act', lambda e, ci=ci, hb_=hb_, s=s: e.activation(hr[s][:, ci, :], ps_h[hb_][:, 0:129], AF.Copy),
                     r=['ps_h%d' % hb_], w=['hr%d' % s])
                if c + 1 < NCH:
                    P.op('dve', lambda e, c=c, ci=ci, s=s: e.scalar_tensor_tensor(Cst[(c + 1) % 3][:], Cst[c % 3][:], ebl[:, c:c + 1],
                                                                         Us[s][:, ci, :], ALU.mult, ALU.add),
                         r=['Cst%d' % (c % 3), 'ebl', 'Us%d' % s], w=['Cst%d' % ((c + 1) % 3)])
                    P.op('dve', lambda e, c=c, ci=ci, s=s: e.scalar_tensor_tensor(Cbf[(c + 1) % RING][:], Cst[c % 3][:], ebl[:, c:c + 1],
                                                                              Us[s][:, ci, :], ALU.mult, ALU.add),
                         r=['Cst%d' % (c % 3), 'ebl', 'Us%d' % s], w=['Cbf%d' % ((c + 1) % RING)])
            sl = slice(b * CB, (b + 1) * CB)
            P.op('dve', lambda e, sl=sl, s=s: e.tensor_tensor(tn[s][:], hr[s][:, :, 128], eb[:, sl], ALU.mult),
                 r=['hr%d' % s, 'eb'], w=['tn%d' % s])
            P.op('act', lambda e, s=s: e.activation(tn[s][:], tn[s][:], AF.Abs), r=['tn%d' % s], w=['tn%d' % s])
            P.op('dve', lambda e, s=s: e.tensor_scalar(tn[s][:], tn[s][:], 1.0, None, ALU.max), r=['tn%d' % s], w=['tn%d' % s])
            P.op('dve', lambda e, s=s: e.reciprocal(tn[s][:], tn[s][:]), r=['tn%d' % s], w=['tn%d' % s])
            P.op('dve', lambda e, sl=sl, s=s: e.tensor_tensor(tn[s][:], tn[s][:], eb[:, sl], ALU.mult),
                 r=['tn%d' % s, 'eb'], w=['tn%d' % s])
            P.op('dve', lambda e, s=s: e.tensor_tensor(ho[s][:], hr[s][:, :, 0:128],
                                                  tn[s][:].unsqueeze(2).to_broadcast([64, CB, 128]), ALU.mult),
                 r=['hr%d' % s, 'tn%d' % s], w=['ho'])
            P.dma('sp', H[:, sl, :], ho[s][:], r=['ho'])
        P.finish()
    return nc


def ln_tile(P, z, zkey, st, mv, rs, sfx, gB=None, bB=None, gkeys=()):
    for hh in range(2):
        P.op('dve', lambda e, hh=hh: e.bn_stats(st[:, hh * 6:(hh + 1) * 6], z[:, hh * 512:(hh + 1) * 512]),
             r=[zkey], w=['st' + sfx])
    P.op('dve', lambda e: e.bn_aggr(mv[:], st[:]), r=['st' + sfx], w=['mv' + sfx])
    ln_rstd(P, mv[:, 1:2], rs[:], ['mv' + sfx], 'rs' + sfx)
    P.op('dve', lambda e: e.tensor_scalar(z[:], z[:], mv[:, 0:1], rs[:, 0:1], ALU.subtract, ALU.mult),
         r=[zkey, 'mv' + sfx, 'rs' + sfx], w=[zkey])
    if gB is not None:
        P.op('dve', lambda e: e.tensor_tensor(z[:], z[:], gB[:], ALU.mult), r=[zkey] + list(gkeys), w=[zkey])
        P.op('dve', lambda e: e.tensor_tensor(z[:], z[:], bB[:], ALU.add), r=[zkey] + list(gkeys), w=[zkey])


def transpose_to(P, src, skey, nch, tp, idt, dst_fn, dkeys, evac):
    for k in range(nch):
        P.op('pe', lambda e, k=k: e.transpose(tp[:, k, :], src[:, k * 128:(k + 1) * 128], idt[:]),
             r=[skey, 'idt'], w=['tp%d' % (k // 4)])
    for k in range(nch):
        evac(k, tp[:, k, :], 'tp%d' % (k // 4))


def build_C1():
    nc = bass.Bass("TRN2", target_bir_lowering=False)
    xd = dram_in(nc, "x", [TC, D])
    od = dram_in(nc, "o", [TC, 8, 65])
    hfd = dram_in(nc, "hf", [TC, 512])
    hbd = dram_in(nc, "hb", [TC, 512])
    spod = dram_in(nc, "spo", [TC, 512], BF16)
    spmd = dram_in(nc, "spm", [TC, 2048], BF16)
    ident = dram_in(nc, "ident", [128, 128])
    wmla_d = dram_in(nc, "wmla", [128, 4, D])
    wmls_d = dram_in(nc, "wmls", [128, 4, D])
    wout_d = dram_in(nc, "wout", [128, 8, D])
    gmh_d = dram_in(nc, "gmhB", [128, 512])
    g1_d = dram_in(nc, "g1B", [128, 2, D])
    lng_d = dram_in(nc, "lngB", [128, D])
    lnb_d = dram_in(nc, "lnbB", [128, D])
    xo = dram_out(nc, "xo", [TC, D])
    with ExitStack() as es:
        P = Prog(nc, es)
        idt = P.sb([128, 128], F32)
        wmla = P.sb([128, 4, D], BF16)
        wmls = P.sb([128, 4, D], BF16)
        wout = P.sb([128, 8, D], BF16)
        gmh = P.sb([128, 512], F32)
        g1B = P.sb([128, 2, D], F32)
        lng = P.sb([128, D], F32)
        lnb = P.sb([128, D], F32)
        xs = [P.sb([128, D], F32) for _ in range(2)]
        os_ = [P.sb([128, 8, 65], F32) for _ in range(2)]
        hf = [P.sb([128, 512], F32) for _ in range(2)]
        hb = [P.sb([128, 512], F32) for _ in range(2)]
        po = [P.sb([128, 512], BF16) for _ in range(2)]
        pm = [P.sb([128, 2048], BF16) for _ in range(2)]
        rec = [P.sb([128, 8], F32) for _ in range(2)]
        on = [P.sb([128, 512], F32) for _ in range(2)]
        onT = [P.sb([128, 4, 128], BF16) for _ in range(2)]
        hnT = [P.sb([128, 4, 128], BF16) for _ in range(2)]
        ymT = P.sb([128, 8, 128], BF16)
        s4 = [P.sb([128, 4], F32) for _ in range(2)]
        v4 = [P.sb([128, 4], F32) for _ in range(2)]
        cen = [P.sb([128, 512], F32) for _ in range(2)]
        sqv = [P.sb([128, 512], F32) for _ in range(2)]
        ta = P.sb([128, D], F32)
        tb = P.sb([128, D], F32)
        st = P.sb([128, 12], F32)
        mv = P.sb([128, 2], F32)
        rs = P.sb([128, 1], F32)
        tp = P.ps([128, 8, 128], F32)
        pA = P.ps([128, 2, 512], F32)
        pB = P.ps([128, 2, 512], F32)
        pY = P.ps([128, 2, 512], F32)
        P.dma('sp', idt[:], ident, w=['idt'])
        P.dma('sp', gmh[:], gmh_d, w=['gmh'])
        P.dma('sp', g1B[:], g1_d, w=['g1B'])
        P.dma('sp', lng[:], lng_d, w=['lng'])
        P.dma('sp', lnb[:], lnb_d, w=['lng'])
        for k in range(4):
            P.dma('pool', wmla[:, k, :], wmla_d[:, k, :], w=['wmla'])
            P.dma('pool', wmls[:, k, :], wmls_d[:, k, :], w=['wmls'])
        for k in range(8):
            P.dma('pool', wout[:, k, :], wout_d[:, k, :], w=['wout'])

        def evac_to(dstT, dkey):
            def f(k, src, skey):
                P.op('act', lambda e: e.activation(dstT[:, k, :], src, AF.Copy), r=[skey], w=[dkey])
            return f

        def proj(ps, pskey, lT, lkey, w, wkey, nk):
            for half in range(2):
                for k in range(nk):
                    P.op('pe', lambda e, half=half, k=k: e.matmul(ps[:, half, :], lT[:, k, :], w[:, k, half * 512:(half + 1) * 512],
                                                                 start=(k == 0), stop=(k == nk - 1)),
                         r=[lkey, wkey], w=[pskey + str(half)])

        def stageX(i):
            b = i % 2
            rows = slice(i * 128, (i + 1) * 128)
            P.dma('sp', os_[b][:], od[rows], w=['o%d' % b])
            P.dma('sp', hf[b][:], hfd[rows], w=['hf%d' % b])
            P.dma('sp', hb[b][:], hbd[rows], w=['hb%d' % b])
            P.dma('sp', po[b][:], spod[rows], w=['po%d' % b])
            P.dma('sp', pm[b][:], spmd[rows], w=['pm%d' % b])
            P.dma('sp', xs[b][:], xd[rows], w=['xs%d' % b])
            on_, onT_, hnT_, cen_, sqv_, rec_, s4_, v4_ = on[b], onT[b], hnT[b], cen[b], sqv[b], rec[b], s4[b], v4[b]
            kb = str(b)
            P.op('dve', lambda e: e.reciprocal(rec_[:], os_[b][:, :, 64]), r=['o' + kb], w=['rec' + kb])
            P.op('dve', lambda e: e.tensor_tensor(on_[:].rearrange("p (h d) -> p h d", h=8), os_[b][:, :, 0:64],
                                                  rec_[:].unsqueeze(2).to_broadcast([128, 8, 64]), ALU.mult),
                 r=['o' + kb, 'rec' + kb], w=['on' + kb])
            transpose_to(P, on_, 'on' + kb, 4, tp, idt, None, None, evac_to(onT_, 'onT' + kb))
            P.op('dve', lambda e: e.tensor_tensor(cen_[:], hf[b][:], hb[b][:], ALU.add), r=['hf' + kb, 'hb' + kb], w=['cen' + kb])
            c3 = cen_[:].rearrange("p (h d) -> p h d", h=4)
            P.op('dve', lambda e: e.tensor_reduce(s4_[:], c3, AX.X, ALU.add), r=['cen' + kb], w=['s4' + kb])
            P.op('dve', lambda e: e.tensor_scalar(s4_[:], s4_[:], -1.0 / 128, None, ALU.mult), r=['s4' + kb], w=['s4' + kb])
            P.op('dve', lambda e: e.tensor_tensor(c3, c3, s4_[:].unsqueeze(2).to_broadcast([128, 4, 128]), ALU.add),
                 r=['cen' + kb, 's4' + kb], w=['cen' + kb])
            P.op('dve', lambda e: e.tensor_tensor(sqv_[:], cen_[:], cen_[:], ALU.mult), r=['cen' + kb], w=['sqv' + kb])
            P.op('dve', lambda e: e.tensor_reduce(v4_[:], sqv_[:].rearrange("p (h d) -> p h d", h=4), AX.X, ALU.add),
                 r=['sqv' + kb], w=['v4' + kb])
            ln_rstd(P, v4_[:], v4_[:], ['v4' + kb], 'v4' + kb, scale=1.0 / 128)
            P.op('dve', lambda e: e.tensor_tensor(c3, c3, v4_[:].unsqueeze(2).to_broadcast([128, 4, 128]), ALU.mult),
                 r=['cen' + kb, 'v4' + kb], w=['cen' + kb])
            P.op('dve', lambda e: e.tensor_tensor(cen_[:], cen_[:], gmh[:], ALU.mult), r=['cen' + kb, 'gmh'], w=['cen' + kb])
            P.op('dve', lambda e: e.tensor_tensor(cen_[:], cen_[:], po[b][:], ALU.mult), r=['cen' + kb, 'po' + kb], w=['cen' + kb])
            transpose_to(P, cen_, 'cen' + kb, 4, tp, idt, None, None, evac_to(hnT_, 'hnT' + kb))

        def stageY(i):
            b = i % 2
            kb = str(b)
            sel = 1 if i < 2 else 0
            rows = slice(i * 128, (i + 1) * 128)
            proj(pA, 'pA', onT[b], 'onT' + kb, wmla, 'wmla', 4)
            proj(pB, 'pB', hnT[b], 'hnT' + kb, wmls, 'wmls', 4)
            for half in range(2):
                hs = slice(half * 512, (half + 1) * 512)
                P.op('dve', lambda e, half=half, hs=hs: e.tensor_tensor(ta[:, hs], pA[:, half, :], pm[b][:, hs], ALU.mult),
                     r=['pA%d' % half, 'pm' + kb], w=['ta'])
                P.op('dve', lambda e, half=half, hs=hs: e.tensor_tensor(
                    tb[:, hs], pB[:, half, :], pm[b][:, 1024 + half * 512:1024 + (half + 1) * 512], ALU.mult),
                    r=['pB%d' % half, 'pm' + kb], w=['tb'])
            P.op('dve', lambda e: e.tensor_tensor(ta[:], ta[:], tb[:], ALU.add), r=['ta', 'tb'], w=['ta'])
            transpose_to(P, ta, 'ta', 8, tp, idt, None, None, evac_to(ymT, 'ymT'))
            proj(pY, 'pY', ymT, 'ymT', wout, 'wout', 8)
            for half in range(2):
                hs = slice(half * 512, (half + 1) * 512)
                P.op('dve', lambda e, half=half, hs=hs: e.tensor_tensor(tb[:, hs], pY[:, half, :], g1B[:, sel, hs], ALU.mult),
                     r=['pY%d' % half, 'g1B'], w=['tb'])
            P.op('dve', lambda e: e.scalar_tensor_tensor(tb[:], xs[b][:], ALPHA, tb[:], ALU.mult, ALU.add),
                 r=['xs' + kb, 'tb'], w=['tb'])
            ln_tile(P, tb, 'tb', st, mv, rs, '', lng, lnb, ['lng'])
            P.dma('sp', xo[rows], tb[:], r=['tb'])

        stageX(0)
        for i in range(NTILE):
            if i + 1 < NTILE:
                stageX(i + 1)
            stageY(i)
        P.finish()
    return nc


def bcast(v, p=128):
    return np.ascontiguousarray(np.broadcast_to(np.asarray(v, np.float32).reshape(1, -1), (p, np.asarray(v).size)))


def prep_C1_weights(w_bo_mla, w_bo_mlstm, w_out, g_mh, ln1_g, ln1_b, g1_lat, g1_ctx):
    return {"wmla": kp(w_bo_mla, 4), "wmls": kp(w_bo_mlstm, 4), "wout": kp(w_out, 8),
            "gmhB": bcast(g_mh), "lngB": bcast(ln1_g), "lnbB": bcast(ln1_b),
            "g1B": np.ascontiguousarray(np.stack([bcast(g1_lat), bcast(g1_ctx)], axis=1)),
            "ident": np.eye(128, dtype=np.float32)}


NEXP = 32
DEXP = 256


def build_C2():
    nc = bass.Bass("TRN2", target_bir_lowering=False)
    xd = dram_in(nc, "x", [TC, D])
    ident = dram_in(nc, "ident", [128, 128])
    mod2 = dram_in(nc, "mod2", [128, 8, 4])
    wr_d = dram_in(nc, "wr", [128, 8, 36])
    br_d = dram_in(nc, "brB", [128, 36])
    g2_d = dram_in(nc, "g2B", [128, 2, D])
    lng_d = dram_in(nc, "lngB", [128, D])
    lnb_d = dram_in(nc, "lnbB", [128, D])
    wg_d = dram_in(nc, "wg", [NEXP, 128, 8, DEXP])
    wu_d = dram_in(nc, "wu", [NEXP, 128, 8, DEXP])
    wd_d = dram_in(nc, "wd", [NEXP, 128, 2, D])
    xo = dram_out(nc, "xo", [TC, D])
    with ExitStack() as es:
        P = Prog(nc, es)
        idt = P.sb([128, 128], F32)
        mods = P.sb([128, 8, 4], F32)
        wr = P.sb([128, 8, 36], F32)
        brB = P.sb([128, 36], F32)
        g2B = P.sb([128, 2, D], F32)
        lng = P.sb([128, D], F32)
        lnb = P.sb([128, D], F32)
        h2T = P.sb([128, 8, TC], BF16)
        hTf = P.sb([128, 8, 128], F32)
        yacc = P.sb([128, NTILE, D], F32)
        comb_all = P.sb([128, NTILE, 32], F32)
        wg = [P.sb([128, 8, DEXP], BF16) for _ in range(2)]
        wu = [P.sb([128, 8, DEXP], BF16) for _ in range(2)]
        wd = [P.sb([128, 2, D], BF16) for _ in range(2)]
        xs = [P.sb([128, D], F32) for _ in range(2)]
        st = P.sb([128, 12], F32)
        mv = P.sb([128, 2], F32)
        rs = P.sb([128, 1], F32)
        lg = P.sb([128, 36], F32)
        sm = [P.sb([128, 8], F32, name="sm%d" % j) for j in range(12)]
        selg = P.sb([128, 4, 8], F32)
        sa = [P.sb([128, 512], F32) for _ in range(2)]
        hid = [P.sb([128, 2, 512], BF16) for _ in range(2)]
        tp = P.ps([128, 8, 128], F32)
        pA = [P.ps([128, 512], F32) for _ in range(2)]
        pU = [P.ps([128, 512], F32) for _ in range(2)]
        pY = [P.ps([128, 512], F32) for _ in range(2)]
        pC = pY[0]
        P.dma('sp', idt[:], ident, w=['idt'])
        P.dma('sp', mods[:], mod2, w=['mods'])
        P.dma('sp', wr[:], wr_d, w=['wr'])
        P.dma('sp', brB[:], br_d, w=['brB'])
        P.dma('sp', g2B[:], g2_d, w=['g2B'])
        P.dma('sp', lng[:], lng_d, w=['lng'])
        P.dma('sp', lnb[:], lnb_d, w=['lng'])
        P.op('dve', lambda e: e.tensor_scalar(mods[:, :, 0:1], mods[:, :, 0:1], 1.0, None, ALU.add), r=['mods'], w=['mods'])
        P.op('dve', lambda e: e.tensor_scalar(mods[:, :, 2:3], mods[:, :, 2:3], 1.0, None, ALU.add), r=['mods'], w=['mods'])

        def load_expert(e_):
            s = e_ % 2
            for k0 in range(0, 8, 4):
                P.dma('pool', wg[s][:, k0:k0 + 4, :], wg_d[e_, :, k0:k0 + 4, :], w=['wg%d' % s])
                P.dma('pool', wu[s][:, k0:k0 + 4, :], wu_d[e_, :, k0:k0 + 4, :], w=['wu%d' % s])
            for f in range(2):
                P.dma('pool', wd[s][:, f, :], wd_d[e_, :, f, :], w=['wd%d' % s])

        load_expert(0)
        for i in range(NTILE):
            b = i % 2
            msel = 2 if i < 2 else 0
            rows = slice(i * 128, (i + 1) * 128)
            P.dma('sp', xs[b][:], xd[rows], w=['xs%d' % b])
            ln_tile(P, xs[b], 'xs%d' % b, st, mv, rs, '')

            def evac(k, src, skey, i=i, msel=msel):
                P.op('act', lambda e: e.activation(h2T[:, k, i * 128:(i + 1) * 128], src, AF.Identity,
                                                   bias=mods[:, k, msel + 1:msel + 2], scale=mods[:, k, msel:msel + 1]),
                     r=[skey, 'mods'], w=['h2T%d' % i])
                P.op('act', lambda e: e.activation(hTf[:, k, :], src, AF.Identity,
                                                   bias=mods[:, k, msel + 1:msel + 2], scale=mods[:, k, msel:msel + 1]),
                     r=[skey, 'mods'], w=['hTf'])
            transpose_to(P, xs[b], 'xs%d' % b, 8, tp, idt, None, None, evac)
            for k in range(8):
                P.op('pe', lambda e, k=k: e.matmul(pC[:, 0:36], hTf[:, k, :], wr[:, k, :], start=(k == 0), stop=(k == 7)),
                     r=['hTf', 'wr'], w=['pY0'])
            P.op('dve', lambda e: e.tensor_tensor(lg[:], pC[:, 0:36], brB[:], ALU.add), r=['pY0', 'brB'], w=['lg'])
            gmax, ohg, negm, eg, sume, lsel, m1, mk1 = [sm[j] for j in range(8)]
            l2, m2, mk2, tt = sm[8], sm[9], sm[10], sm[11]
            lgG = lg[:, 0:4]
            lgE = lg[:, 4:36].rearrange("p (g e) -> p g e", g=4)

            def dv(fn, r, w):
                P.op('dve', fn, r=r, w=w)
            dv(lambda e: e.tensor_reduce(gmax[:, 0:1], lgG, AX.X, ALU.max), ['lg'], ['gmax'])
            dv(lambda e: e.tensor_scalar(ohg[:, 0:4], lgG, gmax[:, 0:1], None, ALU.is_equal), ['lg', 'gmax'], ['ohg'])
            dv(lambda e: e.tensor_scalar(negm[:, 0:1], gmax[:, 0:1], -1.0, None, ALU.mult), ['gmax'], ['negm'])
            P.op('act', lambda e: e.activation(eg[:, 0:4], lgG, AF.Exp, bias=negm[:, 0:1], scale=1.0), r=['lg', 'negm'], w=['eg'])
            dv(lambda e: e.tensor_reduce(sume[:, 0:1], eg[:, 0:4], AX.X, ALU.add), ['eg'], ['sume'])
            dv(lambda e: e.reciprocal(sume[:, 0:1], sume[:, 0:1]), ['sume'], ['sume'])
            dv(lambda e: e.tensor_tensor(selg[:], lgE, ohg[:, 0:4].unsqueeze(2).to_broadcast([128, 4, 8]), ALU.mult),
               ['lg', 'ohg'], ['selg'])
            dv(lambda e: e.tensor_reduce(lsel[:], selg[:].rearrange("p g e -> p e g"), AX.X, ALU.add), ['selg'], ['lsel'])
            dv(lambda e: e.tensor_reduce(m1[:, 0:1], lsel[:], AX.X, ALU.max), ['lsel'], ['m1'])
            dv(lambda e: e.tensor_scalar(mk1[:], lsel[:], m1[:, 0:1], None, ALU.is_equal), ['lsel', 'm1'], ['mk1'])
            dv(lambda e: e.scalar_tensor_tensor(l2[:], mk1[:], -1e30, lsel[:], ALU.mult, ALU.add), ['mk1', 'lsel'], ['l2'])
            dv(lambda e: e.tensor_reduce(m2[:, 0:1], l2[:], AX.X, ALU.max), ['l2'], ['m2'])
            dv(lambda e: e.tensor_scalar(mk2[:], l2[:], m2[:, 0:1], None, ALU.is_equal), ['l2', 'm2'], ['mk2'])
            dv(lambda e: e.tensor_tensor(tt[:, 0:1], m2[:, 0:1], m1[:, 0:1], ALU.subtract), ['m2', 'm1'], ['tt'])
            P.op('act', lambda e: e.activation(tt[:, 1:2], tt[:, 0:1], AF.Exp), r=['tt'], w=['tt'])
            dv(lambda e: e.tensor_scalar(tt[:, 2:3], tt[:, 1:2], 1.0, None, ALU.add), ['tt'], ['tt'])
            dv(lambda e: e.reciprocal(tt[:, 2:3], tt[:, 2:3]), ['tt'], ['tt'])
            dv(lambda e: e.tensor_tensor(tt[:, 3:4], tt[:, 2:3], sume[:, 0:1], ALU.mult), ['tt', 'sume'], ['tt'])
            dv(lambda e: e.tensor_tensor(tt[:, 4:5], tt[:, 3:4], tt[:, 1:2], ALU.mult), ['tt'], ['tt'])
            dv(lambda e: e.tensor_scalar(mk1[:], mk1[:], tt[:, 3:4], None, ALU.mult), ['mk1', 'tt'], ['mk1'])
            dv(lambda e: e.scalar_tensor_tensor(mk2[:], mk2[:], tt[:, 4:5], mk1[:], ALU.mult, ALU.add), ['mk2', 'tt', 'mk1'], ['mk2'])
            dv(lambda e, i=i: e.tensor_tensor(comb_all[:, i, :].rearrange("p (g e) -> p g e", g=4),
                                              ohg[:, 0:4].unsqueeze(2).to_broadcast([128, 4, 8]),
                                              mk2[:].unsqueeze(1).to_broadcast([128, 4, 8]), ALU.mult), ['ohg', 'mk2'], ['comb%d' % i])

        stepn = [0]
        pending = [None]
        for e_ in range(NEXP):
            s = e_ % 2
            if pending[0] is not None:
                pending[0]()
                pending[0] = None
            if e_ + 1 < NEXP:
                load_expert(e_ + 1)
            for (t0, n) in GROUPS:
                hb_ = stepn[0] % 2
                stepn[0] += 1
                tiles = ['h2T%d' % t for t in range(t0 // 128, (t0 + n) // 128)]
                for fc in range(2):
                    ab = (2 * stepn[0] + fc) % 2
                    for k in range(8):
                        P.op('pe', lambda e, k=k, fc=fc, ab=ab, s=s, t0=t0, n=n: e.matmul(
                            pA[ab][:, 0:n], wg[s][:, k, fc * 128:(fc + 1) * 128], h2T[:, k, t0:t0 + n],
                            start=(k == 0), stop=(k == 7)), r=['wg%d' % s] + tiles, w=['pA%d' % ab])
                    for k in range(8):
                        P.op('pe', lambda e, k=k, fc=fc, ab=ab, s=s, t0=t0, n=n: e.matmul(
                            pU[ab][:, 0:n], wu[s][:, k, fc * 128:(fc + 1) * 128], h2T[:, k, t0:t0 + n],
                            start=(k == 0), stop=(k == 7)), r=['wu%d' % s] + tiles, w=['pU%d' % ab])
                    P.op('act', lambda e, ab=ab, n=n: e.activation(sa[ab][:, 0:n], pA[ab][:, 0:n], AF.Silu),
                         r=['pA%d' % ab], w=['sa%d' % ab])
                    P.op('dve', lambda e, ab=ab, n=n, fc=fc, hb_=hb_: e.tensor_tensor(hid[hb_][:, fc, 0:n], sa[ab][:, 0:n], pU[ab][:, 0:n], ALU.mult),
                         r=['sa%d' % ab, 'pU%d' % ab], w=['hid%d' % hb_])
                def down(e_=e_, s=s, t0=t0, n=n, hb_=hb_):
                    for tt_ in range(n // 128):
                        ti = t0 // 128 + tt_
                        for half in range(2):
                            yb = (2 * ti + half) % 2
                            for fc in range(2):
                                P.op('pe', lambda e, fc=fc, half=half, yb=yb, tt_=tt_: e.matmul(
                                    pY[yb][:, :], hid[hb_][:, fc, tt_ * 128:(tt_ + 1) * 128], wd[s][:, fc, half * 512:(half + 1) * 512],
                                    start=(fc == 0), stop=(fc == 1)), r=['hid%d' % hb_, 'wd%d' % s], w=['pY%d' % yb])
                            ysl = yacc[:, ti, half * 512:(half + 1) * 512]
                            cw = comb_all[:, ti, e_:e_ + 1]
                            if e_ == 0:
                                P.op('dve', lambda e, yb=yb, ysl=ysl, cw=cw: e.tensor_scalar(ysl, pY[yb][:, :], cw, None, ALU.mult),
                                     r=['pY%d' % yb, 'comb%d' % ti], w=['yacc%d' % ti])
                            else:
                                P.op('dve', lambda e, yb=yb, ysl=ysl, cw=cw: e.scalar_tensor_tensor(ysl, pY[yb][:, :], cw, ysl, ALU.mult, ALU.add),
                                     r=['pY%d' % yb, 'yacc%d' % ti, 'comb%d' % ti], w=['yacc%d' % ti])
                if pending[0] is not None:
                    pending[0]()
                pending[0] = down
        pending[0]()
        for i in range(NTILE):
            b = i % 2
            gs = 1 if i < 2 else 0
            rows = slice(i * 128, (i + 1) * 128)
            P.dma('sp', xs[b][:], xd[rows], w=['xs%d' % b])
            P.op('dve', lambda e, i=i, gs=gs: e.tensor_tensor(yacc[:, i, :], yacc[:, i, :], g2B[:, gs, :], ALU.mult),
                 r=['yacc%d' % i, 'g2B'], w=['yacc%d' % i])
            P.op('dve', lambda e, i=i, b=b: e.scalar_tensor_tensor(xs[b][:], xs[b][:], ALPHA, yacc[:, i, :], ALU.mult, ALU.add),
                 r=['xs%d' % b, 'yacc%d' % i], w=['xs%d' % b])
            ln_tile(P, xs[b], 'xs%d' % b, st, mv, rs, '', lng, lnb, ['lng'])
            P.dma('sp', xo[rows], xs[b][:], r=['xs%d' % b])
        P.finish()
    return nc


def prep_C2_weights(w_rg, b_rg, w_re, b_re, w_e_gate, w_e_up, w_e_down, ln2_g, ln2_b, g2_lat, g2_ctx):
    wr = np.concatenate([w_rg, w_re], axis=1)
    br = np.concatenate([b_rg, b_re], axis=0)
    return {"wr": kp(wr, 8), "brB": bcast(br),
            "wg": np.ascontiguousarray(w_e_gate.reshape(NEXP, 8, 128, DEXP).transpose(0, 2, 1, 3)),
            "wu": np.ascontiguousarray(w_e_up.reshape(NEXP, 8, 128, DEXP).transpose(0, 2, 1, 3)),
            "wd": np.ascontiguousarray(w_e_down.reshape(NEXP, 2, 128, D).transpose(0, 2, 1, 3)),
            "lngB": bcast(ln2_g), "lnbB": bcast(ln2_b),
            "g2B": np.ascontiguousarray(np.stack([bcast(g2_lat), bcast(g2_ctx)], axis=1)),
            "ident": np.eye(128, dtype=np.float32)}


_PROGS = {}


def _prog(name):
    if name not in _PROGS:
        _PROGS[name] = {"M": build_M, "A": build_A, "B1": build_B1, "B2": build_B2, "C1": build_C1, "C2": build_C2}[name]()
    return _PROGS[name]


def _run(name, in_maps):
    res = run_bass_kernel_spmd(_prog(name), in_maps, core_ids=list(range(NCORE)))
    return res.results


def _gather_tok(outs, key, axis):
    parts = [np.take(outs[0][key], np.arange(CTX), axis=axis)]
    for i in range(NCORE):
        parts.append(np.take(outs[i][key], np.arange(CTX, TC), axis=axis))
    return np.concatenate(parts, axis=axis)


def kernel(x, c, ctx, c_ctx, w_ada, b_ada, w_in, b_gates, w_uq, w_uk, w_uv, g_qn, g_kvn, g_mh,
           w_bo_mla, w_bo_mlstm, w_out, ln1_g, ln1_b, w_rg, b_rg, w_re, b_re,
           w_e_gate, w_e_up, w_e_down, ln2_g, ln2_b):
    f32 = np.float32
    x = np.asarray(x, f32)
    ctx = np.asarray(ctx, f32)
    ident = np.eye(128, dtype=f32)
    ones128 = np.ones((128, 128), f32)
    cc = np.ascontiguousarray(np.stack([np.asarray(c, f32)[0].reshape(8, 128).T, np.asarray(c_ctx, f32).reshape(8, 128).T], axis=-1))
    wall = np.concatenate([np.asarray(w_ada[l], f32) for l in range(DEPTH)], axis=1)
    ball = np.concatenate([np.asarray(b_ada[l], f32) for l in range(DEPTH)], axis=0)
    ins = []
    for i in range(NCORE):
        ins.append({"cc": cc, "wa": kp(wall[:, i * 3072:(i + 1) * 3072], 8),
                    "ba": np.ascontiguousarray(ball[i * 3072:(i + 1) * 3072].reshape(24, 128).T)})
    outs = _run("M", ins)
    mod = np.concatenate([o["mo"].transpose(1, 0, 2).reshape(3072, 2) for o in outs], axis=0).reshape(DEPTH, 6 * D, 2)
    del wall, ins

    cosT, ssinT = rope_tables_np()
    toks = [core_tokens(i) for i in range(NCORE)]
    cos4 = [np.ascontiguousarray(np.tile(cosT[t].T, (4, 1))) for t in toks]
    sin4 = [np.ascontiguousarray(np.tile(ssinT[t].T, (4, 1))) for t in toks]
    xc = [np.ascontiguousarray(np.concatenate([ctx[0], x[0, i * LAT_C:(i + 1) * LAT_C]], axis=0)) for i in range(NCORE)]
    perm_f = np.arange(T)
    perm_b = np.concatenate([np.arange(CTX - 1, -1, -1), np.arange(T - 1, CTX - 1, -1)])
    tri = np.triu(np.ones((64, 64), f32))
    ones64 = np.ones((64, 64), f32)
    onescol = np.ones((T, 1), f32)

    for l in range(DEPTH):
        m = mod[l]
        sh1, sc1, g1, sh2, sc2, g2 = [m[j * D:(j + 1) * D] for j in range(6)]
        mod1 = kp(np.stack([sc1[:, 0], sh1[:, 0], sc1[:, 1], sh1[:, 1]], axis=-1), 8)
        mod2 = kp(np.stack([sc2[:, 0], sh2[:, 0], sc2[:, 1], sh2[:, 1]], axis=-1), 8)
        wA = prep_A_weights(np.asarray(w_in[l], f32), np.asarray(w_uq[l], f32), np.asarray(w_uk[l], f32),
                            np.asarray(w_uv[l], f32), np.asarray(g_qn[l], f32), np.asarray(g_kvn[l], f32),
                            np.asarray(b_gates[l], f32))
        ins = []
        for i in range(NCORE):
            d_ = {"x": xc[i], "mod1": mod1, "ident": ident, "ones": ones128, "cos4": cos4[i], "ssin4": sin4[i]}
            d_.update(wA)
            ins.append(d_)
        oA = _run("A", ins)
        del ins, wA
        QT = _gather_tok(oA, "QT", 2).reshape(768, T)
        KnT = _gather_tok(oA, "KnT", 2).reshape(512, T)
        KrT = _gather_tok(oA, "KrT", 1)
        Vall = _gather_tok(oA, "V", 0)
        mqT = _gather_tok(oA, "mqT", 2).reshape(256, T)
        mkT = _gather_tok(oA, "mkT", 2).reshape(256, T)
        mkv = _gather_tok(oA, "mkv", 0)
        graw = _gather_tok(oA, "graw", 1)
        glsg = _gather_tok(oA, "glsg", 1)
        ins = []
        for h in range(8):
            Q = np.concatenate([QT[h * 64:(h + 1) * 64], QT[512 + h * 32:512 + (h + 1) * 32]], axis=0)
            Kk = np.concatenate([KnT[h * 64:(h + 1) * 64], KrT], axis=0)
            Vx = np.concatenate([Vall[:, h * 64:(h + 1) * 64], onescol.astype(Vall.dtype)], axis=1).reshape(NKT, 128, 65).transpose(1, 0, 2)
            ins.append({"Q": np.ascontiguousarray(Q), "K": np.ascontiguousarray(Kk), "V": np.ascontiguousarray(Vx)})
        oB1 = _run("B1", ins)
        ins = []
        for cidx in range(8):
            hd, dr = cidx // 2, cidx % 2
            perm = perm_b if dr else perm_f
            q_ = mqT[hd * 64:(hd + 1) * 64][:, perm]
            k_ = mkT[hd * 64:(hd + 1) * 64][:, perm]
            kt_ = mkv[perm, hd * 64:(hd + 1) * 64].reshape(NCH, 64, 64).transpose(1, 0, 2)
            vx_ = np.concatenate([mkv[perm, 256 + hd * 128:256 + (hd + 1) * 128], onescol.astype(mkv.dtype)], axis=1).reshape(NCH, 64, 129).transpose(1, 0, 2)
            ig_ = graw[(2 * dr) * 4 + hd][perm].reshape(NCH, 64).T
            lf_ = glsg[(2 * dr + 1) * 4 + hd][perm].reshape(NCH, 64).T
            ins.append({"qT": np.ascontiguousarray(q_), "kT": np.ascontiguousarray(k_), "kt": np.ascontiguousarray(kt_),
                        "vx": np.ascontiguousarray(vx_), "ig": np.ascontiguousarray(ig_), "lf": np.ascontiguousarray(lf_),
                        "tri": tri, "ones": ones64})
        oB2 = _run("B2", ins)
        del ins
        hf_all = np.empty((T, 512), f32)
        hb_all = np.empty((T, 512), f32)
        for cidx in range(8):
            hd, dr = cidx // 2, cidx % 2
            hp = oB2[cidx]["H"].transpose(1, 0, 2).reshape(T, 128)
            if dr:
                hb_all[perm_b, hd * 128:(hd + 1) * 128] = hp
            else:
                hf_all[:, hd * 128:(hd + 1) * 128] = hp
        o_all = np.stack([oB1[h]["O"] for h in range(8)], axis=1)
        wC1 = prep_C1_weights(np.asarray(w_bo_mla[l], f32), np.asarray(w_bo_mlstm[l], f32), np.asarray(w_out[l], f32),
                              np.asarray(g_mh[l], f32), np.asarray(ln1_g[l], f32), np.asarray(ln1_b[l], f32), g1[:, 0], g1[:, 1])
        ins = []
        for i in range(NCORE):
            d_ = {"x": xc[i], "o": np.ascontiguousarray(o_all[toks[i]]), "hf": np.ascontiguousarray(hf_all[toks[i]]),
                  "hb": np.ascontiguousarray(hb_all[toks[i]]), "spo": oA[i]["spo"], "spm": oA[i]["spm"]}
            d_.update(wC1)
            ins.append(d_)
        oC1 = _run("C1", ins)
        del ins, oA, o_all, hf_all, hb_all
        wC2 = prep_C2_weights(np.asarray(w_rg[l], f32), np.asarray(b_rg[l], f32), np.asarray(w_re[l], f32), np.asarray(b_re[l], f32),
                              np.asarray(w_e_gate[l], f32), np.asarray(w_e_up[l], f32), np.asarray(w_e_down[l], f32),
                              np.asarray(ln2_g[l], f32), np.asarray(ln2_b[l], f32), g2[:, 0], g2[:, 1])
        wC2["mod2"] = mod2
        ins = []
        for i in range(NCORE):
            d_ = {"x": oC1[i]["xo"]}
            d_.update(wC2)
            ins.append(d_)
        oC2 = _run("C2", ins)
        del ins, wC2
        xc = [np.ascontiguousarray(oC2[i]["xo"]) for i in range(NCORE)]
    out = np.concatenate([xc[i][CTX:] for i in range(NCORE)], axis=0)[None]
    return np.ascontiguousarray(out.astype(np.float32))
```
